# Optimizing a Trainium2 kernel written in Bass

```python
import math
import jax, jax.numpy as jnp
from jax import lax
import numpy as np

D_MODEL = 1024
BATCH = 16
SEQ = 2048
DEPTH = 2

N_MEM = 256
EPS = 1e-6
MIX_WIDTH = D_MODEL
M_HEADS = 4
M_WIDTH = MIX_WIDTH // 2
M_DV = M_WIDTH // M_HEADS
M_DK = M_DV // 2
CONV_K = 4
CHUNK = 64
F_BIAS_LO = 3.0
F_BIAS_HI = 6.0
A_HEADS = 4
A_DV = 128
A_WIDTH = A_HEADS * A_DV
Q_LORA = 256
KV_LORA = 128
NOPE_DIM = 128
ROPE_DIM = 64
A_QK = NOPE_DIM + ROPE_DIM
ROPE_THETA = 10000.0
Q_BLOCK = 128
IN_SPLITS = (M_WIDTH, M_WIDTH, M_WIDTH, M_HEADS, M_HEADS, Q_LORA, KV_LORA, ROPE_DIM)
IN_COLS = sum(IN_SPLITS)
X_HEADS = 4
X_HD = 128
X_WIDTH = X_HEADS * X_HD
D_FF = 4 * D_MODEL

kernel_name = "hybrid_mlstm_mla_memxattn_block"


def rmsnorm(x, g):
    xf = x.astype(jnp.float32)
    y = xf * lax.rsqrt(jnp.mean(xf * xf, axis=-1, keepdims=True) + EPS)
    return (y * g.astype(jnp.float32)).astype(x.dtype)


def rope_tables(positions):
    inv = 1.0 / (ROPE_THETA ** (jnp.arange(0, ROPE_DIM, 2, dtype=jnp.float32) / ROPE_DIM))
    ang = positions.astype(jnp.float32)[..., None] * inv
    return jnp.cos(ang), jnp.sin(ang)


def apply_rope(x, cos, sin):
    extra = x.ndim - 3
    shp = cos.shape[:2] + (1,) * extra + cos.shape[-1:]
    c = cos.reshape(shp)
    s = sin.reshape(shp)
    x1, x2 = jnp.split(x.astype(jnp.float32), 2, axis=-1)
    return jnp.concatenate([x1 * c - x2 * s, x2 * c + x1 * s], axis=-1).astype(x.dtype)


def causal_dwconv(u, w, b):
    C = u.shape[-1]
    y = lax.conv_general_dilated(
        u, w[:, None, :].astype(u.dtype), window_strides=(1,),
        padding=[(CONV_K - 1, 0)], dimension_numbers=('NWC', 'WIO', 'NWC'),
        feature_group_count=C)
    return y + b


def mlstm_chunkwise(q, k, v, ig, fg):
    f32 = jnp.float32
    Bn, H, S, DK = q.shape
    DV = v.shape[-1]
    L = CHUNK
    NC = S // L
    qc = (q.astype(f32) * (DK ** -0.5)).reshape(Bn, H, NC, L, DK)
    kc = k.astype(f32).reshape(Bn, H, NC, L, DK)
    vc = v.astype(f32).reshape(Bn, H, NC, L, DV)
    logf = jax.nn.log_sigmoid(fg.astype(f32)).reshape(Bn, H, NC, L)
    ic = ig.astype(f32).reshape(Bn, H, NC, L)
    b = jnp.cumsum(logf, axis=-1)
    b_tot = b[..., -1]

    a = b_tot[..., None] - b + ic
    m_loc = jnp.max(a, axis=-1)
    wa = jnp.exp(a - m_loc[..., None])
    C_loc = jnp.einsum('bhcl,bhcld,bhcle->bhcde', wa, kc, vc)
    n_loc = jnp.einsum('bhcl,bhcld->bhcd', wa, kc)

    def step(carry, inp):
        C, n, m = carry
        bt, ml, Cl, nl = inp
        m_new = jnp.maximum(bt + m, ml)
        s_old = jnp.exp(bt + m - m_new)
        s_loc = jnp.exp(ml - m_new)
        C_new = s_old[..., None, None] * C + s_loc[..., None, None] * Cl
        n_new = s_old[..., None] * n + s_loc[..., None] * nl
        return (C_new, n_new, m_new), (C, n, m)

    init = (jnp.zeros((Bn, H, DK, DV), f32), jnp.zeros((Bn, H, DK), f32), jnp.zeros((Bn, H), f32))
    xs = (jnp.moveaxis(b_tot, 2, 0), jnp.moveaxis(m_loc, 2, 0),
          jnp.moveaxis(C_loc, 2, 0), jnp.moveaxis(n_loc, 2, 0))
    _, (C_prev, n_prev, m_prev) = lax.scan(step, init, xs)
    C_prev = jnp.moveaxis(C_prev, 0, 2)
    n_prev = jnp.moveaxis(n_prev, 0, 2)
    m_prev = jnp.moveaxis(m_prev, 0, 2)

    causal = jnp.tril(jnp.ones((L, L), dtype=bool))
    Dlog = b[..., :, None] - b[..., None, :] + ic[..., None, :]
    Dlog = jnp.where(causal, Dlog, -jnp.inf)
    inter_log = b + m_prev[..., None]
    m_j = jnp.maximum(inter_log, jnp.max(Dlog, axis=-1))
    Dw = jnp.exp(Dlog - m_j[..., None])
    qk = jnp.einsum('bhcjd,bhcsd->bhcjs', qc, kc) * Dw
    s_inter = jnp.exp(inter_log - m_j)
    num = (s_inter[..., None] * jnp.einsum('bhcjd,bhcde->bhcje', qc, C_prev)
           + jnp.einsum('bhcjs,bhcse->bhcje', qk, vc))
    den = s_inter * jnp.einsum('bhcjd,bhcd->bhcj', qc, n_prev) + jnp.sum(qk, axis=-1)
    h = num / jnp.maximum(jnp.abs(den), jnp.exp(-m_j))[..., None]
    return h.reshape(Bn, H, S, DV).astype(v.dtype)


def causal_block_attention(q, k, v):
    Bn, H, S, Dq = q.shape
    Dv = v.shape[-1]
    nb = S // Q_BLOCK
    scale = Dq ** -0.5
    qb = q.reshape(Bn, H, nb, Q_BLOCK, Dq).transpose(2, 0, 1, 3, 4)
    kpos = jnp.arange(S)

    def one_block(args):
        qi, bi = args
        s = jnp.einsum('bhqd,bhkd->bhqk', qi, k).astype(jnp.float32) * scale
        qpos = bi * Q_BLOCK + jnp.arange(Q_BLOCK)
        s = jnp.where(kpos[None, :] <= qpos[:, None], s, -jnp.inf)
        p = jax.nn.softmax(s, axis=-1)
        return jnp.einsum('bhqk,bhkd->bhqd', p.astype(v.dtype), v)

    out = lax.map(one_block, (qb, jnp.arange(nb)))
    return out.transpose(1, 2, 0, 3, 4).reshape(Bn, H, S, Dv)


def parallel_mixer(h, cos, sin, w_in, conv_w, conv_b, wq_m, wk_m, b_igate, b_fgate, m_out_g,
                   cq_norm_g, ckv_norm_g, w_uq, w_ukv, qk_norm_q, qk_norm_k, a_out_g, w_out):
    Bn, S, _ = h.shape
    proj = h @ w_in
    idx = np.cumsum(IN_SPLITS)[:-1].tolist()
    u, v_m, o_pre, i_pre, f_pre, c_q, c_kv, k_r = jnp.split(proj, idx, axis=-1)

    u_c = jax.nn.silu(causal_dwconv(u, conv_w, conv_b))
    u_h = u_c.reshape(Bn, S, M_HEADS, M_DV)
    q_m = jnp.einsum('bshd,hde->bhse', u_h, wq_m)
    k_m = jnp.einsum('bshd,hde->bhse', u_h, wk_m)
    v_h = v_m.reshape(Bn, S, M_HEADS, M_DV).transpose(0, 2, 1, 3)
    ig = (i_pre + b_igate).transpose(0, 2, 1)
    fg = (f_pre + b_fgate).transpose(0, 2, 1)
    hm = mlstm_chunkwise(q_m, k_m, v_h, ig, fg)
    hm = rmsnorm(hm.transpose(0, 2, 1, 3), m_out_g)
    y_m = jax.nn.sigmoid(o_pre) * hm.reshape(Bn, S, M_WIDTH)

    q_a = (rmsnorm(c_q, cq_norm_g) @ w_uq).reshape(Bn, S, A_HEADS, A_QK)
    kv = (rmsnorm(c_kv, ckv_norm_g) @ w_ukv).reshape(Bn, S, A_HEADS, NOPE_DIM + A_DV)
    k_nope, v_a = jnp.split(kv, [NOPE_DIM], axis=-1)
    q_nope = rmsnorm(q_a[..., :NOPE_DIM], qk_norm_q[:NOPE_DIM])
    q_rope = apply_rope(rmsnorm(q_a[..., NOPE_DIM:], qk_norm_q[NOPE_DIM:]), cos, sin)
    k_nope = rmsnorm(k_nope, qk_norm_k[:NOPE_DIM])
    k_rope = apply_rope(rmsnorm(k_r, qk_norm_k[NOPE_DIM:]), cos, sin)
    k_rope = jnp.broadcast_to(k_rope[:, :, None, :], (Bn, S, A_HEADS, ROPE_DIM))
    q_full = jnp.concatenate([q_nope, q_rope], axis=-1).transpose(0, 2, 1, 3)
    k_full = jnp.concatenate([k_nope, k_rope], axis=-1).transpose(0, 2, 1, 3)
    o_a = causal_block_attention(q_full, k_full, v_a.transpose(0, 2, 1, 3))
    y_a = rmsnorm(o_a.transpose(0, 2, 1, 3), a_out_g).reshape(Bn, S, A_WIDTH)

    return jnp.concatenate([y_m, y_a], axis=-1) @ w_out


def memory_cross_attention(h, mem_n, wq_x, wkv_x, xq_norm_g, xk_norm_g, wo_x):
    Bn, S, _ = h.shape
    Nm = mem_n.shape[1]
    q = rmsnorm((h @ wq_x).reshape(Bn, S, X_HEADS, X_HD), xq_norm_g)
    kv = (mem_n @ wkv_x).reshape(Bn, Nm, 2, X_HEADS, X_HD)
    k = rmsnorm(kv[:, :, 0], xk_norm_g)
    v = kv[:, :, 1]
    s = jnp.einsum('bshd,bnhd->bhsn', q, k).astype(jnp.float32) * (X_HD ** -0.5)
    p = jax.nn.softmax(s, axis=-1)
    o = jnp.einsum('bhsn,bnhd->bshd', p.astype(v.dtype), v).reshape(Bn, S, X_WIDTH)
    return o @ wo_x


def setup_inputs(seed: int = 0) -> dict:
    key = jax.random.key(seed)
    ks = jax.random.split(key, 32)
    f32 = jnp.float32
    nrm = lambda k, shp, scale: jax.random.normal(k, shp, f32) * scale
    gain = lambda k, shp: 1.0 + 0.02 * jax.random.normal(k, shp, f32)
    Ld = DEPTH
    x = jax.random.normal(ks[0], (BATCH, SEQ, D_MODEL), f32)
    mem = jax.random.normal(ks[1], (BATCH, N_MEM, D_MODEL), f32)
    offset = jax.random.randint(ks[2], (BATCH, 1), 0, 1024, dtype=jnp.int32)
    positions = (offset + jnp.arange(SEQ, dtype=jnp.int32)[None, :]).astype(jnp.int32)
    b_f = (jnp.linspace(F_BIAS_LO, F_BIAS_HI, M_HEADS, dtype=f32)[None, :]
           + 0.1 * jax.random.normal(ks[10], (Ld, M_HEADS), f32))
    return {
        "x": x,
        "mem": mem,
        "positions": positions,
        "norm_mix_g": gain(ks[3], (Ld, D_MODEL)),
        "w_in": nrm(ks[4], (Ld, D_MODEL, IN_COLS), D_MODEL ** -0.5),
        "conv_w": nrm(ks[5], (Ld, CONV_K, M_WIDTH), CONV_K ** -0.5),
        "conv_b": nrm(ks[6], (Ld, M_WIDTH), 0.02),
        "wq_m": nrm(ks[7], (Ld, M_HEADS, M_DV, M_DK), M_DV ** -0.5),
        "wk_m": nrm(ks[8], (Ld, M_HEADS, M_DV, M_DK), M_DV ** -0.5),
        "b_igate": nrm(ks[9], (Ld, M_HEADS), 0.1),
        "b_fgate": b_f,
        "m_out_g": gain(ks[11], (Ld, M_HEADS, M_DV)),
        "cq_norm_g": gain(ks[12], (Ld, Q_LORA)),
        "ckv_norm_g": gain(ks[13], (Ld, KV_LORA)),
        "w_uq": nrm(ks[14], (Ld, Q_LORA, A_HEADS * A_QK), Q_LORA ** -0.5),
        "w_ukv": nrm(ks[15], (Ld, KV_LORA, A_HEADS * (NOPE_DIM + A_DV)), KV_LORA ** -0.5),
        "qk_norm_q": gain(ks[16], (Ld, A_QK)),
        "qk_norm_k": gain(ks[17], (Ld, A_QK)),
        "a_out_g": gain(ks[18], (Ld, A_HEADS, A_DV)),
        "w_out": nrm(ks[19], (Ld, MIX_WIDTH, D_MODEL), MIX_WIDTH ** -0.5),
        "norm_x_g": gain(ks[20], (Ld, D_MODEL)),
        "norm_mem_g": gain(ks[21], (Ld, D_MODEL)),
        "wq_x": nrm(ks[22], (Ld, D_MODEL, X_WIDTH), D_MODEL ** -0.5),
        "wkv_x": nrm(ks[23], (Ld, D_MODEL, 2 * X_WIDTH), D_MODEL ** -0.5),
        "xq_norm_g": gain(ks[24], (Ld, X_HD)),
        "xk_norm_g": gain(ks[25], (Ld, X_HD)),
        "wo_x": nrm(ks[26], (Ld, X_WIDTH, D_MODEL), X_WIDTH ** -0.5),
        "norm_ffn_g": gain(ks[27], (Ld, D_MODEL)),
        "w_ff1": nrm(ks[28], (Ld, D_MODEL, D_FF), D_MODEL ** -0.5),
        "w_ff2": nrm(ks[29], (Ld, D_FF, D_MODEL), D_FF ** -0.5),
    }


def reference(x, mem, positions, norm_mix_g, w_in, conv_w, conv_b, wq_m, wk_m, b_igate, b_fgate,
              m_out_g, cq_norm_g, ckv_norm_g, w_uq, w_ukv, qk_norm_q, qk_norm_k, a_out_g, w_out,
              norm_x_g, norm_mem_g, wq_x, wkv_x, xq_norm_g, xk_norm_g, wo_x,
              norm_ffn_g, w_ff1, w_ff2):
    cos, sin = rope_tables(positions)
    for l in range(DEPTH):
        h = rmsnorm(x, norm_mix_g[l])
        x = x + parallel_mixer(h, cos, sin, w_in[l], conv_w[l], conv_b[l], wq_m[l], wk_m[l],
                               b_igate[l], b_fgate[l], m_out_g[l], cq_norm_g[l], ckv_norm_g[l],
                               w_uq[l], w_ukv[l], qk_norm_q[l], qk_norm_k[l], a_out_g[l], w_out[l])
        h = rmsnorm(x, norm_x_g[l])
        mem_n = rmsnorm(mem, norm_mem_g[l])
        x = x + memory_cross_attention(h, mem_n, wq_x[l], wkv_x[l], xq_norm_g[l], xk_norm_g[l], wo_x[l])
        h = rmsnorm(x, norm_ffn_g[l])
        x = x + jnp.square(jax.nn.relu(h @ w_ff1[l])) @ w_ff2[l]
    return x
```

```python
import numpy as np
from contextlib import ExitStack
import concourse.bass as bass
import concourse.mybir as mybir
from concourse.bass_utils import run_bass_kernel_spmd

F32 = mybir.dt.float32
BF16 = mybir.dt.bfloat16
I32 = mybir.dt.int32
AF = mybir.ActivationFunctionType
ALU = mybir.AluOpType
AX = mybir.AxisListType

ENGS = ("pe", "act", "dve", "pool", "sp")

NCORES = 8
SEQ_PER_CORE = 2
S = 2048
D = 1024
NT = S // 128
DEPTH = 2
NMEM = 256
EPS = 1e-6
IN_COLS = 1992
DFF = 4096
FSL = 512
NSL = DFF // FSL


class Buf:
    __slots__ = ("name", "lw", "rd", "alias", "lo", "hi", "excl")

    def __init__(self, name, lo=None, hi=None, excl=False):
        self.name = name
        self.excl = excl
        self.lw = None
        self.rd = {}
        self.alias = [self]
        self.lo = lo
        self.hi = hi


class Op:
    __slots__ = ("idx", "eng", "fn", "deps", "is_dma", "k", "waits", "signal",
                 "semval", "snap", "sem", "dval")


class Prog:
    def __init__(self, nc):
        self.nc = nc
        self.ops = []
        self.dma_sems = {}
        self.sem_count = {}
        self.sem_last = {}
        self.defer = None

    def _add(self, eng, fn, reads, writes, is_dma, k):
        o = Op()
        o.idx = len(self.ops)
        o.eng = eng
        o.fn = fn
        o.is_dma = is_dma
        o.k = k
        o.waits = []
        o.signal = False
        o.semval = None
        o.snap = None
        o.sem = None
        o.dval = None
        deps = set()
        for b in reads:
            for a in b.alias:
                if a.lw is not None:
                    deps.add(a.lw)
            if b.excl:
                for ke, vi in b.rd.items():
                    if ke != eng:
                        deps.add(vi)
        for b in writes:
            for a in b.alias:
                if a.lw is not None:
                    deps.add(a.lw)
                deps.update(a.rd.values())
        for b in reads:
            if is_dma:
                b.rd[("d", o.idx)] = o.idx
            else:
                b.rd[eng] = o.idx
        for b in writes:
            b.lw = o.idx
            b.rd = {}
        if is_dma:
            key = (list(writes) + list(reads))[0]
            skey = id(key)
            if skey not in self.dma_sems:
                self.dma_sems[skey] = "dsem%d" % len(self.dma_sems)
            o.sem = skey
            prev = self.sem_last.get(skey)
            if prev is not None:
                deps.add(prev)
            self.sem_last[skey] = o.idx
            self.sem_count[skey] = self.sem_count.get(skey, 0) + 16 * k
            o.dval = self.sem_count[skey]
        deps.discard(o.idx)
        o.deps = deps
        self.ops.append(o)
        return o

    def op(self, eng, fn, reads=(), writes=()):
        if self.defer is not None:
            self.defer.append((eng, fn, list(reads), list(writes), False, 0))
            return None
        return self._add(eng, fn, reads, writes, False, 0)

    def dma(self, eng, fn, reads=(), writes=(), k=1):
        if self.defer is not None:
            self.defer.append((eng, fn, list(reads), list(writes), True, k))
            return None
        return self._add(eng, fn, reads, writes, True, k)

    def begin(self):
        assert self.defer is None
        self.defer = []

    def end(self):
        lst = self.defer
        self.defer = None
        return lst

    def merge(self, streams):
        streams = [s_ for s_ in streams if s_]
        pos = [0] * len(streams)
        total = sum(len(s_) for s_ in streams)
        for _ in range(total):
            best = None
            for i, s_ in enumerate(streams):
                if pos[i] < len(s_):
                    frac = pos[i] / float(len(s_))
                    if best is None or frac < best[0]:
                        best = (frac, i)
            i = best[1]
            self._add(*streams[i][pos[i]])
            pos[i] += 1

    def schedule(self):
        ops = self.ops
        cur = {e: {} for e in ENGS}
        for o in ops:
            E = o.eng
            clk = cur[E]
            need = {}
            dwaits = []
            for d in o.deps:
                Dp = ops[d]
                if Dp.is_dma:
                    if clk.get(("s", Dp.sem), 0) >= Dp.dval:
                        continue
                    dwaits.append(Dp)
                else:
                    if Dp.eng == "pe" and E == "pe" and not o.is_dma:
                        continue
                    if clk.get(Dp.eng, -1) >= d:
                        continue
                    if need.get(Dp.eng, -1) < d:
                        need[Dp.eng] = d
            if need or dwaits:
                new = dict(clk)
                targets = [ops[d] for d in need.values()] + dwaits
                for Dp in targets:
                    for kk, vv in Dp.snap.items():
                        if new.get(kk, -1) < vv:
                            new[kk] = vv
                    if Dp.is_dma:
                        kk = ("s", Dp.sem)
                        if new.get(kk, 0) < Dp.dval:
                            new[kk] = Dp.dval
                    else:
                        if new.get(Dp.eng, -1) < Dp.idx:
                            new[Dp.eng] = Dp.idx
                        Dp.signal = True
                    o.waits.append(Dp)
                cur[E] = new
            o.snap = cur[E]
        cnt = {e: 0 for e in ENGS}
        for o in ops:
            if not o.is_dma and o.signal:
                cnt[o.eng] += 1
                o.semval = cnt[o.eng]

    def emit(self, stack):
        nc = self.nc
        self.schedule()
        esem = {e: stack.enter_context(nc.semaphore("es_" + e)) for e in ENGS}
        dsem = {k: stack.enter_context(nc.semaphore(n)) for k, n in self.dma_sems.items()}
        block = stack.enter_context(nc.Block())
        by_eng = {e: [] for e in ENGS}
        for o in self.ops:
            by_eng[o.eng].append(o)

        def run(e, name):
            for o in by_eng[name]:
                for Dp in o.waits:
                    if Dp.is_dma:
                        e.wait_ge(dsem[Dp.sem], Dp.dval)
                    else:
                        e.wait_ge(esem[Dp.eng], Dp.semval)
                if o.fn is None:
                    continue
                if o.is_dma:
                    o.fn(e, dsem[o.sem])
                else:
                    ins = o.fn(e)
                    if o.signal:
                        ins.then_inc(esem[name], 1)

        @block.tensor
        def _(e):
            run(e, "pe")

        @block.scalar
        def _(e):
            run(e, "act")

        @block.vector
        def _(e):
            run(e, "dve")

        @block.gpsimd
        def _(e):
            run(e, "pool")

        @block.sync
        def _(e):
            run(e, "sp")


class Arena:
    def __init__(self, nc, st, nbytes):
        self.nbytes = nbytes
        self.t = st.enter_context(nc.sbuf_tensor("arena", [128, nbytes // 4], F32))
        self.bufs = []

    def view(self, name, off, shape, dt, parts=128):
        n = 1
        for s_ in shape:
            n *= s_
        esz = 4 if dt in (F32, I32) else 2
        nb = n * esz
        assert off % 4 == 0 and nb % 4 == 0, (name, off, nb)
        assert off + nb <= self.nbytes, (name, off, nb, self.nbytes)
        ap = self.t[0:parts, off // 4:(off + nb) // 4]
        if dt != F32:
            ap = ap.bitcast(dt)
        if len(shape) == 2:
            ap = ap.rearrange("p (a b) -> p a b", a=shape[0])
        elif len(shape) == 3:
            ap = ap.rearrange("p (a b c) -> p a b c", a=shape[0], b=shape[1])
        b = Buf(name, off, off + nb)
        for o in self.bufs:
            if o.lo < b.hi and b.lo < o.hi:
                o.alias.append(b)
                b.alias.append(o)
        self.bufs.append(b)
        return ap, b


class Bump:
    def __init__(self, arena, lo, hi):
        self.a = arena
        self.lo = lo
        self.hi = hi
        self.p = lo

    def reset(self):
        self.p = self.lo

    def at(self, name, buf, shape, dt, parts=128, off=0):
        return self.a.view(name, buf.lo + off, shape, dt, parts)

    def get(self, name, shape, dt, parts=128):
        n = 1
        for s_ in shape:
            n *= s_
        nb = n * (4 if dt in (F32, I32) else 2)
        nb = (nb + 31) // 32 * 32
        off = self.p
        assert off + nb <= self.hi, ("scratch overflow", name, off, nb, self.hi)
        self.p += nb
        return self.a.view(name, off, shape, dt, parts)


def f_act(out, in_, func, scale=None, bias=None, accum=None):
    kw = {}
    if scale is not None:
        kw["scale"] = scale
    if bias is not None:
        kw["bias"] = bias
    if accum is not None:
        kw["accum_out"] = accum
    return lambda e: e.activation(out=out, in_=in_, func=func, **kw)


def f_ts(out, in0, s1, op0, s2=None, op1=None):
    if op1 is None:
        return lambda e: e.tensor_scalar(out=out, in0=in0, scalar1=s1, scalar2=None, op0=op0)
    return lambda e: e.tensor_scalar(out=out, in0=in0, scalar1=s1, scalar2=s2, op0=op0, op1=op1)


def f_tt(out, in0, in1, op):
    return lambda e: e.tensor_tensor(out=out, in0=in0, in1=in1, op=op)


def f_stt(out, in0, scalar, in1, op0, op1):
    return lambda e: e.scalar_tensor_tensor(out=out, in0=in0, scalar=scalar, in1=in1, op0=op0, op1=op1)


def f_cp(out, in_):
    return lambda e: e.tensor_copy(out=out, in_=in_)


def f_red(out, in_, op=None):
    return lambda e: e.tensor_reduce(out=out, in_=in_, axis=AX.X, op=(op or ALU.add))


def f_recip(out, in_):
    return lambda e: e.reciprocal(out=out, in_=in_)


def f_memset(ap, val):
    return lambda e: e.memset(ap, val)


def f_mm(groups):
    def fn(e):
        ins = None
        for out, pairs in groups:
            n = len(pairs)
            for i, (l, r) in enumerate(pairs):
                ins = e.matmul(out, lhsT=l, rhs=r, start=(i == 0), stop=(i == n - 1))
        return ins
    return fn


def f_tr(items, ident):
    def fn(e):
        ins = None
        for out, in_ in items:
            ins = e.transpose(out=out, in_=in_, identity=ident)
        return ins
    return fn


WNAMES = ["norm_mix_g", "w_in", "conv_w", "conv_b", "wq_m", "wk_m", "b_igate", "b_fgate",
          "m_out_g", "cq_norm_g", "ckv_norm_g", "w_uq", "w_ukv", "qk_norm_q", "qk_norm_k",
          "a_out_g", "w_out", "norm_x_g", "norm_mem_g", "wq_x", "wkv_x", "xq_norm_g",
          "xk_norm_g", "wo_x", "norm_ffn_g", "w_ff1", "w_ff2"]
WSHAPES = {
    "norm_mix_g": [2, 1024], "w_in": [2, 1024, 1992], "conv_w": [2, 4, 512], "conv_b": [2, 512],
    "wq_m": [2, 4, 128, 64], "wk_m": [2, 4, 128, 64], "b_igate": [2, 4], "b_fgate": [2, 4],
    "m_out_g": [2, 4, 128], "cq_norm_g": [2, 256], "ckv_norm_g": [2, 128], "w_uq": [2, 256, 768],
    "w_ukv": [2, 128, 1024], "qk_norm_q": [2, 192], "qk_norm_k": [2, 192], "a_out_g": [2, 4, 128],
    "w_out": [2, 1024, 1024], "norm_x_g": [2, 1024], "norm_mem_g": [2, 1024], "wq_x": [2, 1024, 512],
    "wkv_x": [2, 1024, 1024], "xq_norm_g": [2, 128], "xk_norm_g": [2, 128], "wo_x": [2, 512, 1024],
    "norm_ffn_g": [2, 1024], "w_ff1": [2, 1024, 4096], "w_ff2": [2, 4096, 1024],
}


def build_program(nseq=SEQ_PER_CORE, depth=DEPTH, do_mlstm=True, do_mla=True, do_xattn=True,
                  do_ffn=True):
    nc = bass.Bass("TRN2", target_bir_lowering=False)
    dr = {}
    dr["x"] = nc.dram_tensor("x", [nseq, S, D], F32, kind="ExternalInput").ap()
    dr["mem"] = nc.dram_tensor("mem", [nseq, NMEM, D], F32, kind="ExternalInput").ap()
    dr["pos"] = nc.dram_tensor("pos", [nseq, S], I32, kind="ExternalInput").ap()
    for n in WNAMES:
        dr[n] = nc.dram_tensor(n, WSHAPES[n], F32, kind="ExternalInput").ap()
    dr["c_ident"] = nc.dram_tensor("c_ident", [128, 128], F32, kind="ExternalInput").ap()
    dr["c_triu"] = nc.dram_tensor("c_triu", [128, 128], F32, kind="ExternalInput").ap()
    dr["c_trisl"] = nc.dram_tensor("c_trisl", [128, 128], F32, kind="ExternalInput").ap()
    dr["c_mneg"] = nc.dram_tensor("c_mneg", [128, 128], F32, kind="ExternalInput").ap()
    dr["c_invf"] = nc.dram_tensor("c_invf", [128, 32], F32, kind="ExternalInput").ap()
    out_d = nc.dram_tensor("out", [nseq, S, D], F32, kind="ExternalOutput").ap()

    st = ExitStack()
    with st:
        P = Prog(nc)
        TOTAL = 212800
        A = Arena(nc, st, TOTAL)
        psum = st.enter_context(nc.psum_tensor("ps", [128, 8, 512], F32))
        PB = [Buf("psb%d" % i, excl=True) for i in range(8)]

        off = 0
        X, _ = A.view("X", off, (NT, D), F32)
        XB = []
        for t in range(NT):
            _, b = A.view("X%d" % t, off + t * D * 4, (D,), F32)
            XB.append(b)
        off += NT * D * 4
        R1_LO = off
        R1_SZ = 61568
        off += R1_SZ
        W_LO = off
        W_SZ = 49152
        off += W_SZ
        C_LO = off
        C_SZ = 9216
        off += C_SZ
        S_LO = off
        S_HI = TOTAL
        CB = Bump(A, C_LO, C_LO + C_SZ)
        SB = Bump(A, S_LO, S_HI)
        RB = Bump(A, R1_LO, R1_LO + R1_SZ)

        identf, b_identf = CB.get("identf", (128,), F32)
        identb, b_identb = CB.get("identb", (128,), BF16)
        triu, b_triu = CB.get("triu", (128,), F32)
        trisl, b_trisl = CB.get("trisl", (128,), F32)
        mneg, b_mneg = CB.get("mneg", (128,), F32)
        m01b, b_m01b = CB.get("m01b", (128,), BF16)
        onesb, b_onesb = CB.get("onesb", (128,), BF16)
        onesf, b_onesf = CB.get("onesf", (128,), F32)
        invf, b_invf = CB.get("invf", (32,), F32)
        gcol, b_gcol = CB.get("gcol", (DEPTH, 4, 8), F32)
        convw, b_convw = CB.get("convw", (DEPTH, 4, 4), F32)
        convb, b_convb = CB.get("convb", (DEPTH, 4), F32)
        mog, b_mog = CB.get("mog", (DEPTH, 4), F32)
        aog, b_aog = CB.get("aog", (DEPTH, 4), F32)
        cqg, b_cqg = CB.get("cqg", (DEPTH, 2), F32)
        misc, b_misc = CB.get("misc", (DEPTH, 8), F32)
        gqr, b_gqr = CB.get("gqr", (DEPTH, 64), F32)
        gkr, b_gkr = CB.get("gkr", (DEPTH, 64), F32)
        bif, b_bif = CB.get("bif", (DEPTH, 8), F32)
        cosb, b_cos = CB.get("cos", (NT, 32), F32)
        sinb, b_sin = CB.get("sin", (NT, 32), F32)
        b_const = [b_identf, b_identb, b_triu, b_trisl, b_mneg, b_m01b, b_onesb, b_onesf, b_invf]

        def bc_rows(src_ap_1d, n):
            return src_ap_1d.unsqueeze(0).to_broadcast([128, n])

        def ld_consts(e, s):
            e.dma_start(out=identf, in_=dr["c_ident"]).then_inc(s, 16)
            e.dma_start(out=triu, in_=dr["c_triu"]).then_inc(s, 16)
            e.dma_start(out=trisl, in_=dr["c_trisl"]).then_inc(s, 16)
            e.dma_start(out=mneg, in_=dr["c_mneg"]).then_inc(s, 16)
            e.dma_start(out=invf, in_=dr["c_invf"]).then_inc(s, 16)
        b_cgrp = Buf("cgrp")
        P.dma("sp", ld_consts, writes=[b_cgrp, b_identf, b_triu, b_trisl, b_mneg, b_invf], k=5)
        P.op("dve", f_cp(identb, identf), reads=[b_identf], writes=[b_identb])
        P.op("dve", f_cp(m01b, triu), reads=[b_triu], writes=[b_m01b])
        P.op("pool", f_memset(onesb, 1.0), writes=[b_onesb])
        P.op("pool", f_memset(onesf, 1.0), writes=[b_onesf])

        b_gains = [b_gcol, b_convw, b_convb, b_mog, b_aog, b_cqg, b_misc, b_gqr, b_gkr, b_bif]

        gl = []
        for l in range(DEPTH):
            for wi, nm in enumerate(["norm_mix_g", "norm_x_g", "norm_mem_g", "norm_ffn_g"]):
                gl.append((gcol[:, l, wi, :], dr[nm][l].rearrange("(k p) -> p k", p=128)))
            for j in range(4):
                gl.append((convw[:, l, :, j], dr["conv_w"][l, j].rearrange("(c p) -> p c", p=128)))
            gl.append((convb[:, l, :], dr["conv_b"][l].rearrange("(c p) -> p c", p=128)))
            gl.append((mog[:, l, :], dr["m_out_g"][l].rearrange("h p -> p h")))
            gl.append((aog[:, l, :], dr["a_out_g"][l].rearrange("h p -> p h")))
            gl.append((cqg[:, l, :], dr["cq_norm_g"][l].rearrange("(k p) -> p k", p=128)))
            gl.append((misc[:, l, 0:1], dr["ckv_norm_g"][l].rearrange("(k p) -> p k", p=128)))
            gl.append((misc[:, l, 1:2], dr["qk_norm_q"][l, 0:128].rearrange("(k p) -> p k", p=128)))
            gl.append((misc[:, l, 2:3], dr["qk_norm_k"][l, 0:128].rearrange("(k p) -> p k", p=128)))
            gl.append((misc[:, l, 3:4], dr["xq_norm_g"][l].rearrange("(k p) -> p k", p=128)))
            gl.append((misc[:, l, 4:5], dr["xk_norm_g"][l].rearrange("(k p) -> p k", p=128)))
            gl.append((gqr[:, l, :], bc_rows(dr["qk_norm_q"][l, 128:192], 64)))
            gl.append((gkr[:, l, :], bc_rows(dr["qk_norm_k"][l, 128:192], 64)))
            gl.append((bif[:, l, 0:4], bc_rows(dr["b_igate"][l], 4)))
            gl.append((bif[:, l, 4:8], bc_rows(dr["b_fgate"][l], 4)))

        def ld_gains(e, s):
            with nc.allow_non_contiguous_dma(reason="tiny per-layer gain vectors"):
                for (o_, i_) in gl:
                    e.dma_start(out=o_, in_=i_).then_inc(s, 16)
        b_ggrp = Buf("ggrp")
        P.dma("sp", ld_gains, writes=[b_ggrp] + b_gains, k=len(gl))

        class PSM:
            def __init__(self, banks=None):
                self.free = list(range(8)) if banks is None else list(banks)

            def get(self, n=1):
                if n == 1:
                    for b in self.free:
                        if (b ^ 1) not in self.free:
                            self.free.remove(b)
                            return [b]
                    b = self.free.pop(0)
                    return [b]
                for i, b in enumerate(self.free):
                    if b % 2 == 0 and (b + 1) in self.free:
                        self.free.remove(b)
                        self.free.remove(b + 1)
                        return [b, b + 1]
                raise RuntimeError("no psum pair free: %s" % self.free)

            def rel(self, banks):
                self.free.extend(banks)
        PS = PSM()

        def pbank(b):
            return psum[:, b, :]

        def pbank_bf(b):
            return psum[:, b, :].bitcast(BF16)

        Win, b_Win = A.view("Win", W_LO, (8, IN_COLS), BF16)
        wuq, b_wuq = A.view("wuq", W_LO + 31872, (2, 768), BF16)
        wukv, b_wukv = A.view("wukv", W_LO + 31872 + 3072, (1024,), BF16)
        wqk, b_wqk = A.view("wqk", W_LO + 31872 + 5120, (4, 128), BF16)
        wout, b_wout = A.view("wout", W_LO + 38016, (4, 1024), BF16)
        wqx, b_wqx = A.view("wqx", W_LO, (8, 512), BF16)
        wkvx, b_wkvx = A.view("wkvx", W_LO + 8192, (8, 1024), BF16)
        wox, b_wox = A.view("wox", W_LO + 24576, (4, 1024), BF16)
        fslots = []
        for i, o_ in enumerate([32768, 0, 16384]):
            w1s, b1 = A.view("w1s%d" % i, W_LO + o_, (8, FSL), BF16)
            w2s, b2 = A.view("w2s%d" % i, W_LO + o_ + 8192, (FSL // 128, 1024), BF16)
            fslots.append((w1s, b1, w2s, b2))

        def rstd_from_ms(stv, b_st, src_col, dst_col, n=1):
            P.op("act", f_act(stv[:, dst_col:dst_col + n], stv[:, src_col:src_col + n], AF.Ln, bias=EPS),
                 reads=[b_st], writes=[b_st])
            P.op("act", f_act(stv[:, dst_col:dst_col + n], stv[:, dst_col:dst_col + n], AF.Exp, scale=-0.5),
                 reads=[b_st], writes=[b_st])

        def norm_to_hT(xin_ap, b_xin, gcols, hT_ap, b_hT, scr):
            xnb, b_xnb, stv, b_st = scr
            P.op("act", f_act(xnb, xin_ap, AF.Square, scale=float(D) ** -0.5, accum=stv[:, 0:1]),
                 reads=[b_xin], writes=[b_xnb, b_st])
            rstd_from_ms(stv, b_st, 0, 1)
            P.op("dve", f_ts(xnb, xin_ap, stv[:, 1:2], ALU.mult), reads=[b_xin, b_st], writes=[b_xnb])
            bk = PS.get()
            pT = pbank_bf(bk[0]).rearrange("p (a b) -> p a b", a=8)
            P.op("pe", f_tr([(pT[:, k, :], xnb[:, k * 128:(k + 1) * 128]) for k in range(8)], identb),
                 reads=[b_xnb, b_identb], writes=[PB[bk[0]]])
            P.op("dve", f_tt(hT_ap, pT, gcols.unsqueeze(2).to_broadcast([128, 8, 128]), ALU.mult),
                 reads=[PB[bk[0]], b_ggrp], writes=[b_hT])
            PS.rel(bk)

        def load_w(eng, dst, b_dst, src, k=1):
            P.dma(eng, lambda e, s: e.dma_start(out=dst, in_=src).then_inc(s, 16), writes=[b_dst])

        def resid_add(T, pY2, banks):
            P.op("dve", f_tt(X[:, T, :], X[:, T, :], pY2, ALU.add),
                 reads=[XB[T], PB[banks[0]], PB[banks[1]]], writes=[XB[T]])

        QSC = 192.0 ** -0.5
        XSC = 128.0 ** -0.5
        fslice_ctr = [0]

        def bc3(ap2, shape):
            return ap2.unsqueeze(2).to_broadcast(shape)

        for sq in range(nseq):
            for T in range(NT):
                P.dma("sp", (lambda e, s, T=T, sq=sq: e.dma_start(out=X[:, T, :], in_=dr["x"][sq, T * 128:(T + 1) * 128, :]).then_inc(s, 16)),
                      writes=[XB[T]])
            if do_mla:
                SB.reset()
                posi, b_posi = SB.get("posi", (NT,), I32)
                posf, b_posf = SB.get("posf", (NT,), F32)
                ang, b_ang = SB.get("ang", (NT, 32), F32)
                angi, b_angi = SB.get("angi", (NT, 32), I32)
                angf, b_angf = SB.get("angf", (NT, 32), F32)
                tmpc, b_tmpc = SB.get("tmpc", (NT, 32), F32)

                def ld_pos(e, s, sq=sq):
                    with nc.allow_non_contiguous_dma(reason="positions to token-major columns"):
                        e.dma_start(out=posi, in_=dr["pos"][sq].rearrange("(t p) -> p t", p=128)).then_inc(s, 16)
                P.dma("sp", ld_pos, writes=[b_posi])
                P.op("dve", f_cp(posf, posi), reads=[b_posi], writes=[b_posf])
                P.op("dve", f_tt(ang, posf.unsqueeze(2).to_broadcast([128, NT, 32]),
                                 invf.unsqueeze(1).to_broadcast([128, NT, 32]), ALU.mult),
                     reads=[b_posf, b_invf, b_cgrp], writes=[b_ang])
                for (dst, b_dst, shift) in [(sinb, b_sin, 0.0), (cosb, b_cos, 0.25)]:
                    if shift != 0.0:
                        P.op("dve", f_ts(tmpc, ang, shift, ALU.add), reads=[b_ang], writes=[b_tmpc])
                        src, b_src = tmpc, b_tmpc
                    else:
                        src, b_src = ang, b_ang
                    P.op("dve", f_cp(angi, src), reads=[b_src], writes=[b_angi])
                    P.op("dve", f_cp(angf, angi), reads=[b_angi], writes=[b_angf])
                    P.op("dve", f_tt(angf, src, angf, ALU.subtract), reads=[b_src, b_angf], writes=[b_angf])
                    P.op("dve", f_ts(angi.bitcast(F32), angf, 0.5, ALU.is_gt), reads=[b_angf], writes=[b_angi])
                    P.op("dve", f_tt(angf, angf, angi.bitcast(F32), ALU.subtract), reads=[b_angf, b_angi], writes=[b_angf])
                    P.op("dve", f_ts(angi.bitcast(F32), angf, -0.5, ALU.is_lt), reads=[b_angf], writes=[b_angi])
                    P.op("dve", f_tt(angf, angf, angi.bitcast(F32), ALU.add), reads=[b_angf, b_angi], writes=[b_angf])
                    P.op("act", f_act(dst, angf, AF.Sin, scale=6.28318), reads=[b_angf], writes=[b_dst])

            for l in range(depth):
                if do_mlstm or do_mla:
                    load_w("pool", Win, b_Win, dr["w_in"][l].rearrange("(k p) n -> p k n", p=128))
                    load_w("pool", wuq, b_wuq, dr["w_uq"][l].rearrange("(k p) n -> p k n", p=128))
                    load_w("pool", wukv, b_wukv, dr["w_ukv"][l])
                    load_w("pool", wqk[:, :, 0:64], b_wqk, dr["wq_m"][l].rearrange("h d e -> d h e"))
                    load_w("pool", wqk[:, :, 64:128], b_wqk, dr["wk_m"][l].rearrange("h d e -> d h e"))
                    load_w("pool", wout, b_wout, dr["w_out"][l, 0:512, :].rearrange("(k p) n -> p k n", p=128))

                    RB.reset()
                    qnT, b_qnT = RB.get("qnT", (4, S), BF16)
                    qrT, b_qrT = RB.get("qrT", (2, S), BF16)
                    knT, b_knT = RB.get("knT", (4, S), BF16)
                    krT, b_krT = RB.get("krT", (S,), BF16)
                    va, b_va = RB.get("va", (NT, 4, 129), BF16)
                    SB.reset()
                    xnb, b_xnb = SB.get("xnb", (D,), BF16)
                    stv, b_st = SB.get("st", (32,), F32)
                    hT, b_hT = SB.get("hT", (8, 128), BF16)
                    sm, b_sm = SB.get("sm", (456,), F32)
                    ust, b_ust = SB.get("ust", (4, 131), F32)
                    Cst, b_Cst = SB.get("Cst", (4, 129), F32, parts=64)
                    Cbf, b_Cbf = SB.get("Cbf", (4, 129), BF16, parts=64)
                    g8, b_g8 = SB.get("g8", (64,), F32)
                    vaug, b_vaug = SB.get("vaug", (4, 129), BF16)
                    TA, b_TA = SB.get("TA", (4, 128), F32)
                    TB, b_TB = SB.get("TB", (4, 128), F32)
                    uc, b_uc = SB.get("uc", (4, 128), BF16)
                    og, b_og = SB.get("og", (512,), BF16)
                    qTs, b_qTs = SB.get("qTs", (4, 128), BF16, parts=64)
                    kTs, b_kTs = SB.get("kTs", (4, 128), BF16, parts=64)
                    PT, b_PT = SB.get("PT", (4, 128), BF16)
                    qtil, b_qtil = SB.at("qtil", b_kTs, (4, 128), BF16, parts=64)
                    ktil, b_ktil = SB.at("ktil", b_qTs, (4, 64), BF16)
                    yms = [A.view("ym%d" % i, W_LO + 46208 + i * 1024, (4, 128), BF16) for i in range(2)]
                    junk, b_junk = SB.at("junk", b_qTs, (128,), BF16, off=512)
                    cqn, b_cqn = SB.get("cqn", (256,), BF16)
                    ckvn, b_ckvn = SB.get("ckvn", (128,), BF16)
                    krn, b_krn = SB.get("krn", (64,), F32)
                    kt4, b_kt4 = SB.get("kt4", (4, 32), F32)
                    krot, b_krot = SB.at("krot", b_krn, (2, 64), BF16)
                    cqnT, b_cqnT = SB.at("cqnT", b_cqn, (2, 128), BF16)
                    ckvnT, b_ckvnT = SB.at("ckvnT", b_ckvn, (128,), BF16)
                    stq, b_stq = SB.get("stq", (32,), F32)
                    qr, b_qr = SB.get("qr", (4, 64), F32)
                    qt4, b_qt4 = SB.get("qt4", (2, 4, 32), F32)
                    qrot, b_qrot = SB.get("qrot", (4, 64), BF16)
                    junkk, b_junkk = SB.get("junkk", (128,), BF16)
                    qn, b_qn = SB.at("qn", b_xnb, (4, 128), BF16)
                    kn, b_kn = SB.at("kn", b_xnb, (4, 128), BF16, off=1024)
                    ymT, b_ymT = SB.at("ymT", b_qr, (4, 128), BF16)
                    PS_all = PS
                    PS_M = PSM([0, 1, 2, 3, 4])
                    PS_M1 = PSM([0, 1, 2])
                    PS_M2 = PSM([3, 4])
                    PS_K = PSM([5, 6, 7])

                    def mix(streams):
                        streams = [s_ for s_ in streams if s_]
                        pos = [0] * len(streams)
                        outl = []
                        for _ in range(sum(len(s_) for s_ in streams)):
                            best = None
                            for i, s_ in enumerate(streams):
                                if pos[i] < len(s_):
                                    fr = pos[i] / float(len(s_))
                                    if best is None or fr < best[0]:
                                        best = (fr, i)
                            outl.append(streams[best[1]][pos[best[1]]])
                            pos[best[1]] += 1
                        return outl

                    def emit_Y(Tp):
                        ymp, b_ymp = yms[Tp % 2]
                        bT = PS.get()
                        pT = pbank_bf(bT[0])[:, 0:512].rearrange("p (a b) -> p a b", a=4)
                        P.op("pe", f_tr([(pT[:, h, :], ymp[:, h, :]) for h in range(4)], identb),
                             reads=[b_ymp, b_identb], writes=[PB[bT[0]]])
                        P.op("dve", f_tt(ymT, pT, bc3(mog[:, l, :], [128, 4, 128]), ALU.mult),
                             reads=[PB[bT[0]], b_ggrp], writes=[b_ymT])
                        for hf in range(2):
                            P.op("pe", f_mm([(pbank(bT[0]), [(ymT[:, h, :], wout[:, h, hf * 512:(hf + 1) * 512]) for h in range(4)])]),
                                 reads=[b_ymT, b_wout], writes=[PB[bT[0]]])
                            P.op("dve", f_tt(X[:, Tp, hf * 512:(hf + 1) * 512], X[:, Tp, hf * 512:(hf + 1) * 512], pbank(bT[0]), ALU.add),
                                 reads=[XB[Tp], PB[bT[0]]], writes=[XB[Tp]])
                        PS.rel(bT)

                    if do_mlstm:
                        P.op("pool", f_memset(vaug[:, :, 128:129], 1.0), writes=[b_vaug])
                    if do_mla:
                        P.op("pool", f_memset(va[:, :, :, 128:129], 1.0), writes=[b_va])

                    for T in range(NT):
                        tc_ = slice(T * 128, (T + 1) * 128)
                        PS = PS_M
                        norm_to_hT(X[:, T, :], XB[T], gcol[:, l, 0, :], hT, b_hT, (xnb, b_xnb, stv, b_st))
                        bk = PS.get()
                        P.op("pe", f_mm([(psum[:, bk[0], 0:456], [(hT[:, k, :], Win[:, k, 1536:1992]) for k in range(8)])]),
                             reads=[b_hT, b_Win], writes=[PB[bk[0]]])
                        P.op("act", f_act(sm, psum[:, bk[0], 0:456], AF.Copy), reads=[PB[bk[0]]], writes=[b_sm])
                        PS.rel(bk)
                        strM1 = strM2 = strJ = []
                        if do_mlstm:
                            P.begin()
                            PS = PS_M1
                            bU = PS.get()
                            pU = pbank(bU[0]).rearrange("p (c t) -> p c t", c=4)
                            P.op("pe", f_mm([(pU[:, c, :], [(Win[:, k, c * 128:(c + 1) * 128], hT[:, k, :]) for k in range(8)])
                                             for c in range(4)]),
                                 reads=[b_hT, b_Win], writes=[PB[bU[0]]])
                            if T == 0:
                                P.op("pool", f_memset(ust[:, :, 0:3], 0.0), writes=[b_ust])
                            else:
                                P.op("dve", f_cp(ust[:, :, 0:3], ust[:, :, 128:131]), reads=[b_ust], writes=[b_ust])
                            P.op("act", f_act(ust[:, :, 3:131], pU, AF.Copy), reads=[PB[bU[0]]], writes=[b_ust])
                            for c in range(4):
                                P.op("dve", f_ts(TA[:, c, :], ust[:, c, 3:131], convw[:, l, c, 3:4], ALU.mult,
                                                 convb[:, l, c:c + 1], ALU.add),
                                     reads=[b_ust, b_ggrp], writes=[b_TA])
                                for j in range(3):
                                    P.op("dve", f_stt(TA[:, c, :], ust[:, c, j:j + 128], convw[:, l, c, j:j + 1], TA[:, c, :],
                                                      ALU.mult, ALU.add),
                                         reads=[b_ust, b_ggrp, b_TA], writes=[b_TA])
                            P.op("act", f_act(pU, TA, AF.Exp, scale=-1.0), reads=[b_TA], writes=[PB[bU[0]]])
                            P.op("act", f_act(pU, pU, AF.Ln, bias=1.0), reads=[PB[bU[0]]], writes=[PB[bU[0]]])
                            P.op("act", f_act(pU, pU, AF.Exp, scale=-1.0), reads=[PB[bU[0]]], writes=[PB[bU[0]]])
                            P.op("dve", f_tt(uc, TA, pU, ALU.mult), reads=[b_TA, PB[bU[0]]], writes=[b_uc])
                            PS.rel(bU)
                            bQ = PS.get()
                            bK = PS.get()
                            bKt = PS.get()
                            pQ = psum[0:64, bQ[0], :].rearrange("p (h t) -> p h t", h=4)
                            pK = psum[0:64, bK[0], :].rearrange("p (h t) -> p h t", h=4)
                            pKt = psum[:, bKt[0], 0:256].rearrange("p (h d) -> p h d", h=4)
                            P.op("pe", f_mm([(pQ[:, h, :], [(wqk[:, h, 0:64], uc[:, h, :])]) for h in range(4)]),
                                 reads=[b_wqk, b_uc], writes=[PB[bQ[0]]])
                            P.op("pe", f_mm([(pK[:, h, :], [(wqk[:, h, 64:128], uc[:, h, :])]) for h in range(4)]),
                                 reads=[b_wqk, b_uc], writes=[PB[bK[0]]])
                            P.op("pe", f_mm([(pKt[:, h, :], [(uc[:, h, :], wqk[:, h, 64:128])]) for h in range(4)]),
                                 reads=[b_wqk, b_uc], writes=[PB[bKt[0]]])
                            P.op("act", f_act(qTs, pQ, AF.Identity, scale=0.125), reads=[PB[bQ[0]]], writes=[b_qTs])
                            P.op("dve", f_cp(kTs, pK), reads=[PB[bK[0]]], writes=[b_kTs])
                            PS.rel(bK)
                            bS = PS.get()
                            pS2 = pbank(bS[0]).rearrange("p (h t) -> p h t", h=4)
                            P.op("pe", f_mm([(pS2[:, h, :], [(kTs[:, h, :], qTs[:, h, :])]) for h in range(4)]),
                                 reads=[b_kTs, b_qTs], writes=[PB[bS[0]]])
                            strM1 = P.end()
                            P.begin()
                            PS = PS_M2
                            gi = g8[:, 0:4]
                            lf = g8[:, 16:20]
                            P.op("dve", f_tt(g8[:, 0:8], sm[:, 0:8], bif[:, l, :], ALU.add), reads=[b_sm, b_ggrp], writes=[b_g8])
                            P.op("act", f_act(g8[:, 8:12], g8[:, 4:8], AF.Exp, scale=-1.0), reads=[b_g8], writes=[b_g8])
                            P.op("act", f_act(g8[:, 12:16], g8[:, 8:12], AF.Ln, bias=1.0), reads=[b_g8], writes=[b_g8])
                            P.op("dve", f_ts(lf, g8[:, 12:16], -1.0, ALU.mult), reads=[b_g8], writes=[b_g8])
                            P.op("dve", f_tt(TB, triu.unsqueeze(1).to_broadcast([128, 4, 128]), bc3(lf, [128, 4, 128]), ALU.mult),
                                 reads=[b_triu, b_g8], writes=[b_TB])
                            bB = PS.get()
                            pB = pbank(bB[0])
                            P.op("pe", f_mm([(pB, [(onesf, TB.rearrange("p a b -> p (a b)"))])]),
                                 reads=[b_onesf, b_TB], writes=[PB[bB[0]]])
                            bC = PS.get()
                            pC = pbank(bC[0])
                            P.op("pe", f_mm([(pC[:, 0:4], [(triu, lf)]), (pC[:, 4:8], [(trisl, lf)])]),
                                 reads=[b_triu, b_trisl, b_g8], writes=[PB[bC[0]]])
                            acol = g8[:, 20:24]
                            wa = g8[:, 28:32]
                            P.op("dve", f_tt(acol, gi, pC[:, 0:4], ALU.subtract), reads=[b_g8, PB[bC[0]]], writes=[b_g8])
                            P.op("dve", f_tt(g8[:, 24:28], gi, pC[:, 4:8], ALU.add), reads=[b_g8, PB[bC[0]]], writes=[b_g8])
                            PS.rel(bC)
                            P.op("act", f_act(wa, g8[:, 24:28], AF.Exp), reads=[b_g8], writes=[b_g8])
                            bk = PS.get()
                            P.op("pe", f_mm([(pbank(bk[0]), [(hT[:, k, :], Win[:, k, 512:1024]) for k in range(8)])]),
                                 reads=[b_hT, b_Win], writes=[PB[bk[0]]])
                            P.op("act", f_act(vaug[:, :, 0:128], pbank(bk[0]).rearrange("p (h e) -> p h e", h=4), AF.Copy),
                                 reads=[PB[bk[0]]], writes=[b_vaug])
                            PS.rel(bk)
                            bk = PS.get()
                            pO_ = pbank(bk[0])
                            P.op("pe", f_mm([(pO_, [(hT[:, k, :], Win[:, k, 1024:1536]) for k in range(8)])]),
                                 reads=[b_hT, b_Win], writes=[PB[bk[0]]])
                            P.op("act", f_act(pO_, pO_, AF.Exp, scale=-1.0), reads=[PB[bk[0]]], writes=[PB[bk[0]]])
                            P.op("act", f_act(pO_, pO_, AF.Ln, bias=1.0), reads=[PB[bk[0]]], writes=[PB[bk[0]]])
                            P.op("act", f_act(og, pO_, AF.Exp, scale=-1.0), reads=[PB[bk[0]]], writes=[b_og])
                            PS.rel(bk)
                            strM2 = P.end()
                            P.begin()
                            PS = PSM([0, 1, 2, 3, 4])
                            ymc, b_ymc = yms[T % 2]
                            P.op("dve", f_tt(ktil, pKt, bc3(wa, [128, 4, 64]), ALU.mult), reads=[PB[bKt[0]], b_g8], writes=[b_ktil])
                            pB3 = pB.rearrange("p (h t) -> p h t", h=4)
                            for h in range(4):
                                P.op("dve", f_stt(TA[:, h, :], pB3[:, h, :], acol[:, h:h + 1], mneg, ALU.add, ALU.add),
                                     reads=[PB[bB[0]], b_g8, b_mneg], writes=[b_TA])
                            P.op("act", f_act(TA, TA, AF.Exp), reads=[b_TA], writes=[b_TA])
                            P.op("dve", f_tt(PT, pS2, TA, ALU.mult), reads=[PB[bS[0]], b_TA], writes=[b_PT])
                            eB = TB[0:64]
                            P.op("act", f_act(eB, pB3[0:64], AF.Exp), reads=[PB[bB[0]]], writes=[b_TB])
                            P.op("dve", f_stt(qtil, pQ, 0.125, eB, ALU.mult, ALU.mult), reads=[PB[bQ[0]], b_TB], writes=[b_qtil])
                            bH = PS.get(2)

                            def Hh(h, bH=bH):
                                return psum[:, bH[0] + h // 2, (h % 2) * 129:(h % 2) * 129 + 129]
                            grp = []
                            for h in range(4):
                                pairs = []
                                if T > 0:
                                    pairs.append((qtil[:, h, :], Cbf[:, h, :]))
                                pairs.append((PT[:, h, :], vaug[:, h, :]))
                                grp.append((Hh(h), pairs))
                            P.op("pe", f_mm(grp), reads=[b_qtil, b_Cbf, b_PT, b_vaug], writes=[PB[bH[0]], PB[bH[1]]])
                            den = g8[:, 44:48]
                            den2 = den.rearrange("p (a b) -> p a b", b=2)
                            P.op("act", f_act(den2[:, :, 0], psum[:, bH[0]:bH[0] + 2, 128], AF.Abs), reads=[PB[bH[0]], PB[bH[1]]], writes=[b_g8])
                            P.op("act", f_act(den2[:, :, 1], psum[:, bH[0]:bH[0] + 2, 257], AF.Abs), reads=[PB[bH[0]], PB[bH[1]]], writes=[b_g8])
                            rr = g8[:, 48:52]
                            P.op("dve", f_ts(rr, den, 1.0, ALU.max), reads=[b_g8], writes=[b_g8])
                            P.op("dve", f_recip(rr, rr), reads=[b_g8], writes=[b_g8])
                            ssm = g8[:, 52:56]
                            for h in range(4):
                                P.op("act", f_act(junk, Hh(h)[:, 0:128], AF.Square, scale=rr[:, h:h + 1], accum=ssm[:, h:h + 1]),
                                     reads=[PB[bH[0]], PB[bH[1]], b_g8], writes=[b_junk, b_g8])
                            P.op("act", f_act(g8[:, 56:60], ssm, AF.Ln, scale=1.0 / 128.0, bias=EPS), reads=[b_g8], writes=[b_g8])
                            P.op("act", f_act(g8[:, 56:60], g8[:, 56:60], AF.Exp, scale=-0.5), reads=[b_g8], writes=[b_g8])
                            tot = g8[:, 60:64]
                            P.op("dve", f_tt(tot, rr, g8[:, 56:60], ALU.mult), reads=[b_g8], writes=[b_g8])
                            for h in range(4):
                                P.op("dve", f_stt(ymc[:, h, :], Hh(h)[:, 0:128], tot[:, h:h + 1], og[:, h * 128:(h + 1) * 128],
                                                  ALU.mult, ALU.mult),
                                     reads=[PB[bH[0]], PB[bH[1]], b_g8, b_og], writes=[b_ymc])
                            PS.rel(bH)
                            if T < NT - 1:
                                bL = PS.get(2)

                                def Cl(h, bL=bL):
                                    return psum[0:64, bL[0] + h // 2, (h % 2) * 129:(h % 2) * 129 + 129]
                                P.op("pe", f_mm([(Cl(h), [(ktil[:, h, :], vaug[:, h, :])]) for h in range(4)]),
                                     reads=[b_ktil, b_vaug], writes=[PB[bL[0]], PB[bL[1]]])
                                for h in range(4):
                                    if T == 0:
                                        P.op("dve", f_cp(Cst[:, h, :], Cl(h)), reads=[PB[bL[0]], PB[bL[1]]], writes=[b_Cst])
                                    else:
                                        P.op("dve", f_stt(Cst[:, h, :], Cst[:, h, :], eB[:, h, 127:128], Cl(h), ALU.mult, ALU.add),
                                             reads=[b_Cst, b_TB, PB[bL[0]], PB[bL[1]]], writes=[b_Cst])
                                PS.rel(bL)
                                P.op("act", f_act(Cbf, Cst, AF.Copy), reads=[b_Cst], writes=[b_Cbf])
                            strJ = P.end()
                            PS_M1.free = [0, 1, 2]
                            PS_M2.free = [3, 4]
                        strM = mix([strM1, strM2]) + strJ
                        PS = PS_K
                        P.begin()
                        if do_mlstm and T > 0:
                            emit_Y(T - 1)
                        if do_mla:
                            P.op("act", f_act(junkk, sm[:, 8:136], AF.Square, scale=256.0 ** -0.5, accum=stq[:, 0:1]),
                                 reads=[b_sm], writes=[b_junkk, b_stq])
                            P.op("act", f_act(junkk, sm[:, 136:264], AF.Square, scale=256.0 ** -0.5, accum=stq[:, 3:4]),
                                 reads=[b_sm], writes=[b_junkk, b_stq])
                            P.op("act", f_act(junkk, sm[:, 264:392], AF.Square, scale=128.0 ** -0.5, accum=stq[:, 1:2]),
                                 reads=[b_sm], writes=[b_junkk, b_stq])
                            P.op("act", f_act(junkk[:, 0:64], sm[:, 392:456], AF.Square, scale=64.0 ** -0.5, accum=stq[:, 2:3]),
                                 reads=[b_sm], writes=[b_junkk, b_stq])
                            P.op("dve", f_tt(stq[:, 0:1], stq[:, 0:1], stq[:, 3:4], ALU.add), reads=[b_stq], writes=[b_stq])
                            rstd_from_ms(stq, b_stq, 0, 4, n=3)
                            P.op("dve", f_ts(cqn, sm[:, 8:264], stq[:, 4:5], ALU.mult), reads=[b_sm, b_stq], writes=[b_cqn])
                            P.op("dve", f_ts(ckvn, sm[:, 264:392], stq[:, 5:6], ALU.mult), reads=[b_sm, b_stq], writes=[b_ckvn])
                            P.op("dve", f_stt(krn, sm[:, 392:456], stq[:, 6:7], gkr[:, l, :], ALU.mult, ALU.mult),
                                 reads=[b_sm, b_stq, b_ggrp], writes=[b_krn])
                            cT = cosb[:, T, :]
                            sT = sinb[:, T, :]
                            P.op("dve", f_tt(kt4[:, 0, :], krn[:, 0:32], cT, ALU.mult), reads=[b_krn, b_cos], writes=[b_kt4])
                            P.op("dve", f_tt(kt4[:, 1, :], krn[:, 32:64], sT, ALU.mult), reads=[b_krn, b_sin], writes=[b_kt4])
                            P.op("dve", f_tt(kt4[:, 2, :], krn[:, 32:64], cT, ALU.mult), reads=[b_krn, b_cos], writes=[b_kt4])
                            P.op("dve", f_tt(kt4[:, 3, :], krn[:, 0:32], sT, ALU.mult), reads=[b_krn, b_sin], writes=[b_kt4])
                            P.op("dve", f_tt(krot[:, 0, 0:32], kt4[:, 0, :], kt4[:, 1, :], ALU.subtract), reads=[b_kt4], writes=[b_krot])
                            P.op("dve", f_tt(krot[:, 0, 32:64], kt4[:, 2, :], kt4[:, 3, :], ALU.add), reads=[b_kt4], writes=[b_krot])
                            P.op("dve", f_cp(krot[:, 1, :], krot[:, 0, :]), reads=[b_krot], writes=[b_krot])
                            bT = PS.get()
                            pT = pbank_bf(bT[0]).rearrange("p (a b) -> p a b", a=8)
                            P.op("pe", f_tr([(pT[:, 0, :], cqn[:, 0:128]), (pT[:, 1, :], cqn[:, 128:256]),
                                             (pT[:, 2, :], ckvn), (pT[:, 3, :], krot.rearrange("p a b -> p (a b)"))], identb),
                                 reads=[b_cqn, b_ckvn, b_krot, b_identb], writes=[PB[bT[0]]])
                            P.op("dve", f_tt(cqnT, pT[:, 0:2, :], bc3(cqg[:, l, :], [128, 2, 128]), ALU.mult),
                                 reads=[PB[bT[0]], b_ggrp], writes=[b_cqnT])
                            P.op("dve", f_ts(ckvnT, pT[:, 2, :], misc[:, l, 0:1], ALU.mult), reads=[PB[bT[0]], b_ggrp], writes=[b_ckvnT])
                            P.op("act", f_act(krT[:, tc_], pT[:, 3, :], AF.Copy), reads=[PB[bT[0]]], writes=[b_krT])
                            PS.rel(bT)
                            bq = PS.get(2)
                            P.op("pe", f_mm([(psum[:, bq[0], :], [(cqnT[:, kc, :], wuq[:, kc, 0:512]) for kc in range(2)]),
                                             (psum[:, bq[1], 0:256], [(cqnT[:, kc, :], wuq[:, kc, 512:768]) for kc in range(2)])]),
                                 reads=[b_cqnT, b_wuq], writes=[PB[bq[0]], PB[bq[1]]])
                            qa2 = psum[:, bq[0]:bq[0] + 2, :].rearrange("p a b -> p (a b)")[:, 0:768]
                            qa = qa2.rearrange("p (h c) -> p h c", h=4)
                            rq = [PB[bq[0]], PB[bq[1]]]
                            for h in range(4):
                                P.op("act", f_act(junkk, qa[:, h, 0:128], AF.Square, scale=128.0 ** -0.5, accum=stq[:, 20 + h:21 + h]),
                                     reads=rq, writes=[b_junkk, b_stq])
                                P.op("act", f_act(junkk[:, 0:64], qa[:, h, 128:192], AF.Square, scale=64.0 ** -0.5, accum=stq[:, 24 + h:25 + h]),
                                     reads=rq, writes=[b_junkk, b_stq])
                            rstd_from_ms(stq, b_stq, 20, 8, n=8)
                            P.op("dve", f_tt(qn, qa[:, :, 0:128], bc3(stq[:, 8:12], [128, 4, 128]), ALU.mult), reads=rq + [b_stq], writes=[b_qn])
                            P.op("dve", f_tt(qr, qa[:, :, 128:192], bc3(stq[:, 12:16], [128, 4, 64]), ALU.mult), reads=rq + [b_stq], writes=[b_qr])
                            PS.rel(bq)
                            P.op("dve", f_tt(qr, qr, gqr[:, l, :].unsqueeze(1).to_broadcast([128, 4, 64]), ALU.mult),
                                 reads=[b_qr, b_ggrp], writes=[b_qr])
                            cT4 = cT.unsqueeze(1).to_broadcast([128, 4, 32])
                            sT4 = sT.unsqueeze(1).to_broadcast([128, 4, 32])
                            P.op("dve", f_tt(qt4[:, 0], qr[:, :, 0:32], cT4, ALU.mult), reads=[b_qr, b_cos], writes=[b_qt4])
                            P.op("dve", f_tt(qt4[:, 1], qr[:, :, 32:64], sT4, ALU.mult), reads=[b_qr, b_sin], writes=[b_qt4])
                            P.op("dve", f_tt(qrot[:, :, 0:32], qt4[:, 0], qt4[:, 1], ALU.subtract), reads=[b_qt4], writes=[b_qrot])
                            P.op("dve", f_tt(qt4[:, 0], qr[:, :, 32:64], cT4, ALU.mult), reads=[b_qr, b_cos], writes=[b_qt4])
                            P.op("dve", f_tt(qt4[:, 1], qr[:, :, 0:32], sT4, ALU.mult), reads=[b_qr, b_sin], writes=[b_qt4])
                            P.op("dve", f_tt(qrot[:, :, 32:64], qt4[:, 0], qt4[:, 1], ALU.add), reads=[b_qt4], writes=[b_qrot])
                            bT = PS.get()
                            pT = pbank_bf(bT[0]).rearrange("p (a b) -> p a b", a=8)
                            qrot2 = qrot.rearrange("p (i a) b -> p i (a b)", i=2)
                            P.op("pe", f_tr([(pT[:, h, :], qn[:, h, :]) for h in range(4)] +
                                            [(pT[:, 4 + i, :], qrot2[:, i, :]) for i in range(2)], identb),
                                 reads=[b_qn, b_qrot, b_identb], writes=[PB[bT[0]]])
                            P.op("dve", f_ts(qnT[:, :, tc_], pT[:, 0:4, :], misc[:, l, 1:2], ALU.mult, QSC, ALU.mult),
                                 reads=[PB[bT[0]], b_ggrp], writes=[b_qnT])
                            P.op("act", f_act(qrT[:, :, tc_], pT[:, 4:6, :], AF.Identity, scale=QSC), reads=[PB[bT[0]]], writes=[b_qrT])
                            PS.rel(bT)
                            bkv = PS.get(2)
                            P.op("pe", f_mm([(pbank(bkv[hf]), [(ckvnT, wukv[:, hf * 512:(hf + 1) * 512])]) for hf in range(2)]),
                                 reads=[b_ckvnT, b_wukv], writes=[PB[bkv[0]], PB[bkv[1]]])
                            kv = psum[:, bkv[0]:bkv[0] + 2, :].rearrange("p a (h c) -> p (a h) c", c=256)
                            rkv = [PB[bkv[0]], PB[bkv[1]]]
                            for h in range(4):
                                P.op("act", f_act(junkk, kv[:, h, 0:128], AF.Square, scale=128.0 ** -0.5, accum=stq[:, 28 + h:29 + h]),
                                     reads=rkv, writes=[b_junkk, b_stq])
                            rstd_from_ms(stq, b_stq, 28, 16, n=4)
                            P.op("dve", f_tt(kn, kv[:, :, 0:128], bc3(stq[:, 16:20], [128, 4, 128]), ALU.mult), reads=rkv + [b_stq], writes=[b_kn])
                            P.op("act", f_act(va[:, T, :, 0:128], kv[:, :, 128:256], AF.Copy), reads=rkv, writes=[b_va])
                            PS.rel(bkv)
                            bT = PS.get()
                            pT = pbank_bf(bT[0])[:, 0:512].rearrange("p (a b) -> p a b", a=4)
                            P.op("pe", f_tr([(pT[:, h, :], kn[:, h, :]) for h in range(4)], identb),
                                 reads=[b_kn, b_identb], writes=[PB[bT[0]]])
                            P.op("dve", f_ts(knT[:, :, tc_], pT, misc[:, l, 2:3], ALU.mult), reads=[PB[bT[0]], b_ggrp], writes=[b_knT])
                            PS.rel(bT)
                        strK = P.end()
                        P.merge([strM, strK])
                    PS = PS_K
                    if do_mlstm:
                        emit_Y(NT - 1)
                    PS = PS_all

                if do_mla:
                    load_w("pool", wout, b_wout, dr["w_out"][l, 512:1024, :].rearrange("(k p) n -> p k n", p=128))
                    SB.reset()
                    LOOKAHEAD = 2
                    ePs = [SB.get("eP%d" % i, (512,), BF16) for i in range(LOOKAHEAD + 2)]
                    ya, b_ya = SB.get("ya", (4, 128), BF16)
                    yaT, b_yaT = SB.get("yaT", (4, 512), BF16)
                    stb, b_stb = SB.get("stb", (16,), F32)
                    junk2, b_junk2 = SB.get("junk2", (128,), BF16)
                    epi = [0]
                    PS_all = PS
                    PS = PSM([4, 5])
                    BT_ = 7
                    BY_ = 6

                    def rec_N(qg, h, bO):
                        P.begin()
                        r_ = h % 2
                        ip = h // 2

                        def Oq(j):
                            return psum[:, bO[0] + j // 2, (j % 2) * 129:(j % 2) * 129 + 129]
                        nkb = 4 * qg + 4

                        def emit_S(kb):
                            j0 = max(0, kb - 4 * qg)
                            n0 = j0 * 128
                            kc_ = slice(kb * 128, (kb + 1) * 128)
                            bS = PS.get()
                            pS = pbank(bS[0])
                            P.op("pe", f_mm([(pS[:, n0:512], [(knT[:, h, kc_], qnT[:, h, qg * 512 + n0:(qg + 1) * 512]),
                                                               (krT[r_ * 64:(r_ + 1) * 64, kc_],
                                                                qrT[r_ * 64:(r_ + 1) * 64, ip, qg * 512 + n0:(qg + 1) * 512])])]),
                                 reads=[b_knT, b_qnT, b_krT, b_qrT], writes=[PB[bS[0]]])
                            eP, b_eP = ePs[epi[0] % len(ePs)]
                            epi[0] += 1
                            P.op("act", f_act(eP[:, n0:512], pS[:, n0:512], AF.Exp), reads=[PB[bS[0]]], writes=[b_eP])
                            PS.rel(bS)
                            if kb >= 4 * qg:
                                P.op("dve", f_tt(eP[:, n0:n0 + 128], eP[:, n0:n0 + 128], m01b, ALU.mult),
                                     reads=[b_eP, b_m01b], writes=[b_eP])
                            return (kb, j0, eP, b_eP)

                        def emit_PV(item):
                            kb, j0, eP, b_eP = item
                            grp = []
                            for j in range(j0, 4):
                                qb = 4 * qg + j
                                grp.append((Oq(j), eP[:, j * 128:(j + 1) * 128], va[:, kb, h, :],
                                            (kb == 0 and j % 2 == 0), kb == qb))

                            def pv(e, grp=grp):
                                ins = None
                                for (o_, l_, r2, st_, sp_) in grp:
                                    ins = e.matmul(o_, lhsT=l_, rhs=r2, start=st_, stop=sp_, skip_group_check=True)
                                return ins
                            P.op("pe", pv, reads=[b_eP, b_va], writes=[PB[bO[0]], PB[bO[1]]])
                        pend_s = []
                        for kb in range(nkb):
                            pend_s.append(emit_S(kb))
                            if len(pend_s) > LOOKAHEAD:
                                emit_PV(pend_s.pop(0))
                        while pend_s:
                            emit_PV(pend_s.pop(0))
                        return P.end()

                    def rec_E(qg, h, bO):
                        P.begin()

                        def Oq(j):
                            return psum[:, bO[0] + j // 2, (j % 2) * 129:(j % 2) * 129 + 129]
                        rO = [PB[bO[0]], PB[bO[1]]]
                        den = stb[:, 0:4]
                        den2 = den.rearrange("p (a b) -> p a b", b=2)
                        P.op("act", f_act(den2[:, :, 0], psum[:, bO[0]:bO[0] + 2, 128], AF.Copy), reads=rO, writes=[b_stb])
                        P.op("act", f_act(den2[:, :, 1], psum[:, bO[0]:bO[0] + 2, 257], AF.Copy), reads=rO, writes=[b_stb])
                        P.op("dve", f_recip(stb[:, 4:8], den), reads=[b_stb], writes=[b_stb])
                        for j in range(4):
                            P.op("act", f_act(junk2, Oq(j)[:, 0:128], AF.Square, scale=stb[:, 4 + j:5 + j], accum=stb[:, 8 + j:9 + j]),
                                 reads=rO + [b_stb], writes=[b_junk2, b_stb])
                        P.op("act", f_act(stb[:, 12:16], stb[:, 8:12], AF.Ln, scale=1.0 / 128.0, bias=EPS), reads=[b_stb], writes=[b_stb])
                        P.op("act", f_act(stb[:, 12:16], stb[:, 12:16], AF.Exp, scale=-0.5), reads=[b_stb], writes=[b_stb])
                        P.op("dve", f_tt(stb[:, 12:16], stb[:, 12:16], stb[:, 4:8], ALU.mult), reads=[b_stb], writes=[b_stb])
                        for j in range(4):
                            P.op("act", f_act(ya[:, j, :], Oq(j)[:, 0:128], AF.Identity, scale=stb[:, 12 + j:13 + j]),
                                 reads=rO + [b_stb], writes=[b_ya])
                        pT = pbank_bf(BT_)[:, 0:512]
                        P.op("pe", f_tr([(pT[:, j * 128:(j + 1) * 128], ya[:, j, :]) for j in range(4)], identb),
                             reads=[b_ya, b_identb], writes=[PB[BT_]])
                        P.op("dve", f_ts(yaT[:, h, :], pT, aog[:, l, h:h + 1], ALU.mult), reads=[PB[BT_], b_ggrp], writes=[b_yaT])
                        return P.end()

                    def rec_W(qg):
                        P.begin()
                        for j in range(4):
                            T = 4 * qg + j
                            for hf in range(2):
                                P.op("pe", f_mm([(pbank(BY_), [(yaT[:, h, j * 128:(j + 1) * 128], wout[:, h, hf * 512:(hf + 1) * 512])
                                                               for h in range(4)])]),
                                     reads=[b_yaT, b_wout], writes=[PB[BY_]])
                                P.op("dve", f_tt(X[:, T, hf * 512:(hf + 1) * 512], X[:, T, hf * 512:(hf + 1) * 512], pbank(BY_), ALU.add),
                                     reads=[XB[T], PB[BY_]], writes=[XB[T]])
                        return P.end()

                    prev = None
                    u = 0
                    for qg in range(4):
                        for h in range(4):
                            bO = [0, 1] if u % 2 == 0 else [2, 3]
                            u += 1
                            strN = rec_N(qg, h, bO)
                            P.merge([prev, strN] if prev else [strN])
                            prev = rec_E(qg, h, bO)
                            if h == 3:
                                prev = prev + rec_W(qg)
                    P.merge([prev])
                    PS = PS_all

                if do_xattn:
                    load_w("pool", wqx, b_wqx, dr["wq_x"][l].rearrange("(k p) n -> p k n", p=128))
                    load_w("pool", wkvx, b_wkvx, dr["wkv_x"][l].rearrange("(k p) n -> p k n", p=128))
                    load_w("pool", wox, b_wox, dr["wo_x"][l].rearrange("(k p) n -> p k n", p=128))
                    RB.reset()
                    memf, b_memf = RB.get("memf", (2, D), F32)
                    memT, b_memT = RB.get("memT", (2, 8, 128), BF16)
                    xkT, b_xkT = RB.get("xkT", (4, 256), BF16)
                    xv, b_xv = RB.get("xv", (2, 512), BF16)
                    hTg, b_hTg = RB.get("hTg", (4, 8, 128), BF16)
                    xqT, b_xqT = RB.get("xqT", (4, 512), BF16)
                    oT, b_oT = RB.get("oT", (4, 512), BF16)
                    lnS, b_lnS = RB.get("lnS", (512,), F32)
                    ePx = [RB.get("ePx%d" % i, (512,), BF16) for i in range(2)]
                    xkn, b_xkn = RB.get("xkn", (4, 128), BF16)
                    SB.reset()
                    xnb, b_xnb = SB.get("xnb", (D,), BF16)
                    stv, b_st = SB.get("st", (32,), F32)
                    junk3, b_junk3 = SB.get("junk3", (128,), BF16)
                    P.dma("sp", (lambda e, s, sq=sq: e.dma_start(out=memf, in_=dr["mem"][sq].rearrange("(m p) d -> p m d", p=128)).then_inc(s, 16)),
                          writes=[b_memf])
                    for m in range(2):
                        norm_to_hT(memf[:, m, :], b_memf, gcol[:, l, 2, :], memT[:, m], b_memT, (xnb, b_xnb, stv, b_st))
                    for m in range(2):
                        bkv = PS.get(2)
                        P.op("pe", f_mm([(pbank(bkv[hf]), [(memT[:, m, k, :], wkvx[:, k, hf * 512:(hf + 1) * 512]) for k in range(8)])
                                         for hf in range(2)]),
                             reads=[b_memT, b_wkvx], writes=[PB[bkv[0]], PB[bkv[1]]])
                        pK4 = pbank(bkv[0]).rearrange("p (h d) -> p h d", h=4)
                        for h in range(4):
                            P.op("act", f_act(junk3, pK4[:, h, :], AF.Square, scale=XSC, accum=stv[:, 8 + h:9 + h]),
                                 reads=[PB[bkv[0]]], writes=[b_junk3, b_st])
                        rstd_from_ms(stv, b_st, 8, 12, n=4)
                        P.op("dve", f_tt(xkn, pK4, bc3(stv[:, 12:16], [128, 4, 128]), ALU.mult), reads=[PB[bkv[0]], b_st], writes=[b_xkn])
                        P.op("act", f_act(xv[:, m, :], pbank(bkv[1]), AF.Copy), reads=[PB[bkv[1]]], writes=[b_xv])
                        PS.rel(bkv)
                        bT = PS.get()
                        pT = pbank_bf(bT[0])[:, 0:512].rearrange("p (a b) -> p a b", a=4)
                        P.op("pe", f_tr([(pT[:, h, :], xkn[:, h, :]) for h in range(4)], identb),
                             reads=[b_xkn, b_identb], writes=[PB[bT[0]]])
                        P.op("dve", f_ts(xkT[:, :, m * 128:(m + 1) * 128], pT, misc[:, l, 4:5], ALU.mult),
                             reads=[PB[bT[0]], b_ggrp], writes=[b_xkT])
                        PS.rel(bT)
                    for g in range(4):
                        for tt in range(4):
                            T = 4 * g + tt
                            norm_to_hT(X[:, T, :], XB[T], gcol[:, l, 1, :], hTg[:, tt], b_hTg, (xnb, b_xnb, stv, b_st))
                            bq = PS.get()
                            pQ4 = pbank(bq[0]).rearrange("p (h d) -> p h d", h=4)
                            P.op("pe", f_mm([(pbank(bq[0]), [(hTg[:, tt, k, :], wqx[:, k, :]) for k in range(8)])]),
                                 reads=[b_hTg, b_wqx], writes=[PB[bq[0]]])
                            for h in range(4):
                                P.op("act", f_act(junk3, pQ4[:, h, :], AF.Square, scale=XSC, accum=stv[:, 16 + h:17 + h]),
                                     reads=[PB[bq[0]]], writes=[b_junk3, b_st])
                            rstd_from_ms(stv, b_st, 16, 20, n=4)
                            P.op("dve", f_tt(xkn, pQ4, bc3(stv[:, 20:24], [128, 4, 128]), ALU.mult), reads=[PB[bq[0]], b_st], writes=[b_xkn])
                            PS.rel(bq)
                            bT = PS.get()
                            pT = pbank_bf(bT[0])[:, 0:512].rearrange("p (a b) -> p a b", a=4)
                            P.op("pe", f_tr([(pT[:, h, :], xkn[:, h, :]) for h in range(4)], identb),
                                 reads=[b_xkn, b_identb], writes=[PB[bT[0]]])
                            P.op("dve", f_ts(xqT[:, :, tt * 128:(tt + 1) * 128], pT, misc[:, l, 3:4], ALU.mult, XSC, ALU.mult),
                                 reads=[PB[bT[0]], b_ggrp], writes=[b_xqT])
                            PS.rel(bT)
                        for h in range(4):
                            for m in range(2):
                                bS = PS.get()
                                P.op("pe", f_mm([(pbank(bS[0]), [(xkT[:, h, m * 128:(m + 1) * 128], xqT[:, h, :])])]),
                                     reads=[b_xkT, b_xqT], writes=[PB[bS[0]]])
                                P.op("act", f_act(ePx[m][0], pbank(bS[0]), AF.Exp), reads=[PB[bS[0]]], writes=[ePx[m][1]])
                                PS.rel(bS)
                            bO = PS.get()
                            bSm = PS.get()
                            P.op("pe", f_mm([(pbank(bO[0]), [(xv[:, m, h * 128:(h + 1) * 128], ePx[m][0]) for m in range(2)]),
                                             (pbank(bSm[0]), [(onesb, ePx[m][0]) for m in range(2)])]),
                                 reads=[b_xv, b_onesb, ePx[0][1], ePx[1][1]], writes=[PB[bO[0]], PB[bSm[0]]])
                            P.op("act", f_act(lnS, pbank(bSm[0]), AF.Ln), reads=[PB[bSm[0]]], writes=[b_lnS])
                            PS.rel(bSm)
                            P.op("act", f_act(lnS, lnS, AF.Exp, scale=-1.0), reads=[b_lnS], writes=[b_lnS])
                            P.op("dve", f_tt(oT[:, h, :], pbank(bO[0]), lnS, ALU.mult), reads=[PB[bO[0]], b_lnS], writes=[b_oT])
                            PS.rel(bO)
                        for tt in range(4):
                            T = 4 * g + tt
                            bY = PS.get(2)
                            P.op("pe", f_mm([(pbank(bY[0] + hf), [(oT[:, h, tt * 128:(tt + 1) * 128], wox[:, h, hf * 512:(hf + 1) * 512])
                                                                  for h in range(4)]) for hf in range(2)]),
                                 reads=[b_oT, b_wox], writes=[PB[bY[0]], PB[bY[1]]])
                            resid_add(T, psum[:, bY[0]:bY[0] + 2, :].rearrange("p a b -> p (a b)"), bY)
                            PS.rel(bY)

                if do_ffn:
                    RB.reset()
                    hTall, b_hTall0 = RB.get("hTall", (NT, 8, 128), BF16)
                    b_hTt = []
                    for T in range(NT):
                        _, bb = A.view("hTall%d" % T, R1_LO + T * 2048, (1024,), BF16)
                        b_hTt.append(bb)
                    SB.reset()
                    xnb, b_xnb = SB.get("xnb", (D,), BF16)
                    stv, b_st = SB.get("st", (32,), F32)
                    h1s = [SB.get("h1T%d" % i, (FSL // 128, 512), BF16) for i in range(2)]
                    rls = [SB.get("rl%d" % i, (512,), BF16) for i in range(2)]

                    def load_slice(j):
                        w1s, b1, w2s, b2 = fslots[fslice_ctr[0] % 3]
                        fslice_ctr[0] += 1
                        load_w("pool", w1s, b1, dr["w_ff1"][l][:, j * FSL:(j + 1) * FSL].rearrange("(k p) n -> p k n", p=128))
                        load_w("pool", w2s, b2, dr["w_ff2"][l][j * FSL:(j + 1) * FSL, :].rearrange("(c p) n -> p c n", p=128))
                        return (w1s, b1, w2s, b2)
                    pend = [load_slice(0), load_slice(1)]
                    PS_Dall = PS
                    PS_FF = PSM([0, 1, 2, 3, 4, 5, 6])
                    PS_NM = PSM([7])

                    def rec_norms(g):
                        for T in range(4 * g, 4 * g + 4):
                            norm_to_hT(X[:, T, :], XB[T], gcol[:, l, 3, :], hTall[:, T], b_hTt[T], (xnb, b_xnb, stv, b_st))
                    PS = PS_NM
                    rec_norms(0)
                    it = 0
                    for j in range(NSL):
                        w1s, b1, w2s, b2 = pend.pop(0)
                        for g in range(4):
                            if j == 0:
                                strNM = []
                                if g < 3:
                                    PS = PS_NM
                                    P.begin()
                                    rec_norms(g + 1)
                                    strNM = P.end()
                                P.begin()
                            PS = PS_FF
                            h1T, b_h1T = h1s[it % 2]
                            it += 1
                            for fc in range(FSL // 128):
                                bH1 = PS.get()
                                pH1 = pbank(bH1[0])
                                P.op("pe", f_mm([(pH1.rearrange("p (a b) -> p a b", a=4),
                                                  [(w1s[:, k, fc * 128:(fc + 1) * 128], hTall[:, 4 * g:4 * g + 4, k, :]) for k in range(8)])]),
                                     reads=[b1] + b_hTt[4 * g:4 * g + 4], writes=[PB[bH1[0]]])
                                rl, b_rl = rls[fc % 2]
                                P.op("act", f_act(rl, pH1, AF.Relu), reads=[PB[bH1[0]]], writes=[b_rl])
                                P.op("dve", f_tt(h1T[:, fc, :], pH1, rl, ALU.mult), reads=[PB[bH1[0]], b_rl], writes=[b_h1T])
                                PS.rel(bH1)
                            for tt in range(4):
                                T = 4 * g + tt
                                bY = PS.get(2)
                                P.op("pe", f_mm([(pbank(bY[0] + hf), [(h1T[:, fc, tt * 128:(tt + 1) * 128], w2s[:, fc, hf * 512:(hf + 1) * 512])
                                                                      for fc in range(FSL // 128)]) for hf in range(2)]),
                                     reads=[b_h1T, b2], writes=[PB[bY[0]], PB[bY[1]]])
                                resid_add(T, psum[:, bY[0]:bY[0] + 2, :].rearrange("p a b -> p (a b)"), bY)
                                PS.rel(bY)
                                if j == NSL - 1 and l == depth - 1:
                                    P.dma("sp", (lambda e, s, T=T, sq=sq: e.dma_start(out=out_d[sq, T * 128:(T + 1) * 128, :], in_=X[:, T, :]).then_inc(s, 16)),
                                          reads=[XB[T]])
                            if j == 0:
                                strFF = P.end()
                                P.merge([strFF, strNM])
                        if j + 2 < NSL:
                            pend.append(load_slice(j + 2))
                    PS = PS_Dall
                elif l == depth - 1:
                    for T in range(NT):
                        P.dma("sp", (lambda e, s, T=T, sq=sq: e.dma_start(out=out_d[sq, T * 128:(T + 1) * 128, :], in_=X[:, T, :]).then_inc(s, 16)),
                              reads=[XB[T]])
        P.op("sp", None, reads=[], writes=XB)
        P.emit(st)
    return nc


def _consts():
    idx = np.arange(128)
    triu = (idx[:, None] <= idx[None, :]).astype(np.float32)
    trisl = (idx[:, None] > idx[None, :]).astype(np.float32)
    mneg = ((1.0 - triu) * -30000.0).astype(np.float32)
    inv = (1.0 / (10000.0 ** (np.arange(0, 64, 2, dtype=np.float32) / 64.0))).astype(np.float32)
    invf = np.broadcast_to((inv / np.float32(2.0 * np.pi)).astype(np.float32)[None, :], (128, 32)).copy()
    return {"c_ident": np.eye(128, dtype=np.float32), "c_triu": triu, "c_trisl": trisl, "c_mneg": mneg, "c_invf": invf}


_NC_CACHE = {}


def kernel(**inputs):
    x = np.ascontiguousarray(inputs["x"], dtype=np.float32)
    mem = np.ascontiguousarray(inputs["mem"], dtype=np.float32)
    pos = np.ascontiguousarray(inputs["positions"], dtype=np.int32)
    if "nc" not in _NC_CACHE:
        _NC_CACHE["nc"] = build_program()
    nc = _NC_CACHE["nc"]
    cst = _consts()
    in_maps = []
    for c in range(NCORES):
        sl = slice(c * SEQ_PER_CORE, (c + 1) * SEQ_PER_CORE)
        m = {"x": x[sl], "mem": mem[sl], "pos": pos[sl]}
        for n in WNAMES:
            m[n] = np.ascontiguousarray(inputs[n], dtype=np.float32)
        m.update(cst)
        in_maps.append(m)
    res = run_bass_kernel_spmd(nc, in_maps, core_ids=list(range(NCORES)))
    out = np.concatenate([np.asarray(r["out"]) for r in res.results], axis=0)
    return out.astype(np.float32)
```

```python
import numpy as np
from contextlib import ExitStack
import concourse.bass as bass
import concourse.mybir as mybir
from concourse.bass_utils import run_bass_kernel_spmd

F32 = mybir.dt.float32
BF16 = mybir.dt.bfloat16
I32 = mybir.dt.int32
AF = mybir.ActivationFunctionType
ALU = mybir.AluOpType
AX = mybir.AxisListType

ENGS = ("pe", "act", "dve", "pool", "sp")

NCORES = 8
SEQ_PER_CORE = 2
S = 2048
D = 1024
NT = S // 128
DEPTH = 2
NMEM = 256
EPS = 1e-6
IN_COLS = 1992
DFF = 4096
FSL = 512
NSL = DFF // FSL


class Buf:
    __slots__ = ("name", "lw", "rd", "alias", "lo", "hi", "excl")

    def __init__(self, name, lo=None, hi=None, excl=False):
        self.name = name
        self.excl = excl
        self.lw = None
        self.rd = {}
        self.alias = [self]
        self.lo = lo
        self.hi = hi


class Op:
    __slots__ = ("idx", "eng", "fn", "deps", "is_dma", "k", "waits", "signal",
                 "semval", "snap", "sem", "dval")


class Prog:
    def __init__(self, nc):
        self.nc = nc
        self.ops = []
        self.dma_sems = {}
        self.sem_count = {}
        self.sem_last = {}
        self.defer = None

    def _add(self, eng, fn, reads, writes, is_dma, k):
        o = Op()
        o.idx = len(self.ops)
        o.eng = eng
        o.fn = fn
        o.is_dma = is_dma
        o.k = k
        o.waits = []
        o.signal = False
        o.semval = None
        o.snap = None
        o.sem = None
        o.dval = None
        deps = set()
        for b in reads:
            for a in b.alias:
                if a.lw is not None:
                    deps.add(a.lw)
            if b.excl:
                for ke, vi in b.rd.items():
                    if ke != eng:
                        deps.add(vi)
        for b in writes:
            for a in b.alias:
                if a.lw is not None:
                    deps.add(a.lw)
                deps.update(a.rd.values())
        for b in reads:
            if is_dma:
                b.rd[("d", o.idx)] = o.idx
            else:
                b.rd[eng] = o.idx
        for b in writes:
            b.lw = o.idx
            b.rd = {}
        if is_dma:
            key = (list(writes) + list(reads))[0]
            skey = id(key)
            if skey not in self.dma_sems:
                self.dma_sems[skey] = "dsem%d" % len(self.dma_sems)
            o.sem = skey
            prev = self.sem_last.get(skey)
            if prev is not None:
                deps.add(prev)
            self.sem_last[skey] = o.idx
            self.sem_count[skey] = self.sem_count.get(skey, 0) + 16 * k
            o.dval = self.sem_count[skey]
        deps.discard(o.idx)
        o.deps = deps
        self.ops.append(o)
        return o

    def op(self, eng, fn, reads=(), writes=()):
        if self.defer is not None:
            self.defer.append((eng, fn, list(reads), list(writes), False, 0))
            return None
        return self._add(eng, fn, reads, writes, False, 0)

    def dma(self, eng, fn, reads=(), writes=(), k=1):
        if self.defer is not None:
            self.defer.append((eng, fn, list(reads), list(writes), True, k))
            return None
        return self._add(eng, fn, reads, writes, True, k)

    def begin(self):
        assert self.defer is None
        self.defer = []

    def end(self):
        lst = self.defer
        self.defer = None
        return lst

    def merge(self, streams):
        streams = [s_ for s_ in streams if s_]
        pos = [0] * len(streams)
        total = sum(len(s_) for s_ in streams)
        for _ in range(total):
            best = None
            for i, s_ in enumerate(streams):
                if pos[i] < len(s_):
                    frac = pos[i] / float(len(s_))
                    if best is None or frac < best[0]:
                        best = (frac, i)
            i = best[1]
            self._add(*streams[i][pos[i]])
            pos[i] += 1

    def schedule(self):
        ops = self.ops
        cur = {e: {} for e in ENGS}
        for o in ops:
            E = o.eng
            clk = cur[E]
            need = {}
            dwaits = []
            for d in o.deps:
                Dp = ops[d]
                if Dp.is_dma:
                    if clk.get(("s", Dp.sem), 0) >= Dp.dval:
                        continue
                    dwaits.append(Dp)
                else:
                    if Dp.eng == "pe" and E == "pe" and not o.is_dma:
                        continue
                    if clk.get(Dp.eng, -1) >= d:
                        continue
                    if need.get(Dp.eng, -1) < d:
                        need[Dp.eng] = d
            if need or dwaits:
                new = dict(clk)
                targets = [ops[d] for d in need.values()] + dwaits
                for Dp in targets:
                    for kk, vv in Dp.snap.items():
                        if new.get(kk, -1) < vv:
                            new[kk] = vv
                    if Dp.is_dma:
                        kk = ("s", Dp.sem)
                        if new.get(kk, 0) < Dp.dval:
                            new[kk] = Dp.dval
                    else:
                        if new.get(Dp.eng, -1) < Dp.idx:
                            new[Dp.eng] = Dp.idx
                        Dp.signal = True
                    o.waits.append(Dp)
                cur[E] = new
            o.snap = cur[E]
        cnt = {e: 0 for e in ENGS}
        for o in ops:
            if not o.is_dma and o.signal:
                cnt[o.eng] += 1
                o.semval = cnt[o.eng]

    def emit(self, stack):
        nc = self.nc
        self.schedule()
        esem = {e: stack.enter_context(nc.semaphore("es_" + e)) for e in ENGS}
        dsem = {k: stack.enter_context(nc.semaphore(n)) for k, n in self.dma_sems.items()}
        block = stack.enter_context(nc.Block())
        by_eng = {e: [] for e in ENGS}
        for o in self.ops:
            by_eng[o.eng].append(o)

        def run(e, name):
            for o in by_eng[name]:
                for Dp in o.waits:
                    if Dp.is_dma:
                        e.wait_ge(dsem[Dp.sem], Dp.dval)
                    else:
                        e.wait_ge(esem[Dp.eng], Dp.semval)
                if o.fn is None:
                    continue
                if o.is_dma:
                    o.fn(e, dsem[o.sem])
                else:
                    ins = o.fn(e)
                    if o.signal:
                        ins.then_inc(esem[name], 1)

        @block.tensor
        def _(e):
            run(e, "pe")

        @block.scalar
        def _(e):
            run(e, "act")

        @block.vector
        def _(e):
            run(e, "dve")

        @block.gpsimd
        def _(e):
            run(e, "pool")

        @block.sync
        def _(e):
            run(e, "sp")


class Arena:
    def __init__(self, nc, st, nbytes):
        self.nbytes = nbytes
        self.t = st.enter_context(nc.sbuf_tensor("arena", [128, nbytes // 4], F32))
        self.bufs = []

    def view(self, name, off, shape, dt, parts=128):
        n = 1
        for s_ in shape:
            n *= s_
        esz = 4 if dt in (F32, I32) else 2
        nb = n * esz
        assert off % 4 == 0 and nb % 4 == 0, (name, off, nb)
        assert off + nb <= self.nbytes, (name, off, nb, self.nbytes)
        ap = self.t[0:parts, off // 4:(off + nb) // 4]
        if dt != F32:
            ap = ap.bitcast(dt)
        if len(shape) == 2:
            ap = ap.rearrange("p (a b) -> p a b", a=shape[0])
        elif len(shape) == 3:
            ap = ap.rearrange("p (a b c) -> p a b c", a=shape[0], b=shape[1])
        b = Buf(name, off, off + nb)
        for o in self.bufs:
            if o.lo < b.hi and b.lo < o.hi:
                o.alias.append(b)
                b.alias.append(o)
        self.bufs.append(b)
        return ap, b


class Bump:
    def __init__(self, arena, lo, hi):
        self.a = arena
        self.lo = lo
        self.hi = hi
        self.p = lo

    def reset(self):
        self.p = self.lo

    def at(self, name, buf, shape, dt, parts=128, off=0):
        return self.a.view(name, buf.lo + off, shape, dt, parts)

    def get(self, name, shape, dt, parts=128):
        n = 1
        for s_ in shape:
            n *= s_
        nb = n * (4 if dt in (F32, I32) else 2)
        nb = (nb + 31) // 32 * 32
        off = self.p
        assert off + nb <= self.hi, ("scratch overflow", name, off, nb, self.hi)
        self.p += nb
        return self.a.view(name, off, shape, dt, parts)


def f_act(out, in_, func, scale=None, bias=None, accum=None):
    kw = {}
    if scale is not None:
        kw["scale"] = scale
    if bias is not None:
        kw["bias"] = bias
    if accum is not None:
        kw["accum_out"] = accum
    return lambda e: e.activation(out=out, in_=in_, func=func, **kw)


def f_ts(out, in0, s1, op0, s2=None, op1=None):
    if op1 is None:
        return lambda e: e.tensor_scalar(out=out, in0=in0, scalar1=s1, scalar2=None, op0=op0)
    return lambda e: e.tensor_scalar(out=out, in0=in0, scalar1=s1, scalar2=s2, op0=op0, op1=op1)


def f_tt(out, in0, in1, op):
    return lambda e: e.tensor_tensor(out=out, in0=in0, in1=in1, op=op)


def f_stt(out, in0, scalar, in1, op0, op1):
    return lambda e: e.scalar_tensor_tensor(out=out, in0=in0, scalar=scalar, in1=in1, op0=op0, op1=op1)


def f_cp(out, in_):
    return lambda e: e.tensor_copy(out=out, in_=in_)


def f_red(out, in_, op=None):
    return lambda e: e.tensor_reduce(out=out, in_=in_, axis=AX.X, op=(op or ALU.add))


def f_recip(out, in_):
    return lambda e: e.reciprocal(out=out, in_=in_)


def f_memset(ap, val):
    return lambda e: e.memset(ap, val)


def f_mm(groups):
    def fn(e):
        ins = None
        for out, pairs in groups:
            n = len(pairs)
            for i, (l, r) in enumerate(pairs):
                ins = e.matmul(out, lhsT=l, rhs=r, start=(i == 0), stop=(i == n - 1))
        return ins
    return fn


def f_tr(items, ident):
    def fn(e):
        ins = None
        for out, in_ in items:
            ins = e.transpose(out=out, in_=in_, identity=ident)
        return ins
    return fn


WNAMES = ["norm_mix_g", "w_in", "conv_w", "conv_b", "wq_m", "wk_m", "b_igate", "b_fgate",
          "m_out_g", "cq_norm_g", "ckv_norm_g", "w_uq", "w_ukv", "qk_norm_q", "qk_norm_k",
          "a_out_g", "w_out", "norm_x_g", "norm_mem_g", "wq_x", "wkv_x", "xq_norm_g",
          "xk_norm_g", "wo_x", "norm_ffn_g", "w_ff1", "w_ff2"]
WSHAPES = {
    "norm_mix_g": [2, 1024], "w_in": [2, 1024, 1992], "conv_w": [2, 4, 512], "conv_b": [2, 512],
    "wq_m": [2, 4, 128, 64], "wk_m": [2, 4, 128, 64], "b_igate": [2, 4], "b_fgate": [2, 4],
    "m_out_g": [2, 4, 128], "cq_norm_g": [2, 256], "ckv_norm_g": [2, 128], "w_uq": [2, 256, 768],
    "w_ukv": [2, 128, 1024], "qk_norm_q": [2, 192], "qk_norm_k": [2, 192], "a_out_g": [2, 4, 128],
    "w_out": [2, 1024, 1024], "norm_x_g": [2, 1024], "norm_mem_g": [2, 1024], "wq_x": [2, 1024, 512],
    "wkv_x": [2, 1024, 1024], "xq_norm_g": [2, 128], "xk_norm_g": [2, 128], "wo_x": [2, 512, 1024],
    "norm_ffn_g": [2, 1024], "w_ff1": [2, 1024, 4096], "w_ff2": [2, 4096, 1024],
}


def build_program(nseq=SEQ_PER_CORE, depth=DEPTH, do_mlstm=True, do_mla=True, do_xattn=True,
                  do_ffn=True):
    nc = bass.Bass("TRN2", target_bir_lowering=False)
    dr = {}
    dr["x"] = nc.dram_tensor("x", [nseq, S, D], F32, kind="ExternalInput").ap()
    dr["mem"] = nc.dram_tensor("mem", [nseq, NMEM, D], F32, kind="ExternalInput").ap()
    dr["pos"] = nc.dram_tensor("pos", [nseq, S], I32, kind="ExternalInput").ap()
    for n in WNAMES:
        dr[n] = nc.dram_tensor(n, WSHAPES[n], F32, kind="ExternalInput").ap()
    dr["c_ident"] = nc.dram_tensor("c_ident", [128, 128], F32, kind="ExternalInput").ap()
    dr["c_triu"] = nc.dram_tensor("c_triu", [128, 128], F32, kind="ExternalInput").ap()
    dr["c_trisl"] = nc.dram_tensor("c_trisl", [128, 128], F32, kind="ExternalInput").ap()
    dr["c_mneg"] = nc.dram_tensor("c_mneg", [128, 128], F32, kind="ExternalInput").ap()
    dr["c_invf"] = nc.dram_tensor("c_invf", [128, 32], F32, kind="ExternalInput").ap()
    out_d = nc.dram_tensor("out", [nseq, S, D], F32, kind="ExternalOutput").ap()

    st = ExitStack()
    with st:
        P = Prog(nc)
        TOTAL = 212800
        A = Arena(nc, st, TOTAL)
        psum = st.enter_context(nc.psum_tensor("ps", [128, 8, 512], F32))
        PB = [Buf("psb%d" % i, excl=True) for i in range(8)]

        off = 0
        X, _ = A.view("X", off, (NT, D), F32)
        XB = []
        for t in range(NT):
            _, b = A.view("X%d" % t, off + t * D * 4, (D,), F32)
            XB.append(b)
        off += NT * D * 4
        R1_LO = off
        R1_SZ = 61568
        off += R1_SZ
        W_LO = off
        W_SZ = 49152
        off += W_SZ
        C_LO = off
        C_SZ = 9216
        off += C_SZ
        S_LO = off
        S_HI = TOTAL
        CB = Bump(A, C_LO, C_LO + C_SZ)
        SB = Bump(A, S_LO, S_HI)
        RB = Bump(A, R1_LO, R1_LO + R1_SZ)

        identf, b_identf = CB.get("identf", (128,), F32)
        identb, b_identb = CB.get("identb", (128,), BF16)
        triu, b_triu = CB.get("triu", (128,), F32)
        trisl, b_trisl = CB.get("trisl", (128,), F32)
        mneg, b_mneg = CB.get("mneg", (128,), F32)
        m01b, b_m01b = CB.get("m01b", (128,), BF16)
        onesb, b_onesb = CB.get("onesb", (128,), BF16)
        onesf, b_onesf = CB.get("onesf", (128,), F32)
        invf, b_invf = CB.get("invf", (32,), F32)
        gcol, b_gcol = CB.get("gcol", (DEPTH, 4, 8), F32)
        convw, b_convw = CB.get("convw", (DEPTH, 4, 4), F32)
        convb, b_convb = CB.get("convb", (DEPTH, 4), F32)
        mog, b_mog = CB.get("mog", (DEPTH, 4), F32)
        aog, b_aog = CB.get("aog", (DEPTH, 4), F32)
        cqg, b_cqg = CB.get("cqg", (DEPTH, 2), F32)
        misc, b_misc = CB.get("misc", (DEPTH, 8), F32)
        gqr, b_gqr = CB.get("gqr", (DEPTH, 64), F32)
        gkr, b_gkr = CB.get("gkr", (DEPTH, 64), F32)
        bif, b_bif = CB.get("bif", (DEPTH, 8), F32)
        cosb, b_cos = CB.get("cos", (NT, 32), F32)
        sinb, b_sin = CB.get("sin", (NT, 32), F32)
        b_const = [b_identf, b_identb, b_triu, b_trisl, b_mneg, b_m01b, b_onesb, b_onesf, b_invf]

        def bc_rows(src_ap_1d, n):
            return src_ap_1d.unsqueeze(0).to_broadcast([128, n])

        def ld_consts(e, s):
            e.dma_start(out=identf, in_=dr["c_ident"]).then_inc(s, 16)
            e.dma_start(out=triu, in_=dr["c_triu"]).then_inc(s, 16)
            e.dma_start(out=trisl, in_=dr["c_trisl"]).then_inc(s, 16)
            e.dma_start(out=mneg, in_=dr["c_mneg"]).then_inc(s, 16)
            e.dma_start(out=invf, in_=dr["c_invf"]).then_inc(s, 16)
        b_cgrp = Buf("cgrp")
        P.dma("sp", ld_consts, writes=[b_cgrp, b_identf, b_triu, b_trisl, b_mneg, b_invf], k=5)
        P.op("dve", f_cp(identb, identf), reads=[b_identf], writes=[b_identb])
        P.op("dve", f_cp(m01b, triu), reads=[b_triu], writes=[b_m01b])
        P.op("pool", f_memset(onesb, 1.0), writes=[b_onesb])
        P.op("pool", f_memset(onesf, 1.0), writes=[b_onesf])

        b_gains = [b_gcol, b_convw, b_convb, b_mog, b_aog, b_cqg, b_misc, b_gqr, b_gkr, b_bif]

        gl = []
        for l in range(DEPTH):
            for wi, nm in enumerate(["norm_mix_g", "norm_x_g", "norm_mem_g", "norm_ffn_g"]):
                gl.append((gcol[:, l, wi, :], dr[nm][l].rearrange("(k p) -> p k", p=128)))
            for j in range(4):
                gl.append((convw[:, l, :, j], dr["conv_w"][l, j].rearrange("(c p) -> p c", p=128)))
            gl.append((convb[:, l, :], dr["conv_b"][l].rearrange("(c p) -> p c", p=128)))
            gl.append((mog[:, l, :], dr["m_out_g"][l].rearrange("h p -> p h")))
            gl.append((aog[:, l, :], dr["a_out_g"][l].rearrange("h p -> p h")))
            gl.append((cqg[:, l, :], dr["cq_norm_g"][l].rearrange("(k p) -> p k", p=128)))
            gl.append((misc[:, l, 0:1], dr["ckv_norm_g"][l].rearrange("(k p) -> p k", p=128)))
            gl.append((misc[:, l, 1:2], dr["qk_norm_q"][l, 0:128].rearrange("(k p) -> p k", p=128)))
            gl.append((misc[:, l, 2:3], dr["qk_norm_k"][l, 0:128].rearrange("(k p) -> p k", p=128)))
            gl.append((misc[:, l, 3:4], dr["xq_norm_g"][l].rearrange("(k p) -> p k", p=128)))
            gl.append((misc[:, l, 4:5], dr["xk_norm_g"][l].rearrange("(k p) -> p k", p=128)))
            gl.append((gqr[:, l, :], bc_rows(dr["qk_norm_q"][l, 128:192], 64)))
            gl.append((gkr[:, l, :], bc_rows(dr["qk_norm_k"][l, 128:192], 64)))
            gl.append((bif[:, l, 0:4], bc_rows(dr["b_igate"][l], 4)))
            gl.append((bif[:, l, 4:8], bc_rows(dr["b_fgate"][l], 4)))

        def ld_gains(e, s):
            with nc.allow_non_contiguous_dma(reason="tiny per-layer gain vectors"):
                for (o_, i_) in gl:
                    e.dma_start(out=o_, in_=i_).then_inc(s, 16)
        b_ggrp = Buf("ggrp")
        P.dma("sp", ld_gains, writes=[b_ggrp] + b_gains, k=len(gl))

        class PSM:
            def __init__(self, banks=None):
                self.free = list(range(8)) if banks is None else list(banks)

            def get(self, n=1):
                if n == 1:
                    for b in self.free:
                        if (b ^ 1) not in self.free:
                            self.free.remove(b)
                            return [b]
                    b = self.free.pop(0)
                    return [b]
                for i, b in enumerate(self.free):
                    if b % 2 == 0 and (b + 1) in self.free:
                        self.free.remove(b)
                        self.free.remove(b + 1)
                        return [b, b + 1]
                raise RuntimeError("no psum pair free: %s" % self.free)

            def rel(self, banks):
                self.free.extend(banks)
        PS = PSM()

        def pbank(b):
            return psum[:, b, :]

        def pbank_bf(b):
            return psum[:, b, :].bitcast(BF16)

        Win, b_Win = A.view("Win", W_LO, (8, IN_COLS), BF16)
        wuq, b_wuq = A.view("wuq", W_LO + 31872, (2, 768), BF16)
        wukv, b_wukv = A.view("wukv", W_LO + 31872 + 3072, (1024,), BF16)
        wqk, b_wqk = A.view("wqk", W_LO + 31872 + 5120, (4, 128), BF16)
        wout, b_wout = A.view("wout", W_LO + 38016, (4, 1024), BF16)
        wqx, b_wqx = A.view("wqx", W_LO, (8, 512), BF16)
        wkvx, b_wkvx = A.view("wkvx", W_LO + 8192, (8, 1024), BF16)
        wox, b_wox = A.view("wox", W_LO + 24576, (4, 1024), BF16)
        fslots = []
        for i, o_ in enumerate([32768, 0, 16384]):
            w1s, b1 = A.view("w1s%d" % i, W_LO + o_, (8, FSL), BF16)
            w2s, b2 = A.view("w2s%d" % i, W_LO + o_ + 8192, (FSL // 128, 1024), BF16)
            fslots.append((w1s, b1, w2s, b2))

        def rstd_from_ms(stv, b_st, src_col, dst_col, n=1):
            P.op("act", f_act(stv[:, dst_col:dst_col + n], stv[:, src_col:src_col + n], AF.Ln, bias=EPS),
                 reads=[b_st], writes=[b_st])
            P.op("act", f_act(stv[:, dst_col:dst_col + n], stv[:, dst_col:dst_col + n], AF.Exp, scale=-0.5),
                 reads=[b_st], writes=[b_st])

        def norm_to_hT(xin_ap, b_xin, gcols, hT_ap, b_hT, scr):
            xnb, b_xnb, stv, b_st = scr
            P.op("act", f_act(xnb, xin_ap, AF.Square, scale=float(D) ** -0.5, accum=stv[:, 0:1]),
                 reads=[b_xin], writes=[b_xnb, b_st])
            rstd_from_ms(stv, b_st, 0, 1)
            P.op("dve", f_ts(xnb, xin_ap, stv[:, 1:2], ALU.mult), reads=[b_xin, b_st], writes=[b_xnb])
            bk = PS.get()
            pT = pbank_bf(bk[0]).rearrange("p (a b) -> p a b", a=8)
            P.op("pe", f_tr([(pT[:, k, :], xnb[:, k * 128:(k + 1) * 128]) for k in range(8)], identb),
                 reads=[b_xnb, b_identb], writes=[PB[bk[0]]])
            P.op("dve", f_tt(hT_ap, pT, gcols.unsqueeze(2).to_broadcast([128, 8, 128]), ALU.mult),
                 reads=[PB[bk[0]], b_ggrp], writes=[b_hT])
            PS.rel(bk)

        def load_w(eng, dst, b_dst, src, k=1):
            P.dma(eng, lambda e, s: e.dma_start(out=dst, in_=src).then_inc(s, 16), writes=[b_dst])

        def resid_add(T, pY2, banks):
            P.op("dve", f_tt(X[:, T, :], X[:, T, :], pY2, ALU.add),
                 reads=[XB[T], PB[banks[0]], PB[banks[1]]], writes=[XB[T]])

        QSC = 192.0 ** -0.5
        XSC = 128.0 ** -0.5
        fslice_ctr = [0]

        def bc3(ap2, shape):
            return ap2.unsqueeze(2).to_broadcast(shape)

        for sq in range(nseq):
            for T in range(NT):
                P.dma("sp", (lambda e, s, T=T, sq=sq: e.dma_start(out=X[:, T, :], in_=dr["x"][sq, T * 128:(T + 1) * 128, :]).then_inc(s, 16)),
                      writes=[XB[T]])
            if do_mla:
                SB.reset()
                posi, b_posi = SB.get("posi", (NT,), I32)
                posf, b_posf = SB.get("posf", (NT,), F32)
                ang, b_ang = SB.get("ang", (NT, 32), F32)
                angi, b_angi = SB.get("angi", (NT, 32), I32)
                angf, b_angf = SB.get("angf", (NT, 32), F32)
                tmpc, b_tmpc = SB.get("tmpc", (NT, 32), F32)

                def ld_pos(e, s, sq=sq):
                    with nc.allow_non_contiguous_dma(reason="positions to token-major columns"):
                        e.dma_start(out=posi, in_=dr["pos"][sq].rearrange("(t p) -> p t", p=128)).then_inc(s, 16)
                P.dma("sp", ld_pos, writes=[b_posi])
                P.op("dve", f_cp(posf, posi), reads=[b_posi], writes=[b_posf])
                P.op("dve", f_tt(ang, posf.unsqueeze(2).to_broadcast([128, NT, 32]),
                                 invf.unsqueeze(1).to_broadcast([128, NT, 32]), ALU.mult),
                     reads=[b_posf, b_invf, b_cgrp], writes=[b_ang])
                for (dst, b_dst, shift) in [(sinb, b_sin, 0.0), (cosb, b_cos, 0.25)]:
                    if shift != 0.0:
                        P.op("dve", f_ts(tmpc, ang, shift, ALU.add), reads=[b_ang], writes=[b_tmpc])
                        src, b_src = tmpc, b_tmpc
                    else:
                        src, b_src = ang, b_ang
                    P.op("dve", f_cp(angi, src), reads=[b_src], writes=[b_angi])
                    P.op("dve", f_cp(angf, angi), reads=[b_angi], writes=[b_angf])
                    P.op("dve", f_tt(angf, src, angf, ALU.subtract), reads=[b_src, b_angf], writes=[b_angf])
                    P.op("dve", f_ts(angi.bitcast(F32), angf, 0.5, ALU.is_gt), reads=[b_angf], writes=[b_angi])
                    P.op("dve", f_tt(angf, angf, angi.bitcast(F32), ALU.subtract), reads=[b_angf, b_angi], writes=[b_angf])
                    P.op("dve", f_ts(angi.bitcast(F32), angf, -0.5, ALU.is_lt), reads=[b_angf], writes=[b_angi])
                    P.op("dve", f_tt(angf, angf, angi.bitcast(F32), ALU.add), reads=[b_angf, b_angi], writes=[b_angf])
                    P.op("act", f_act(dst, angf, AF.Sin, scale=6.28318), reads=[b_angf], writes=[b_dst])

            for l in range(depth):
                if do_mlstm or do_mla:
                    load_w("pool", Win, b_Win, dr["w_in"][l].rearrange("(k p) n -> p k n", p=128))
                    load_w("pool", wuq, b_wuq, dr["w_uq"][l].rearrange("(k p) n -> p k n", p=128))
                    load_w("pool", wukv, b_wukv, dr["w_ukv"][l])
                    load_w("pool", wqk[:, :, 0:64], b_wqk, dr["wq_m"][l].rearrange("h d e -> d h e"))
                    load_w("pool", wqk[:, :, 64:128], b_wqk, dr["wk_m"][l].rearrange("h d e -> d h e"))
                    load_w("pool", wout, b_wout, dr["w_out"][l, 0:512, :].rearrange("(k p) n -> p k n", p=128))

                    RB.reset()
                    qnT, b_qnT = RB.get("qnT", (4, S), BF16)
                    qrT, b_qrT = RB.get("qrT", (2, S), BF16)
                    knT, b_knT = RB.get("knT", (4, S), BF16)
                    krT, b_krT = RB.get("krT", (S,), BF16)
                    va, b_va = RB.get("va", (NT, 4, 129), BF16)
                    SB.reset()
                    xnb, b_xnb = SB.get("xnb", (D,), BF16)
                    stv, b_st = SB.get("st", (32,), F32)
                    hT, b_hT = SB.get("hT", (8, 128), BF16)
                    sm, b_sm = SB.get("sm", (456,), F32)
                    ust, b_ust = SB.get("ust", (4, 131), F32)
                    Cst, b_Cst = SB.get("Cst", (4, 129), F32, parts=64)
                    Cbf, b_Cbf = SB.get("Cbf", (4, 129), BF16, parts=64)
                    g8, b_g8 = SB.get("g8", (64,), F32)
                    vaug, b_vaug = SB.get("vaug", (4, 129), BF16)
                    TA, b_TA = SB.get("TA", (4, 128), F32)
                    TB, b_TB = SB.get("TB", (4, 128), F32)
                    uc, b_uc = SB.get("uc", (4, 128), BF16)
                    og, b_og = SB.get("og", (512,), BF16)
                    qTs, b_qTs = SB.get("qTs", (4, 128), BF16, parts=64)
                    kTs, b_kTs = SB.get("kTs", (4, 128), BF16, parts=64)
                    PT, b_PT = SB.get("PT", (4, 128), BF16)
                    qtil, b_qtil = SB.at("qtil", b_kTs, (4, 128), BF16, parts=64)
                    ktil, b_ktil = SB.at("ktil", b_qTs, (4, 64), BF16)
                    yms = [A.view("ym%d" % i, W_LO + 46208 + i * 1024, (4, 128), BF16) for i in range(2)]
                    junk, b_junk = SB.at("junk", b_qTs, (128,), BF16, off=512)
                    cqn, b_cqn = SB.get("cqn", (256,), BF16)
                    ckvn, b_ckvn = SB.get("ckvn", (128,), BF16)
                    krn, b_krn = SB.get("krn", (64,), F32)
                    kt4, b_kt4 = SB.get("kt4", (4, 32), F32)
                    krot, b_krot = SB.at("krot", b_krn, (2, 64), BF16)
                    cqnT, b_cqnT = SB.at("cqnT", b_cqn, (2, 128), BF16)
                    ckvnT, b_ckvnT = SB.at("ckvnT", b_ckvn, (128,), BF16)
                    stq, b_stq = SB.get("stq", (32,), F32)
                    qr, b_qr = SB.get("qr", (4, 64), F32)
                    qt4, b_qt4 = SB.get("qt4", (2, 4, 32), F32)
                    qrot, b_qrot = SB.get("qrot", (4, 64), BF16)
                    junkk, b_junkk = SB.get("junkk", (128,), BF16)
                    qn, b_qn = SB.at("qn", b_xnb, (4, 128), BF16)
                    kn, b_kn = SB.at("kn", b_xnb, (4, 128), BF16, off=1024)
                    ymT, b_ymT = SB.at("ymT", b_qr, (4, 128), BF16)
                    PS_all = PS
                    PS_M = PSM([0, 1, 2, 3, 4])
                    PS_M1 = PSM([0, 1, 2])
                    PS_M2 = PSM([3, 4])
                    PS_K = PSM([5, 6, 7])

                    def mix(streams):
                        streams = [s_ for s_ in streams if s_]
                        pos = [0] * len(streams)
                        outl = []
                        for _ in range(sum(len(s_) for s_ in streams)):
                            best = None
                            for i, s_ in enumerate(streams):
                                if pos[i] < len(s_):
                                    fr = pos[i] / float(len(s_))
                                    if best is None or fr < best[0]:
                                        best = (fr, i)
                            outl.append(streams[best[1]][pos[best[1]]])
                            pos[best[1]] += 1
                        return outl

                    def emit_Y(Tp):
                        ymp, b_ymp = yms[Tp % 2]
                        bT = PS.get()
                        pT = pbank_bf(bT[0])[:, 0:512].rearrange("p (a b) -> p a b", a=4)
                        P.op("pe", f_tr([(pT[:, h, :], ymp[:, h, :]) for h in range(4)], identb),
                             reads=[b_ymp, b_identb], writes=[PB[bT[0]]])
                        P.op("dve", f_tt(ymT, pT, bc3(mog[:, l, :], [128, 4, 128]), ALU.mult),
                             reads=[PB[bT[0]], b_ggrp], writes=[b_ymT])
                        for hf in range(2):
                            P.op("pe", f_mm([(pbank(bT[0]), [(ymT[:, h, :], wout[:, h, hf * 512:(hf + 1) * 512]) for h in range(4)])]),
                                 reads=[b_ymT, b_wout], writes=[PB[bT[0]]])
                            P.op("dve", f_tt(X[:, Tp, hf * 512:(hf + 1) * 512], X[:, Tp, hf * 512:(hf + 1) * 512], pbank(bT[0]), ALU.add),
                                 reads=[XB[Tp], PB[bT[0]]], writes=[XB[Tp]])
                        PS.rel(bT)

                    if do_mlstm:
                        P.op("pool", f_memset(vaug[:, :, 128:129], 1.0), writes=[b_vaug])
                    if do_mla:
                        P.op("pool", f_memset(va[:, :, :, 128:129], 1.0), writes=[b_va])

                    for T in range(NT):
                        tc_ = slice(T * 128, (T + 1) * 128)
                        PS = PS_M
                        norm_to_hT(X[:, T, :], XB[T], gcol[:, l, 0, :], hT, b_hT, (xnb, b_xnb, stv, b_st))
                        bk = PS.get()
                        P.op("pe", f_mm([(psum[:, bk[0], 0:456], [(hT[:, k, :], Win[:, k, 1536:1992]) for k in range(8)])]),
                             reads=[b_hT, b_Win], writes=[PB[bk[0]]])
                        P.op("act", f_act(sm, psum[:, bk[0], 0:456], AF.Copy), reads=[PB[bk[0]]], writes=[b_sm])
                        PS.rel(bk)
                        strM1 = strM2 = strJ = []
                        if do_mlstm:
                            P.begin()
                            PS = PS_M1
                            bU = PS.get()
                            pU = pbank(bU[0]).rearrange("p (c t) -> p c t", c=4)
                            P.op("pe", f_mm([(pU[:, c, :], [(Win[:, k, c * 128:(c + 1) * 128], hT[:, k, :]) for k in range(8)])
                                             for c in range(4)]),
                                 reads=[b_hT, b_Win], writes=[PB[bU[0]]])
                            if T == 0:
                                P.op("pool", f_memset(ust[:, :, 0:3], 0.0), writes=[b_ust])
                            else:
                                P.op("dve", f_cp(ust[:, :, 0:3], ust[:, :, 128:131]), reads=[b_ust], writes=[b_ust])
                            P.op("act", f_act(ust[:, :, 3:131], pU, AF.Copy), reads=[PB[bU[0]]], writes=[b_ust])
                            for c in range(4):
                                P.op("dve", f_ts(TA[:, c, :], ust[:, c, 3:131], convw[:, l, c, 3:4], ALU.mult,
                                                 convb[:, l, c:c + 1], ALU.add),
                                     reads=[b_ust, b_ggrp], writes=[b_TA])
                                for j in range(3):
                                    P.op("dve", f_stt(TA[:, c, :], ust[:, c, j:j + 128], convw[:, l, c, j:j + 1], TA[:, c, :],
                                                      ALU.mult, ALU.add),
                                         reads=[b_ust, b_ggrp, b_TA], writes=[b_TA])
                            P.op("act", f_act(pU, TA, AF.Exp, scale=-1.0), reads=[b_TA], writes=[PB[bU[0]]])
                            P.op("act", f_act(pU, pU, AF.Ln, bias=1.0), reads=[PB[bU[0]]], writes=[PB[bU[0]]])
                            P.op("act", f_act(pU, pU, AF.Exp, scale=-1.0), reads=[PB[bU[0]]], writes=[PB[bU[0]]])
                            P.op("dve", f_tt(uc, TA, pU, ALU.mult), reads=[b_TA, PB[bU[0]]], writes=[b_uc])
                            PS.rel(bU)
                            bQ = PS.get()
                            bK = PS.get()
                            bKt = PS.get()
                            pQ = psum[0:64, bQ[0], :].rearrange("p (h t) -> p h t", h=4)
                            pK = psum[0:64, bK[0], :].rearrange("p (h t) -> p h t", h=4)
                            pKt = psum[:, bKt[0], 0:256].rearrange("p (h d) -> p h d", h=4)
                            P.op("pe", f_mm([(pQ[:, h, :], [(wqk[:, h, 0:64], uc[:, h, :])]) for h in range(4)]),
                                 reads=[b_wqk, b_uc], writes=[PB[bQ[0]]])
                            P.op("pe", f_mm([(pK[:, h, :], [(wqk[:, h, 64:128], uc[:, h, :])]) for h in range(4)]),
                                 reads=[b_wqk, b_uc], writes=[PB[bK[0]]])
                            P.op("pe", f_mm([(pKt[:, h, :], [(uc[:, h, :], wqk[:, h, 64:128])]) for h in range(4)]),
                                 reads=[b_wqk, b_uc], writes=[PB[bKt[0]]])
                            P.op("act", f_act(qTs, pQ, AF.Identity, scale=0.125), reads=[PB[bQ[0]]], writes=[b_qTs])
                            P.op("dve", f_cp(kTs, pK), reads=[PB[bK[0]]], writes=[b_kTs])
                            PS.rel(bK)
                            bS = PS.get()
                            pS2 = pbank(bS[0]).rearrange("p (h t) -> p h t", h=4)
                            P.op("pe", f_mm([(pS2[:, h, :], [(kTs[:, h, :], qTs[:, h, :])]) for h in range(4)]),
                                 reads=[b_kTs, b_qTs], writes=[PB[bS[0]]])
                            strM1 = P.end()
                            P.begin()
                            PS = PS_M2
                            gi = g8[:, 0:4]
                            lf = g8[:, 16:20]
                            P.op("dve", f_tt(g8[:, 0:8], sm[:, 0:8], bif[:, l, :], ALU.add), reads=[b_sm, b_ggrp], writes=[b_g8])
                            P.op("act", f_act(g8[:, 8:12], g8[:, 4:8], AF.Exp, scale=-1.0), reads=[b_g8], writes=[b_g8])
                            P.op("act", f_act(g8[:, 12:16], g8[:, 8:12], AF.Ln, bias=1.0), reads=[b_g8], writes=[b_g8])
                            P.op("dve", f_ts(lf, g8[:, 12:16], -1.0, ALU.mult), reads=[b_g8], writes=[b_g8])
                            P.op("dve", f_tt(TB, triu.unsqueeze(1).to_broadcast([128, 4, 128]), bc3(lf, [128, 4, 128]), ALU.mult),
                                 reads=[b_triu, b_g8], writes=[b_TB])
                            bB = PS.get()
                            pB = pbank(bB[0])
                            P.op("pe", f_mm([(pB, [(onesf, TB.rearrange("p a b -> p (a b)"))])]),
                                 reads=[b_onesf, b_TB], writes=[PB[bB[0]]])
                            bC = PS.get()
                            pC = pbank(bC[0])
                            P.op("pe", f_mm([(pC[:, 0:4], [(triu, lf)]), (pC[:, 4:8], [(trisl, lf)])]),
                                 reads=[b_triu, b_trisl, b_g8], writes=[PB[bC[0]]])
                            acol = g8[:, 20:24]
                            wa = g8[:, 28:32]
                            P.op("dve", f_tt(acol, gi, pC[:, 0:4], ALU.subtract), reads=[b_g8, PB[bC[0]]], writes=[b_g8])
                            P.op("dve", f_tt(g8[:, 24:28], gi, pC[:, 4:8], ALU.add), reads=[b_g8, PB[bC[0]]], writes=[b_g8])
                            PS.rel(bC)
                            P.op("act", f_act(wa, g8[:, 24:28], AF.Exp), reads=[b_g8], writes=[b_g8])
                            bk = PS.get()
                            P.op("pe", f_mm([(pbank(bk[0]), [(hT[:, k, :], Win[:, k, 512:1024]) for k in range(8)])]),
                                 reads=[b_hT, b_Win], writes=[PB[bk[0]]])
                            P.op("act", f_act(vaug[:, :, 0:128], pbank(bk[0]).rearrange("p (h e) -> p h e", h=4), AF.Copy),
                                 reads=[PB[bk[0]]], writes=[b_vaug])
                            PS.rel(bk)
                            bk = PS.get()
                            pO_ = pbank(bk[0])
                            P.op("pe", f_mm([(pO_, [(hT[:, k, :], Win[:, k, 1024:1536]) for k in range(8)])]),
                                 reads=[b_hT, b_Win], writes=[PB[bk[0]]])
                            P.op("act", f_act(pO_, pO_, AF.Exp, scale=-1.0), reads=[PB[bk[0]]], writes=[PB[bk[0]]])
                            P.op("act", f_act(pO_, pO_, AF.Ln, bias=1.0), reads=[PB[bk[0]]], writes=[PB[bk[0]]])
                            P.op("act", f_act(og, pO_, AF.Exp, scale=-1.0), reads=[PB[bk[0]]], writes=[b_og])
                            PS.rel(bk)
                            strM2 = P.end()
                            P.begin()
                            PS = PSM([0, 1, 2, 3, 4])
                            ymc, b_ymc = yms[T % 2]
                            P.op("dve", f_tt(ktil, pKt, bc3(wa, [128, 4, 64]), ALU.mult), reads=[PB[bKt[0]], b_g8], writes=[b_ktil])
                            pB3 = pB.rearrange("p (h t) -> p h t", h=4)
                            for h in range(4):
                                P.op("dve", f_stt(TA[:, h, :], pB3[:, h, :], acol[:, h:h + 1], mneg, ALU.add, ALU.add),
                                     reads=[PB[bB[0]], b_g8, b_mneg], writes=[b_TA])
                            P.op("act", f_act(TA, TA, AF.Exp), reads=[b_TA], writes=[b_TA])
                            P.op("dve", f_tt(PT, pS2, TA, ALU.mult), reads=[PB[bS[0]], b_TA], writes=[b_PT])
                            eB = TB[0:64]
                            P.op("act", f_act(eB, pB3[0:64], AF.Exp), reads=[PB[bB[0]]], writes=[b_TB])
                            P.op("dve", f_stt(qtil, pQ, 0.125, eB, ALU.mult, ALU.mult), reads=[PB[bQ[0]], b_TB], writes=[b_qtil])
                            bH = PS.get(2)

                            def Hh(h, bH=bH):
                                return psum[:, bH[0] + h // 2, (h % 2) * 129:(h % 2) * 129 + 129]
                            grp = []
                            for h in range(4):
                                pairs = []
                                if T > 0:
                                    pairs.append((qtil[:, h, :], Cbf[:, h, :]))
                                pairs.append((PT[:, h, :], vaug[:, h, :]))
                                grp.append((Hh(h), pairs))
                            P.op("pe", f_mm(grp), reads=[b_qtil, b_Cbf, b_PT, b_vaug], writes=[PB[bH[0]], PB[bH[1]]])
                            den = g8[:, 44:48]
                            den2 = den.rearrange("p (a b) -> p a b", b=2)
                            P.op("act", f_act(den2[:, :, 0], psum[:, bH[0]:bH[0] + 2, 128], AF.Abs), reads=[PB[bH[0]], PB[bH[1]]], writes=[b_g8])
                            P.op("act", f_act(den2[:, :, 1], psum[:, bH[0]:bH[0] + 2, 257], AF.Abs), reads=[PB[bH[0]], PB[bH[1]]], writes=[b_g8])
                            rr = g8[:, 48:52]
                            P.op("dve", f_ts(rr, den, 1.0, ALU.max), reads=[b_g8], writes=[b_g8])
                            P.op("dve", f_recip(rr, rr), reads=[b_g8], writes=[b_g8])
                            ssm = g8[:, 52:56]
                            for h in range(4):
                                P.op("act", f_act(junk, Hh(h)[:, 0:128], AF.Square, scale=rr[:, h:h + 1], accum=ssm[:, h:h + 1]),
                                     reads=[PB[bH[0]], PB[bH[1]], b_g8], writes=[b_junk, b_g8])
                            P.op("act", f_act(g8[:, 56:60], ssm, AF.Ln, scale=1.0 / 128.0, bias=EPS), reads=[b_g8], writes=[b_g8])
                            P.op("act", f_act(g8[:, 56:60], g8[:, 56:60], AF.Exp, scale=-0.5), reads=[b_g8], writes=[b_g8])
                            tot = g8[:, 60:64]
                            P.op("dve", f_tt(tot, rr, g8[:, 56:60], ALU.mult), reads=[b_g8], writes=[b_g8])
                            for h in range(4):
                                P.op("dve", f_stt(ymc[:, h, :], Hh(h)[:, 0:128], tot[:, h:h + 1], og[:, h * 128:(h + 1) * 128],
                                                  ALU.mult, ALU.mult),
                                     reads=[PB[bH[0]], PB[bH[1]], b_g8, b_og], writes=[b_ymc])
                            PS.rel(bH)
                            if T < NT - 1:
                                bL = PS.get(2)

                                def Cl(h, bL=bL):
                                    return psum[0:64, bL[0] + h // 2, (h % 2) * 129:(h % 2) * 129 + 129]
                                P.op("pe", f_mm([(Cl(h), [(ktil[:, h, :], vaug[:, h, :])]) for h in range(4)]),
                                     reads=[b_ktil, b_vaug], writes=[PB[bL[0]], PB[bL[1]]])
                                for h in range(4):
                                    if T == 0:
                                        P.op("dve", f_cp(Cst[:, h, :], Cl(h)), reads=[PB[bL[0]], PB[bL[1]]], writes=[b_Cst])
                                    else:
                                        P.op("dve", f_stt(Cst[:, h, :], Cst[:, h, :], eB[:, h, 127:128], Cl(h), ALU.mult, ALU.add),
                                             reads=[b_Cst, b_TB, PB[bL[0]], PB[bL[1]]], writes=[b_Cst])
                                PS.rel(bL)
                                P.op("act", f_act(Cbf, Cst, AF.Copy), reads=[b_Cst], writes=[b_Cbf])
                            strJ = P.end()
                            PS_M1.free = [0, 1, 2]
                            PS_M2.free = [3, 4]
                        strM = mix([strM1, strM2]) + strJ
                        PS = PS_K
                        P.begin()
                        if do_mlstm and T > 0:
                            emit_Y(T - 1)
                        if do_mla:
                            P.op("act", f_act(junkk, sm[:, 8:136], AF.Square, scale=256.0 ** -0.5, accum=stq[:, 0:1]),
                                 reads=[b_sm], writes=[b_junkk, b_stq])
                            P.op("act", f_act(junkk, sm[:, 136:264], AF.Square, scale=256.0 ** -0.5, accum=stq[:, 3:4]),
                                 reads=[b_sm], writes=[b_junkk, b_stq])
                            P.op("act", f_act(junkk, sm[:, 264:392], AF.Square, scale=128.0 ** -0.5, accum=stq[:, 1:2]),
                                 reads=[b_sm], writes=[b_junkk, b_stq])
                            P.op("act", f_act(junkk[:, 0:64], sm[:, 392:456], AF.Square, scale=64.0 ** -0.5, accum=stq[:, 2:3]),
                                 reads=[b_sm], writes=[b_junkk, b_stq])
                            P.op("dve", f_tt(stq[:, 0:1], stq[:, 0:1], stq[:, 3:4], ALU.add), reads=[b_stq], writes=[b_stq])
                            rstd_from_ms(stq, b_stq, 0, 4, n=3)
                            P.op("dve", f_ts(cqn, sm[:, 8:264], stq[:, 4:5], ALU.mult), reads=[b_sm, b_stq], writes=[b_cqn])
                            P.op("dve", f_ts(ckvn, sm[:, 264:392], stq[:, 5:6], ALU.mult), reads=[b_sm, b_stq], writes=[b_ckvn])
                            P.op("dve", f_stt(krn, sm[:, 392:456], stq[:, 6:7], gkr[:, l, :], ALU.mult, ALU.mult),
                                 reads=[b_sm, b_stq, b_ggrp], writes=[b_krn])
                            cT = cosb[:, T, :]
                            sT = sinb[:, T, :]
                            P.op("dve", f_tt(kt4[:, 0, :], krn[:, 0:32], cT, ALU.mult), reads=[b_krn, b_cos], writes=[b_kt4])
                            P.op("dve", f_tt(kt4[:, 1, :], krn[:, 32:64], sT, ALU.mult), reads=[b_krn, b_sin], writes=[b_kt4])
                            P.op("dve", f_tt(kt4[:, 2, :], krn[:, 32:64], cT, ALU.mult), reads=[b_krn, b_cos], writes=[b_kt4])
                            P.op("dve", f_tt(kt4[:, 3, :], krn[:, 0:32], sT, ALU.mult), reads=[b_krn, b_sin], writes=[b_kt4])
                            P.op("dve", f_tt(krot[:, 0, 0:32], kt4[:, 0, :], kt4[:, 1, :], ALU.subtract), reads=[b_kt4], writes=[b_krot])
                            P.op("dve", f_tt(krot[:, 0, 32:64], kt4[:, 2, :], kt4[:, 3, :], ALU.add), reads=[b_kt4], writes=[b_krot])
                            P.op("dve", f_cp(krot[:, 1, :], krot[:, 0, :]), reads=[b_krot], writes=[b_krot])
                            bT = PS.get()
                            pT = pbank_bf(bT[0]).rearrange("p (a b) -> p a b", a=8)
                            P.op("pe", f_tr([(pT[:, 0, :], cqn[:, 0:128]), (pT[:, 1, :], cqn[:, 128:256]),
                                             (pT[:, 2, :], ckvn), (pT[:, 3, :], krot.rearrange("p a b -> p (a b)"))], identb),
                                 reads=[b_cqn, b_ckvn, b_krot, b_identb], writes=[PB[bT[0]]])
                            P.op("dve", f_tt(cqnT, pT[:, 0:2, :], bc3(cqg[:, l, :], [128, 2, 128]), ALU.mult),
                                 reads=[PB[bT[0]], b_ggrp], writes=[b_cqnT])
                            P.op("dve", f_ts(ckvnT, pT[:, 2, :], misc[:, l, 0:1], ALU.mult), reads=[PB[bT[0]], b_ggrp], writes=[b_ckvnT])
                            P.op("act", f_act(krT[:, tc_], pT[:, 3, :], AF.Copy), reads=[PB[bT[0]]], writes=[b_krT])
                            PS.rel(bT)
                            bq = PS.get(2)
                            P.op("pe", f_mm([(psum[:, bq[0], :], [(cqnT[:, kc, :], wuq[:, kc, 0:512]) for kc in range(2)]),
                                             (psum[:, bq[1], 0:256], [(cqnT[:, kc, :], wuq[:, kc, 512:768]) for kc in range(2)])]),
                                 reads=[b_cqnT, b_wuq], writes=[PB[bq[0]], PB[bq[1]]])
                            qa2 = psum[:, bq[0]:bq[0] + 2, :].rearrange("p a b -> p (a b)")[:, 0:768]
                            qa = qa2.rearrange("p (h c) -> p h c", h=4)
                            rq = [PB[bq[0]], PB[bq[1]]]
                            for h in range(4):
                                P.op("act", f_act(junkk, qa[:, h, 0:128], AF.Square, scale=128.0 ** -0.5, accum=stq[:, 20 + h:21 + h]),
                                     reads=rq, writes=[b_junkk, b_stq])
                                P.op("act", f_act(junkk[:, 0:64], qa[:, h, 128:192], AF.Square, scale=64.0 ** -0.5, accum=stq[:, 24 + h:25 + h]),
                                     reads=rq, writes=[b_junkk, b_stq])
                            rstd_from_ms(stq, b_stq, 20, 8, n=8)
                            P.op("dve", f_tt(qn, qa[:, :, 0:128], bc3(stq[:, 8:12], [128, 4, 128]), ALU.mult), reads=rq + [b_stq], writes=[b_qn])
                            P.op("dve", f_tt(qr, qa[:, :, 128:192], bc3(stq[:, 12:16], [128, 4, 64]), ALU.mult), reads=rq + [b_stq], writes=[b_qr])
                            PS.rel(bq)
                            P.op("dve", f_tt(qr, qr, gqr[:, l, :].unsqueeze(1).to_broadcast([128, 4, 64]), ALU.mult),
                                 reads=[b_qr, b_ggrp], writes=[b_qr])
                            cT4 = cT.unsqueeze(1).to_broadcast([128, 4, 32])
                            sT4 = sT.unsqueeze(1).to_broadcast([128, 4, 32])
                            P.op("dve", f_tt(qt4[:, 0], qr[:, :, 0:32], cT4, ALU.mult), reads=[b_qr, b_cos], writes=[b_qt4])
                            P.op("dve", f_tt(qt4[:, 1], qr[:, :, 32:64], sT4, ALU.mult), reads=[b_qr, b_sin], writes=[b_qt4])
                            P.op("dve", f_tt(qrot[:, :, 0:32], qt4[:, 0], qt4[:, 1], ALU.subtract), reads=[b_qt4], writes=[b_qrot])
                            P.op("dve", f_tt(qt4[:, 0], qr[:, :, 32:64], cT4, ALU.mult), reads=[b_qr, b_cos], writes=[b_qt4])
                            P.op("dve", f_tt(qt4[:, 1], qr[:, :, 0:32], sT4, ALU.mult), reads=[b_qr, b_sin], writes=[b_qt4])
                            P.op("dve", f_tt(qrot[:, :, 32:64], qt4[:, 0], qt4[:, 1], ALU.add), reads=[b_qt4], writes=[b_qrot])
                            bT = PS.get()
                            pT = pbank_bf(bT[0]).rearrange("p (a b) -> p a b", a=8)
                            qrot2 = qrot.rearrange("p (i a) b -> p i (a b)", i=2)
                            P.op("pe", f_tr([(pT[:, h, :], qn[:, h, :]) for h in range(4)] +
                                            [(pT[:, 4 + i, :], qrot2[:, i, :]) for i in range(2)], identb),
                                 reads=[b_qn, b_qrot, b_identb], writes=[PB[bT[0]]])
                            P.op("dve", f_ts(qnT[:, :, tc_], pT[:, 0:4, :], misc[:, l, 1:2], ALU.mult, QSC, ALU.mult),
                                 reads=[PB[bT[0]], b_ggrp], writes=[b_qnT])
                            P.op("act", f_act(qrT[:, :, tc_], pT[:, 4:6, :], AF.Identity, scale=QSC), reads=[PB[bT[0]]], writes=[b_qrT])
                            PS.rel(bT)
                            bkv = PS.get(2)
                            P.op("pe", f_mm([(pbank(bkv[hf]), [(ckvnT, wukv[:, hf * 512:(hf + 1) * 512])]) for hf in range(2)]),
                                 reads=[b_ckvnT, b_wukv], writes=[PB[bkv[0]], PB[bkv[1]]])
                            kv = psum[:, bkv[0]:bkv[0] + 2, :].rearrange("p a (h c) -> p (a h) c", c=256)
                            rkv = [PB[bkv[0]], PB[bkv[1]]]
                            for h in range(4):
                                P.op("act", f_act(junkk, kv[:, h, 0:128], AF.Square, scale=128.0 ** -0.5, accum=stq[:, 28 + h:29 + h]),
                                     reads=rkv, writes=[b_junkk, b_stq])
                            rstd_from_ms(stq, b_stq, 28, 16, n=4)
                            P.op("dve", f_tt(kn, kv[:, :, 0:128], bc3(stq[:, 16:20], [128, 4, 128]), ALU.mult), reads=rkv + [b_stq], writes=[b_kn])
                            P.op("act", f_act(va[:, T, :, 0:128], kv[:, :, 128:256], AF.Copy), reads=rkv, writes=[b_va])
                            PS.rel(bkv)
                            bT = PS.get()
                            pT = pbank_bf(bT[0])[:, 0:512].rearrange("p (a b) -> p a b", a=4)
                            P.op("pe", f_tr([(pT[:, h, :], kn[:, h, :]) for h in range(4)], identb),
                                 reads=[b_kn, b_identb], writes=[PB[bT[0]]])
                            P.op("dve", f_ts(knT[:, :, tc_], pT, misc[:, l, 2:3], ALU.mult), reads=[PB[bT[0]], b_ggrp], writes=[b_knT])
                            PS.rel(bT)
                        strK = P.end()
                        P.merge([strM, strK])
                    PS = PS_K
                    if do_mlstm:
                        emit_Y(NT - 1)
                    PS = PS_all

                if do_mla:
                    load_w("pool", wout, b_wout, dr["w_out"][l, 512:1024, :].rearrange("(k p) n -> p k n", p=128))
                    SB.reset()
                    LOOKAHEAD = 2
                    ePs = [SB.get("eP%d" % i, (512,), BF16) for i in range(LOOKAHEAD + 2)]
                    ya, b_ya = SB.get("ya", (4, 128), BF16)
                    yaT, b_yaT = SB.get("yaT", (4, 512), BF16)
                    stb, b_stb = SB.get("stb", (16,), F32)
                    junk2, b_junk2 = SB.get("junk2", (128,), BF16)
                    epi = [0]
                    PS_all = PS
                    PS = PSM([4, 5])
                    BT_ = 7
                    BY_ = 6

                    def rec_N(qg, h, bO):
                        P.begin()
                        r_ = h % 2
                        ip = h // 2

                        def Oq(j):
                            return psum[:, bO[0] + j // 2, (j % 2) * 129:(j % 2) * 129 + 129]
                        nkb = 4 * qg + 4

                        def emit_S(kb):
                            j0 = max(0, kb - 4 * qg)
                            n0 = j0 * 128
                            kc_ = slice(kb * 128, (kb + 1) * 128)
                            bS = PS.get()
                            pS = pbank(bS[0])
                            P.op("pe", f_mm([(pS[:, n0:512], [(knT[:, h, kc_], qnT[:, h, qg * 512 + n0:(qg + 1) * 512]),
                                                               (krT[r_ * 64:(r_ + 1) * 64, kc_],
                                                                qrT[r_ * 64:(r_ + 1) * 64, ip, qg * 512 + n0:(qg + 1) * 512])])]),
                                 reads=[b_knT, b_qnT, b_krT, b_qrT], writes=[PB[bS[0]]])
                            eP, b_eP = ePs[epi[0] % len(ePs)]
                            epi[0] += 1
                            P.op("act", f_act(eP[:, n0:512], pS[:, n0:512], AF.Exp), reads=[PB[bS[0]]], writes=[b_eP])
                            PS.rel(bS)
                            if kb >= 4 * qg:
                                P.op("dve", f_tt(eP[:, n0:n0 + 128], eP[:, n0:n0 + 128], m01b, ALU.mult),
                                     reads=[b_eP, b_m01b], writes=[b_eP])
                            return (kb, j0, eP, b_eP)

                        def emit_PV(item):
                            kb, j0, eP, b_eP = item
                            grp = []
                            for j in range(j0, 4):
                                qb = 4 * qg + j
                                grp.append((Oq(j), eP[:, j * 128:(j + 1) * 128], va[:, kb, h, :],
                                            (kb == 0 and j % 2 == 0), kb == qb))

                            def pv(e, grp=grp):
                                ins = None
                                for (o_, l_, r2, st_, sp_) in grp:
                                    ins = e.matmul(o_, lhsT=l_, rhs=r2, start=st_, stop=sp_, skip_group_check=True)
                                return ins
                            P.op("pe", pv, reads=[b_eP, b_va], writes=[PB[bO[0]], PB[bO[1]]])
                        pend_s = []
                        for kb in range(nkb):
                            pend_s.append(emit_S(kb))
                            if len(pend_s) > LOOKAHEAD:
                                emit_PV(pend_s.pop(0))
                        while pend_s:
                            emit_PV(pend_s.pop(0))
                        return P.end()

                    def rec_E(qg, h, bO):
                        P.begin()

                        def Oq(j):
                            return psum[:, bO[0] + j // 2, (j % 2) * 129:(j % 2) * 129 + 129]
                        rO = [PB[bO[0]], PB[bO[1]]]
                        den = stb[:, 0:4]
                        den2 = den.rearrange("p (a b) -> p a b", b=2)
                        P.op("act", f_act(den2[:, :, 0], psum[:, bO[0]:bO[0] + 2, 128], AF.Copy), reads=rO, writes=[b_stb])
                        P.op("act", f_act(den2[:, :, 1], psum[:, bO[0]:bO[0] + 2, 257], AF.Copy), reads=rO, writes=[b_stb])
                        P.op("dve", f_recip(stb[:, 4:8], den), reads=[b_stb], writes=[b_stb])
                        for j in range(4):
                            P.op("act", f_act(junk2, Oq(j)[:, 0:128], AF.Square, scale=stb[:, 4 + j:5 + j], accum=stb[:, 8 + j:9 + j]),
                                 reads=rO + [b_stb], writes=[b_junk2, b_stb])
                        P.op("act", f_act(stb[:, 12:16], stb[:, 8:12], AF.Ln, scale=1.0 / 128.0, bias=EPS), reads=[b_stb], writes=[b_stb])
                        P.op("act", f_act(stb[:, 12:16], stb[:, 12:16], AF.Exp, scale=-0.5), reads=[b_stb], writes=[b_stb])
                        P.op("dve", f_tt(stb[:, 12:16], stb[:, 12:16], stb[:, 4:8], ALU.mult), reads=[b_stb], writes=[b_stb])
                        for j in range(4):
                            P.op("act", f_act(ya[:, j, :], Oq(j)[:, 0:128], AF.Identity, scale=stb[:, 12 + j:13 + j]),
                                 reads=rO + [b_stb], writes=[b_ya])
                        pT = pbank_bf(BT_)[:, 0:512]
                        P.op("pe", f_tr([(pT[:, j * 128:(j + 1) * 128], ya[:, j, :]) for j in range(4)], identb),
                             reads=[b_ya, b_identb], writes=[PB[BT_]])
                        P.op("dve", f_ts(yaT[:, h, :], pT, aog[:, l, h:h + 1], ALU.mult), reads=[PB[BT_], b_ggrp], writes=[b_yaT])
                        return P.end()

                    def rec_W(qg):
                        P.begin()
                        for j in range(4):
                            T = 4 * qg + j
                            for hf in range(2):
                                P.op("pe", f_mm([(pbank(BY_), [(yaT[:, h, j * 128:(j + 1) * 128], wout[:, h, hf * 512:(hf + 1) * 512])
                                                               for h in range(4)])]),
                                     reads=[b_yaT, b_wout], writes=[PB[BY_]])
                                P.op("dve", f_tt(X[:, T, hf * 512:(hf + 1) * 512], X[:, T, hf * 512:(hf + 1) * 512], pbank(BY_), ALU.add),
                                     reads=[XB[T], PB[BY_]], writes=[XB[T]])
                        return P.end()

                    prev = None
                    u = 0
                    for qg in range(4):
                        for h in range(4):
                            bO = [0, 1] if u % 2 == 0 else [2, 3]
                            u += 1
                            strN = rec_N(qg, h, bO)
                            P.merge([prev, strN] if prev else [strN])
                            prev = rec_E(qg, h, bO)
                            if h == 3:
                                prev = prev + rec_W(qg)
                    P.merge([prev])
                    PS = PS_all

                if do_xattn:
                    load_w("pool", wqx, b_wqx, dr["wq_x"][l].rearrange("(k p) n -> p k n", p=128))
                    load_w("pool", wkvx, b_wkvx, dr["wkv_x"][l].rearrange("(k p) n -> p k n", p=128))
                    load_w("pool", wox, b_wox, dr["wo_x"][l].rearrange("(k p) n -> p k n", p=128))
                    RB.reset()
                    memf, b_memf = RB.get("memf", (2, D), F32)
                    memT, b_memT = RB.get("memT", (2, 8, 128), BF16)
                    xkT, b_xkT = RB.get("xkT", (4, 256), BF16)
                    xv, b_xv = RB.get("xv", (2, 512), BF16)
                    hTg, b_hTg = RB.get("hTg", (4, 8, 128), BF16)
                    xqT, b_xqT = RB.get("xqT", (4, 512), BF16)
                    oT, b_oT = RB.get("oT", (4, 512), BF16)
                    lnS, b_lnS = RB.get("lnS", (512,), F32)
                    ePx = [RB.get("ePx%d" % i, (512,), BF16) for i in range(2)]
                    xkn, b_xkn = RB.get("xkn", (4, 128), BF16)
                    SB.reset()
                    xnb, b_xnb = SB.get("xnb", (D,), BF16)
                    stv, b_st = SB.get("st", (32,), F32)
                    junk3, b_junk3 = SB.get("junk3", (128,), BF16)
                    qscr = [SB.get("xnbq%d" % i, (D,), BF16) + SB.get("stq%d" % i, (32,), F32) for i in range(2)]
                    qjk = [SB.get("junkq%d" % i, (128,), BF16) for i in range(2)]
                    qxk = [RB.get("xknq%d" % i, (4, 128), BF16) for i in range(2)]
                    P.dma("sp", (lambda e, s, sq=sq: e.dma_start(out=memf, in_=dr["mem"][sq].rearrange("(m p) d -> p m d", p=128)).then_inc(s, 16)),
                          writes=[b_memf])
                    for m in range(2):
                        norm_to_hT(memf[:, m, :], b_memf, gcol[:, l, 2, :], memT[:, m], b_memT, (xnb, b_xnb, stv, b_st))
                    for m in range(2):
                        bkv = PS.get(2)
                        P.op("pe", f_mm([(pbank(bkv[hf]), [(memT[:, m, k, :], wkvx[:, k, hf * 512:(hf + 1) * 512]) for k in range(8)])
                                         for hf in range(2)]),
                             reads=[b_memT, b_wkvx], writes=[PB[bkv[0]], PB[bkv[1]]])
                        pK4 = pbank(bkv[0]).rearrange("p (h d) -> p h d", h=4)
                        for h in range(4):
                            P.op("act", f_act(junk3, pK4[:, h, :], AF.Square, scale=XSC, accum=stv[:, 8 + h:9 + h]),
                                 reads=[PB[bkv[0]]], writes=[b_junk3, b_st])
                        rstd_from_ms(stv, b_st, 8, 12, n=4)
                        P.op("dve", f_tt(xkn, pK4, bc3(stv[:, 12:16], [128, 4, 128]), ALU.mult), reads=[PB[bkv[0]], b_st], writes=[b_xkn])
                        P.op("act", f_act(xv[:, m, :], pbank(bkv[1]), AF.Copy), reads=[PB[bkv[1]]], writes=[b_xv])
                        PS.rel(bkv)
                        bT = PS.get()
                        pT = pbank_bf(bT[0])[:, 0:512].rearrange("p (a b) -> p a b", a=4)
                        P.op("pe", f_tr([(pT[:, h, :], xkn[:, h, :]) for h in range(4)], identb),
                             reads=[b_xkn, b_identb], writes=[PB[bT[0]]])
                        P.op("dve", f_ts(xkT[:, :, m * 128:(m + 1) * 128], pT, misc[:, l, 4:5], ALU.mult),
                             reads=[PB[bT[0]], b_ggrp], writes=[b_xkT])
                        PS.rel(bT)
                    PS_Call = PS
                    PS_Q = PSM([5, 6, 7])
                    PS_AT = PSM([0, 1, 2, 3, 4])
                    hTgs = [(hTg, b_hTg), RB.get("hTg1", (4, 8, 128), BF16)]
                    xqTs = [(xqT, b_xqT), RB.get("xqT1", (4, 512), BF16)]

                    def rec_Q(g):
                        hTg_, b_hTg_ = hTgs[g % 2]
                        xqT_, b_xqT_ = xqTs[g % 2]
                        P.begin()
                        for tt in range(4):
                            T = 4 * g + tt
                            xq_, bxq_, sq_, bsq_ = qscr[tt % 2]
                            jq_, bjq_ = qjk[tt % 2]
                            xkq_, bxkq_ = qxk[tt % 2]
                            norm_to_hT(X[:, T, :], XB[T], gcol[:, l, 1, :], hTg_[:, tt], b_hTg_, (xq_, bxq_, sq_, bsq_))
                            bq = PS.get()
                            pQ4 = pbank(bq[0]).rearrange("p (h d) -> p h d", h=4)
                            P.op("pe", f_mm([(pbank(bq[0]), [(hTg_[:, tt, k, :], wqx[:, k, :]) for k in range(8)])]),
                                 reads=[b_hTg_, b_wqx], writes=[PB[bq[0]]])
                            for h in range(4):
                                P.op("act", f_act(jq_, pQ4[:, h, :], AF.Square, scale=XSC, accum=sq_[:, 16 + h:17 + h]),
                                     reads=[PB[bq[0]]], writes=[bjq_, bsq_])
                            rstd_from_ms(sq_, bsq_, 16, 20, n=4)
                            P.op("dve", f_tt(xkq_, pQ4, bc3(sq_[:, 20:24], [128, 4, 128]), ALU.mult), reads=[PB[bq[0]], bsq_], writes=[bxkq_])
                            PS.rel(bq)
                            bT = PS.get()
                            pT = pbank_bf(bT[0])[:, 0:512].rearrange("p (a b) -> p a b", a=4)
                            P.op("pe", f_tr([(pT[:, h, :], xkq_[:, h, :]) for h in range(4)], identb),
                                 reads=[bxkq_, b_identb], writes=[PB[bT[0]]])
                            P.op("dve", f_ts(xqT_[:, :, tt * 128:(tt + 1) * 128], pT, misc[:, l, 3:4], ALU.mult, XSC, ALU.mult),
                                 reads=[PB[bT[0]], b_ggrp], writes=[b_xqT_])
                            PS.rel(bT)
                        return P.end()

                    def rec_AT(g):
                        xqT_, b_xqT_ = xqTs[g % 2]
                        P.begin()
                        for h in range(4):
                            for m in range(2):
                                bS = PS.get()
                                P.op("pe", f_mm([(pbank(bS[0]), [(xkT[:, h, m * 128:(m + 1) * 128], xqT_[:, h, :])])]),
                                     reads=[b_xkT, b_xqT_], writes=[PB[bS[0]]])
                                P.op("act", f_act(ePx[m][0], pbank(bS[0]), AF.Exp), reads=[PB[bS[0]]], writes=[ePx[m][1]])
                                PS.rel(bS)
                            bO = PS.get()
                            bSm = PS.get()
                            P.op("pe", f_mm([(pbank(bO[0]), [(xv[:, m, h * 128:(h + 1) * 128], ePx[m][0]) for m in range(2)]),
                                             (pbank(bSm[0]), [(onesb, ePx[m][0]) for m in range(2)])]),
                                 reads=[b_xv, b_onesb, ePx[0][1], ePx[1][1]], writes=[PB[bO[0]], PB[bSm[0]]])
                            P.op("act", f_act(lnS, pbank(bSm[0]), AF.Ln), reads=[PB[bSm[0]]], writes=[b_lnS])
                            PS.rel(bSm)
                            P.op("act", f_act(lnS, lnS, AF.Exp, scale=-1.0), reads=[b_lnS], writes=[b_lnS])
                            P.op("dve", f_tt(oT[:, h, :], pbank(bO[0]), lnS, ALU.mult), reads=[PB[bO[0]], b_lnS], writes=[b_oT])
                            PS.rel(bO)
                        for tt in range(4):
                            T = 4 * g + tt
                            bY = PS.get(2)
                            P.op("pe", f_mm([(pbank(bY[0] + hf), [(oT[:, h, tt * 128:(tt + 1) * 128], wox[:, h, hf * 512:(hf + 1) * 512])
                                                                  for h in range(4)]) for hf in range(2)]),
                                 reads=[b_oT, b_wox], writes=[PB[bY[0]], PB[bY[1]]])
                            resid_add(T, psum[:, bY[0]:bY[0] + 2, :].rearrange("p a b -> p (a b)"), bY)
                            PS.rel(bY)
                        return P.end()

                    PS = PS_Q
                    strQ = rec_Q(0)
                    P.merge([strQ])
                    for g in range(4):
                        PS = PS_AT
                        strs = [rec_AT(g)]
                        if g < 3:
                            PS = PS_Q
                            strs.append(rec_Q(g + 1))
                        P.merge(strs)
                    PS = PS_Call

                if do_ffn:
                    RB.reset()
                    hTall, b_hTall0 = RB.get("hTall", (NT, 8, 128), BF16)
                    b_hTt = []
                    for T in range(NT):
                        _, bb = A.view("hTall%d" % T, R1_LO + T * 2048, (1024,), BF16)
                        b_hTt.append(bb)
                    SB.reset()
                    dscr = [SB.get("xnbd%d" % i, (D,), BF16) + SB.get("std%d" % i, (32,), F32) for i in range(3)]
                    h1s = [SB.get("h1T%d" % i, (FSL // 128, 512), BF16) for i in range(2)]
                    rls = [SB.get("rl%d" % i, (512,), BF16) for i in range(2)]

                    def load_slice(j):
                        w1s, b1, w2s, b2 = fslots[fslice_ctr[0] % 3]
                        fslice_ctr[0] += 1
                        load_w("pool", w1s, b1, dr["w_ff1"][l][:, j * FSL:(j + 1) * FSL].rearrange("(k p) n -> p k n", p=128))
                        load_w("pool", w2s, b2, dr["w_ff2"][l][j * FSL:(j + 1) * FSL, :].rearrange("(c p) n -> p c n", p=128))
                        return (w1s, b1, w2s, b2)
                    pend = [load_slice(0), load_slice(1)]
                    for T in range(NT):
                        norm_to_hT(X[:, T, :], XB[T], gcol[:, l, 3, :], hTall[:, T], b_hTt[T], dscr[T % 3])
                    it = 0
                    for j in range(NSL):
                        w1s, b1, w2s, b2 = pend.pop(0)
                        for g in range(4):
                            h1T, b_h1T = h1s[it % 2]
                            it += 1
                            for fc in range(FSL // 128):
                                bH1 = PS.get()
                                pH1 = pbank(bH1[0])
                                P.op("pe", f_mm([(pH1.rearrange("p (a b) -> p a b", a=4),
                                                  [(w1s[:, k, fc * 128:(fc + 1) * 128], hTall[:, 4 * g:4 * g + 4, k, :]) for k in range(8)])]),
                                     reads=[b1] + b_hTt[4 * g:4 * g + 4], writes=[PB[bH1[0]]])
                                rl, b_rl = rls[fc % 2]
                                P.op("act", f_act(rl, pH1, AF.Relu), reads=[PB[bH1[0]]], writes=[b_rl])
                                P.op("dve", f_tt(h1T[:, fc, :], pH1, rl, ALU.mult), reads=[PB[bH1[0]], b_rl], writes=[b_h1T])
                                PS.rel(bH1)
                            for tt in range(4):
                                T = 4 * g + tt
                                bY = PS.get(2)
                                P.op("pe", f_mm([(pbank(bY[0] + hf), [(h1T[:, fc, tt * 128:(tt + 1) * 128], w2s[:, fc, hf * 512:(hf + 1) * 512])
                                                                      for fc in range(FSL // 128)]) for hf in range(2)]),
                                     reads=[b_h1T, b2], writes=[PB[bY[0]], PB[bY[1]]])
                                resid_add(T, psum[:, bY[0]:bY[0] + 2, :].rearrange("p a b -> p (a b)"), bY)
                                PS.rel(bY)
                                if j == NSL - 1 and l == depth - 1:
                                    P.dma("sp", (lambda e, s, T=T, sq=sq: e.dma_start(out=out_d[sq, T * 128:(T + 1) * 128, :], in_=X[:, T, :]).then_inc(s, 16)),
                                          reads=[XB[T]])
                        if j + 2 < NSL:
                            pend.append(load_slice(j + 2))
                elif l == depth - 1:
                    for T in range(NT):
                        P.dma("sp", (lambda e, s, T=T, sq=sq: e.dma_start(out=out_d[sq, T * 128:(T + 1) * 128, :], in_=X[:, T, :]).then_inc(s, 16)),
                              reads=[XB[T]])
        P.op("sp", None, reads=[], writes=XB)
        P.emit(st)
    return nc


def _consts():
    idx = np.arange(128)
    triu = (idx[:, None] <= idx[None, :]).astype(np.float32)
    trisl = (idx[:, None] > idx[None, :]).astype(np.float32)
    mneg = ((1.0 - triu) * -30000.0).astype(np.float32)
    inv = (1.0 / (10000.0 ** (np.arange(0, 64, 2, dtype=np.float32) / 64.0))).astype(np.float32)
    invf = np.broadcast_to((inv / np.float32(2.0 * np.pi)).astype(np.float32)[None, :], (128, 32)).copy()
    return {"c_ident": np.eye(128, dtype=np.float32), "c_triu": triu, "c_trisl": trisl, "c_mneg": mneg, "c_invf": invf}


_NC_CACHE = {}


def kernel(**inputs):
    x = np.ascontiguousarray(inputs["x"], dtype=np.float32)
    mem = np.ascontiguousarray(inputs["mem"], dtype=np.float32)
    pos = np.ascontiguousarray(inputs["positions"], dtype=np.int32)
    if "nc" not in _NC_CACHE:
        _NC_CACHE["nc"] = build_program()
    nc = _NC_CACHE["nc"]
    cst = _consts()
    in_maps = []
    for c in range(NCORES):
        sl = slice(c * SEQ_PER_CORE, (c + 1) * SEQ_PER_CORE)
        m = {"x": x[sl], "mem": mem[sl], "pos": pos[sl]}
        for n in WNAMES:
            m[n] = np.ascontiguousarray(inputs[n], dtype=np.float32)
        m.update(cst)
        in_maps.append(m)
    res = run_bass_kernel_spmd(nc, in_maps, core_ids=list(range(NCORES)))
    out = np.concatenate([np.asarray(r["out"]) for r in res.results], axis=0)
    return out.astype(np.float32)
```

```python
import numpy as np
from contextlib import ExitStack
import concourse.bass as bass
import concourse.mybir as mybir
from concourse.bass_utils import run_bass_kernel_spmd

F32 = mybir.dt.float32
BF16 = mybir.dt.bfloat16
I32 = mybir.dt.int32
AF = mybir.ActivationFunctionType
ALU = mybir.AluOpType
AX = mybir.AxisListType

ENGS = ("pe", "act", "dve", "pool", "sp")

NCORES = 8
SEQ_PER_CORE = 2
S = 2048
D = 1024
NT = S // 128
DEPTH = 2
NMEM = 256
EPS = 1e-6
IN_COLS = 1992
DFF = 4096
FSL = 512
NSL = DFF // FSL


class Buf:
    __slots__ = ("name", "lw", "rd", "alias", "lo", "hi", "excl")

    def __init__(self, name, lo=None, hi=None, excl=False):
        self.name = name
        self.excl = excl
        self.lw = None
        self.rd = {}
        self.alias = [self]
        self.lo = lo
        self.hi = hi


class Op:
    __slots__ = ("idx", "eng", "fn", "deps", "is_dma", "k", "waits", "signal",
                 "semval", "snap", "sem", "dval")


class Prog:
    def __init__(self, nc):
        self.nc = nc
        self.ops = []
        self.dma_sems = {}
        self.sem_count = {}
        self.sem_last = {}
        self.defer = None

    def _add(self, eng, fn, reads, writes, is_dma, k):
        o = Op()
        o.idx = len(self.ops)
        o.eng = eng
        o.fn = fn
        o.is_dma = is_dma
        o.k = k
        o.waits = []
        o.signal = False
        o.semval = None
        o.snap = None
        o.sem = None
        o.dval = None
        deps = set()
        for b in reads:
            for a in b.alias:
                if a.lw is not None:
                    deps.add(a.lw)
            if b.excl:
                for ke, vi in b.rd.items():
                    if ke != eng:
                        deps.add(vi)
        for b in writes:
            for a in b.alias:
                if a.lw is not None:
                    deps.add(a.lw)
                deps.update(a.rd.values())
        for b in reads:
            if is_dma:
                b.rd[("d", o.idx)] = o.idx
            else:
                b.rd[eng] = o.idx
        for b in writes:
            b.lw = o.idx
            b.rd = {}
        if is_dma:
            key = (list(writes) + list(reads))[0]
            skey = id(key)
            if skey not in self.dma_sems:
                self.dma_sems[skey] = "dsem%d" % len(self.dma_sems)
            o.sem = skey
            prev = self.sem_last.get(skey)
            if prev is not None:
                deps.add(prev)
            self.sem_last[skey] = o.idx
            self.sem_count[skey] = self.sem_count.get(skey, 0) + 16 * k
            o.dval = self.sem_count[skey]
        deps.discard(o.idx)
        o.deps = deps
        self.ops.append(o)
        return o

    def op(self, eng, fn, reads=(), writes=()):
        if self.defer is not None:
            self.defer.append((eng, fn, list(reads), list(writes), False, 0))
            return None
        return self._add(eng, fn, reads, writes, False, 0)

    def dma(self, eng, fn, reads=(), writes=(), k=1):
        if self.defer is not None:
            self.defer.append((eng, fn, list(reads), list(writes), True, k))
            return None
        return self._add(eng, fn, reads, writes, True, k)

    def begin(self):
        assert self.defer is None
        self.defer = []

    def end(self):
        lst = self.defer
        self.defer = None
        return lst

    def merge(self, streams):
        streams = [s_ for s_ in streams if s_]
        pos = [0] * len(streams)
        total = sum(len(s_) for s_ in streams)
        for _ in range(total):
            best = None
            for i, s_ in enumerate(streams):
                if pos[i] < len(s_):
                    frac = pos[i] / float(len(s_))
                    if best is None or frac < best[0]:
                        best = (frac, i)
            i = best[1]
            self._add(*streams[i][pos[i]])
            pos[i] += 1

    def schedule(self):
        ops = self.ops
        cur = {e: {} for e in ENGS}
        for o in ops:
            E = o.eng
            clk = cur[E]
            need = {}
            dwaits = []
            for d in o.deps:
                Dp = ops[d]
                if Dp.is_dma:
                    if clk.get(("s", Dp.sem), 0) >= Dp.dval:
                        continue
                    dwaits.append(Dp)
                else:
                    if Dp.eng == "pe" and E == "pe" and not o.is_dma:
                        continue
                    if clk.get(Dp.eng, -1) >= d:
                        continue
                    if need.get(Dp.eng, -1) < d:
                        need[Dp.eng] = d
            if need or dwaits:
                new = dict(clk)
                targets = [ops[d] for d in need.values()] + dwaits
                for Dp in targets:
                    for kk, vv in Dp.snap.items():
                        if new.get(kk, -1) < vv:
                            new[kk] = vv
                    if Dp.is_dma:
                        kk = ("s", Dp.sem)
                        if new.get(kk, 0) < Dp.dval:
                            new[kk] = Dp.dval
                    else:
                        if new.get(Dp.eng, -1) < Dp.idx:
                            new[Dp.eng] = Dp.idx
                        Dp.signal = True
                    o.waits.append(Dp)
                cur[E] = new
            o.snap = cur[E]
        cnt = {e: 0 for e in ENGS}
        for o in ops:
            if not o.is_dma and o.signal:
                cnt[o.eng] += 1
                o.semval = cnt[o.eng]

    def emit(self, stack):
        nc = self.nc
        self.schedule()
        esem = {e: stack.enter_context(nc.semaphore("es_" + e)) for e in ENGS}
        dsem = {k: stack.enter_context(nc.semaphore(n)) for k, n in self.dma_sems.items()}
        block = stack.enter_context(nc.Block())
        by_eng = {e: [] for e in ENGS}
        for o in self.ops:
            by_eng[o.eng].append(o)

        def run(e, name):
            for o in by_eng[name]:
                for Dp in o.waits:
                    if Dp.is_dma:
                        e.wait_ge(dsem[Dp.sem], Dp.dval)
                    else:
                        e.wait_ge(esem[Dp.eng], Dp.semval)
                if o.fn is None:
                    continue
                if o.is_dma:
                    o.fn(e, dsem[o.sem])
                else:
                    ins = o.fn(e)
                    if o.signal:
                        ins.then_inc(esem[name], 1)

        @block.tensor
        def _(e):
            run(e, "pe")

        @block.scalar
        def _(e):
            run(e, "act")

        @block.vector
        def _(e):
            run(e, "dve")

        @block.gpsimd
        def _(e):
            run(e, "pool")

        @block.sync
        def _(e):
            run(e, "sp")


class Arena:
    def __init__(self, nc, st, nbytes):
        self.nbytes = nbytes
        self.t = st.enter_context(nc.sbuf_tensor("arena", [128, nbytes // 4], F32))
        self.bufs = []

    def view(self, name, off, shape, dt, parts=128):
        n = 1
        for s_ in shape:
            n *= s_
        esz = 4 if dt in (F32, I32) else 2
        nb = n * esz
        assert off % 4 == 0 and nb % 4 == 0, (name, off, nb)
        assert off + nb <= self.nbytes, (name, off, nb, self.nbytes)
        ap = self.t[0:parts, off // 4:(off + nb) // 4]
        if dt != F32:
            ap = ap.bitcast(dt)
        if len(shape) == 2:
            ap = ap.rearrange("p (a b) -> p a b", a=shape[0])
        elif len(shape) == 3:
            ap = ap.rearrange("p (a b c) -> p a b c", a=shape[0], b=shape[1])
        b = Buf(name, off, off + nb)
        for o in self.bufs:
            if o.lo < b.hi and b.lo < o.hi:
                o.alias.append(b)
                b.alias.append(o)
        self.bufs.append(b)
        return ap, b


class Bump:
    def __init__(self, arena, lo, hi):
        self.a = arena
        self.lo = lo
        self.hi = hi
        self.p = lo

    def reset(self):
        self.p = self.lo

    def at(self, name, buf, shape, dt, parts=128, off=0):
        return self.a.view(name, buf.lo + off, shape, dt, parts)

    def get(self, name, shape, dt, parts=128):
        n = 1
        for s_ in shape:
            n *= s_
        nb = n * (4 if dt in (F32, I32) else 2)
        nb = (nb + 31) // 32 * 32
        off = self.p
        assert off + nb <= self.hi, ("scratch overflow", name, off, nb, self.hi)
        self.p += nb
        return self.a.view(name, off, shape, dt, parts)


def f_act(out, in_, func, scale=None, bias=None, accum=None):
    kw = {}
    if scale is not None:
        kw["scale"] = scale
    if bias is not None:
        kw["bias"] = bias
    if accum is not None:
        kw["accum_out"] = accum
    return lambda e: e.activation(out=out, in_=in_, func=func, **kw)


def f_ts(out, in0, s1, op0, s2=None, op1=None):
    if op1 is None:
        return lambda e: e.tensor_scalar(out=out, in0=in0, scalar1=s1, scalar2=None, op0=op0)
    return lambda e: e.tensor_scalar(out=out, in0=in0, scalar1=s1, scalar2=s2, op0=op0, op1=op1)


def f_tt(out, in0, in1, op):
    return lambda e: e.tensor_tensor(out=out, in0=in0, in1=in1, op=op)


def f_stt(out, in0, scalar, in1, op0, op1):
    return lambda e: e.scalar_tensor_tensor(out=out, in0=in0, scalar=scalar, in1=in1, op0=op0, op1=op1)


def f_cp(out, in_):
    return lambda e: e.tensor_copy(out=out, in_=in_)


def f_red(out, in_, op=None):
    return lambda e: e.tensor_reduce(out=out, in_=in_, axis=AX.X, op=(op or ALU.add))


def f_recip(out, in_):
    return lambda e: e.reciprocal(out=out, in_=in_)


def f_memset(ap, val):
    return lambda e: e.memset(ap, val)


def f_mm(groups):
    def fn(e):
        ins = None
        for out, pairs in groups:
            n = len(pairs)
            for i, (l, r) in enumerate(pairs):
                ins = e.matmul(out, lhsT=l, rhs=r, start=(i == 0), stop=(i == n - 1))
        return ins
    return fn


def f_tr(items, ident):
    def fn(e):
        ins = None
        for out, in_ in items:
            ins = e.transpose(out=out, in_=in_, identity=ident)
        return ins
    return fn


WNAMES = ["norm_mix_g", "w_in", "conv_w", "conv_b", "wq_m", "wk_m", "b_igate", "b_fgate",
          "m_out_g", "cq_norm_g", "ckv_norm_g", "w_uq", "w_ukv", "qk_norm_q", "qk_norm_k",
          "a_out_g", "w_out", "norm_x_g", "norm_mem_g", "wq_x", "wkv_x", "xq_norm_g",
          "xk_norm_g", "wo_x", "norm_ffn_g", "w_ff1", "w_ff2"]
WSHAPES = {
    "norm_mix_g": [2, 1024], "w_in": [2, 1024, 1992], "conv_w": [2, 4, 512], "conv_b": [2, 512],
    "wq_m": [2, 4, 128, 64], "wk_m": [2, 4, 128, 64], "b_igate": [2, 4], "b_fgate": [2, 4],
    "m_out_g": [2, 4, 128], "cq_norm_g": [2, 256], "ckv_norm_g": [2, 128], "w_uq": [2, 256, 768],
    "w_ukv": [2, 128, 1024], "qk_norm_q": [2, 192], "qk_norm_k": [2, 192], "a_out_g": [2, 4, 128],
    "w_out": [2, 1024, 1024], "norm_x_g": [2, 1024], "norm_mem_g": [2, 1024], "wq_x": [2, 1024, 512],
    "wkv_x": [2, 1024, 1024], "xq_norm_g": [2, 128], "xk_norm_g": [2, 128], "wo_x": [2, 512, 1024],
    "norm_ffn_g": [2, 1024], "w_ff1": [2, 1024, 4096], "w_ff2": [2, 4096, 1024],
}


def build_program(nseq=SEQ_PER_CORE, depth=DEPTH, do_mlstm=True, do_mla=True, do_xattn=True,
                  do_ffn=True):
    nc = bass.Bass("TRN2", target_bir_lowering=False)
    dr = {}
    dr["x"] = nc.dram_tensor("x", [nseq, S, D], F32, kind="ExternalInput").ap()
    dr["mem"] = nc.dram_tensor("mem", [nseq, NMEM, D], F32, kind="ExternalInput").ap()
    dr["pos"] = nc.dram_tensor("pos", [nseq, S], I32, kind="ExternalInput").ap()
    for n in WNAMES:
        dr[n] = nc.dram_tensor(n, WSHAPES[n], F32, kind="ExternalInput").ap()
    dr["c_ident"] = nc.dram_tensor("c_ident", [128, 128], F32, kind="ExternalInput").ap()
    dr["c_triu"] = nc.dram_tensor("c_triu", [128, 128], F32, kind="ExternalInput").ap()
    dr["c_trisl"] = nc.dram_tensor("c_trisl", [128, 128], F32, kind="ExternalInput").ap()
    dr["c_mneg"] = nc.dram_tensor("c_mneg", [128, 128], F32, kind="ExternalInput").ap()
    dr["c_invf"] = nc.dram_tensor("c_invf", [128, 32], F32, kind="ExternalInput").ap()
    out_d = nc.dram_tensor("out", [nseq, S, D], F32, kind="ExternalOutput").ap()

    st = ExitStack()
    with st:
        P = Prog(nc)
        TOTAL = 212800
        A = Arena(nc, st, TOTAL)
        psum = st.enter_context(nc.psum_tensor("ps", [128, 8, 512], F32))
        PB = [Buf("psb%d" % i, excl=True) for i in range(8)]

        off = 0
        X, _ = A.view("X", off, (NT, D), F32)
        XB = []
        for t in range(NT):
            _, b = A.view("X%d" % t, off + t * D * 4, (D,), F32)
            XB.append(b)
        off += NT * D * 4
        R1_LO = off
        R1_SZ = 61568
        off += R1_SZ
        W_LO = off
        W_SZ = 49152
        off += W_SZ
        C_LO = off
        C_SZ = 9216
        off += C_SZ
        S_LO = off
        S_HI = TOTAL
        CB = Bump(A, C_LO, C_LO + C_SZ)
        SB = Bump(A, S_LO, S_HI)
        RB = Bump(A, R1_LO, R1_LO + R1_SZ)

        identf, b_identf = CB.get("identf", (128,), F32)
        identb, b_identb = CB.get("identb", (128,), BF16)
        triu, b_triu = CB.get("triu", (128,), F32)
        trisl, b_trisl = CB.get("trisl", (128,), F32)
        mneg, b_mneg = CB.get("mneg", (128,), F32)
        m01b, b_m01b = CB.get("m01b", (128,), BF16)
        onesb, b_onesb = CB.get("onesb", (128,), BF16)
        onesf, b_onesf = CB.get("onesf", (128,), F32)
        invf, b_invf = CB.get("invf", (32,), F32)
        gcol, b_gcol = CB.get("gcol", (DEPTH, 4, 8), F32)
        convw, b_convw = CB.get("convw", (DEPTH, 4, 4), F32)
        convb, b_convb = CB.get("convb", (DEPTH, 4), F32)
        mog, b_mog = CB.get("mog", (DEPTH, 4), F32)
        aog, b_aog = CB.get("aog", (DEPTH, 4), F32)
        cqg, b_cqg = CB.get("cqg", (DEPTH, 2), F32)
        misc, b_misc = CB.get("misc", (DEPTH, 8), F32)
        gqr, b_gqr = CB.get("gqr", (DEPTH, 64), F32)
        gkr, b_gkr = CB.get("gkr", (DEPTH, 64), F32)
        bif, b_bif = CB.get("bif", (DEPTH, 8), F32)
        cosb, b_cos = CB.get("cos", (NT, 32), F32)
        sinb, b_sin = CB.get("sin", (NT, 32), F32)
        b_const = [b_identf, b_identb, b_triu, b_trisl, b_mneg, b_m01b, b_onesb, b_onesf, b_invf]

        def bc_rows(src_ap_1d, n):
            return src_ap_1d.unsqueeze(0).to_broadcast([128, n])

        def ld_consts(e, s):
            e.dma_start(out=identf, in_=dr["c_ident"]).then_inc(s, 16)
            e.dma_start(out=triu, in_=dr["c_triu"]).then_inc(s, 16)
            e.dma_start(out=trisl, in_=dr["c_trisl"]).then_inc(s, 16)
            e.dma_start(out=mneg, in_=dr["c_mneg"]).then_inc(s, 16)
            e.dma_start(out=invf, in_=dr["c_invf"]).then_inc(s, 16)
        b_cgrp = Buf("cgrp")
        P.dma("sp", ld_consts, writes=[b_cgrp, b_identf, b_triu, b_trisl, b_mneg, b_invf], k=5)
        P.op("dve", f_cp(identb, identf), reads=[b_identf], writes=[b_identb])
        P.op("dve", f_cp(m01b, triu), reads=[b_triu], writes=[b_m01b])
        P.op("pool", f_memset(onesb, 1.0), writes=[b_onesb])
        P.op("pool", f_memset(onesf, 1.0), writes=[b_onesf])

        b_gains = [b_gcol, b_convw, b_convb, b_mog, b_aog, b_cqg, b_misc, b_gqr, b_gkr, b_bif]

        gls = []
        for l in range(DEPTH):
            gl = []
            gls.append(gl)
            for wi, nm in enumerate(["norm_mix_g", "norm_x_g", "norm_mem_g", "norm_ffn_g"]):
                gl.append((gcol[:, l, wi, :], dr[nm][l].rearrange("(k p) -> p k", p=128)))
            for j in range(4):
                gl.append((convw[:, l, :, j], dr["conv_w"][l, j].rearrange("(c p) -> p c", p=128)))
            gl.append((convb[:, l, :], dr["conv_b"][l].rearrange("(c p) -> p c", p=128)))
            gl.append((mog[:, l, :], dr["m_out_g"][l].rearrange("h p -> p h")))
            gl.append((aog[:, l, :], dr["a_out_g"][l].rearrange("h p -> p h")))
            gl.append((cqg[:, l, :], dr["cq_norm_g"][l].rearrange("(k p) -> p k", p=128)))
            gl.append((misc[:, l, 0:1], dr["ckv_norm_g"][l].rearrange("(k p) -> p k", p=128)))
            gl.append((misc[:, l, 1:2], dr["qk_norm_q"][l, 0:128].rearrange("(k p) -> p k", p=128)))
            gl.append((misc[:, l, 2:3], dr["qk_norm_k"][l, 0:128].rearrange("(k p) -> p k", p=128)))
            gl.append((misc[:, l, 3:4], dr["xq_norm_g"][l].rearrange("(k p) -> p k", p=128)))
            gl.append((misc[:, l, 4:5], dr["xk_norm_g"][l].rearrange("(k p) -> p k", p=128)))
            gl.append((gqr[:, l, :], bc_rows(dr["qk_norm_q"][l, 128:192], 64)))
            gl.append((gkr[:, l, :], bc_rows(dr["qk_norm_k"][l, 128:192], 64)))
            gl.append((bif[:, l, 0:4], bc_rows(dr["b_igate"][l], 4)))
            gl.append((bif[:, l, 4:8], bc_rows(dr["b_fgate"][l], 4)))

        def mk_ld_gains(gl_):
            def ld_gains(e, s):
                with nc.allow_non_contiguous_dma(reason="tiny per-layer gain vectors"):
                    for (o_, i_) in gl_:
                        e.dma_start(out=o_, in_=i_).then_inc(s, 16)
            return ld_gains
        ggrps = [Buf("ggrp%d" % l) for l in range(DEPTH)]
        b_ggrp = ggrps[0]
        P.dma("sp", mk_ld_gains(gls[0]), writes=[ggrps[0]], k=len(gls[0]))

        class PSM:
            def __init__(self, banks=None):
                self.free = list(range(8)) if banks is None else list(banks)

            def get(self, n=1):
                if n == 1:
                    for b in self.free:
                        if (b ^ 1) not in self.free:
                            self.free.remove(b)
                            return [b]
                    b = self.free.pop(0)
                    return [b]
                for i, b in enumerate(self.free):
                    if b % 2 == 0 and (b + 1) in self.free:
                        self.free.remove(b)
                        self.free.remove(b + 1)
                        return [b, b + 1]
                raise RuntimeError("no psum pair free: %s" % self.free)

            def rel(self, banks):
                self.free.extend(banks)
        PS = PSM()

        def pbank(b):
            return psum[:, b, :]

        def pbank_bf(b):
            return psum[:, b, :].bitcast(BF16)

        Win, b_Win = A.view("Win", W_LO, (8, IN_COLS), BF16)
        wuq, b_wuq = A.view("wuq", W_LO + 31872, (2, 768), BF16)
        wukv, b_wukv = A.view("wukv", W_LO + 31872 + 3072, (1024,), BF16)
        wqk, b_wqk = A.view("wqk", W_LO + 31872 + 5120, (4, 128), BF16)
        wout, b_wout = A.view("wout", W_LO + 38016, (4, 1024), BF16)
        wqx, b_wqx = A.view("wqx", W_LO, (8, 512), BF16)
        wkvx, b_wkvx = A.view("wkvx", W_LO + 8192, (8, 1024), BF16)
        wox, b_wox = A.view("wox", W_LO + 24576, (4, 1024), BF16)
        fslots = []
        for i, o_ in enumerate([32768, 0, 16384]):
            w1s, b1 = A.view("w1s%d" % i, W_LO + o_, (8, FSL), BF16)
            w2s, b2 = A.view("w2s%d" % i, W_LO + o_ + 8192, (FSL // 128, 1024), BF16)
            fslots.append((w1s, b1, w2s, b2))

        def rstd_from_ms(stv, b_st, src_col, dst_col, n=1):
            P.op("act", f_act(stv[:, dst_col:dst_col + n], stv[:, src_col:src_col + n], AF.Ln, bias=EPS),
                 reads=[b_st], writes=[b_st])
            P.op("act", f_act(stv[:, dst_col:dst_col + n], stv[:, dst_col:dst_col + n], AF.Exp, scale=-0.5),
                 reads=[b_st], writes=[b_st])

        def norm_to_hT(xin_ap, b_xin, gcols, hT_ap, b_hT, scr):
            xnb, b_xnb, stv, b_st = scr
            P.op("act", f_act(xnb, xin_ap, AF.Square, scale=float(D) ** -0.5, accum=stv[:, 0:1]),
                 reads=[b_xin], writes=[b_xnb, b_st])
            rstd_from_ms(stv, b_st, 0, 1)
            P.op("dve", f_ts(xnb, xin_ap, stv[:, 1:2], ALU.mult), reads=[b_xin, b_st], writes=[b_xnb])
            bk = PS.get()
            pT = pbank_bf(bk[0]).rearrange("p (a b) -> p a b", a=8)
            P.op("pe", f_tr([(pT[:, k, :], xnb[:, k * 128:(k + 1) * 128]) for k in range(8)], identb),
                 reads=[b_xnb, b_identb], writes=[PB[bk[0]]])
            P.op("dve", f_tt(hT_ap, pT, gcols.unsqueeze(2).to_broadcast([128, 8, 128]), ALU.mult),
                 reads=[PB[bk[0]], b_ggrp], writes=[b_hT])
            PS.rel(bk)

        def load_w(eng, dst, b_dst, src, k=1):
            P.dma(eng, lambda e, s: e.dma_start(out=dst, in_=src).then_inc(s, 16), writes=[b_dst])

        def resid_add(T, pY2, banks):
            P.op("dve", f_tt(X[:, T, :], X[:, T, :], pY2, ALU.add),
                 reads=[XB[T], PB[banks[0]], PB[banks[1]]], writes=[XB[T]])

        QSC = 192.0 ** -0.5
        XSC = 128.0 ** -0.5
        fslice_ctr = [0]

        def bc3(ap2, shape):
            return ap2.unsqueeze(2).to_broadcast(shape)

        for sq in range(nseq):
            for T in range(NT):
                P.dma("sp", (lambda e, s, T=T, sq=sq: e.dma_start(out=X[:, T, :], in_=dr["x"][sq, T * 128:(T + 1) * 128, :]).then_inc(s, 16)),
                      writes=[XB[T]])
            if sq == 0:
                for l_ in range(1, DEPTH):
                    P.dma("sp", mk_ld_gains(gls[l_]), writes=[ggrps[l_]], k=len(gls[l_]))
            if do_mla:
                SB.reset()
                posi, b_posi = SB.get("posi", (NT,), I32)
                posf, b_posf = SB.get("posf", (NT,), F32)
                ang, b_ang = SB.get("ang", (NT, 32), F32)
                angi, b_angi = SB.get("angi", (NT, 32), I32)
                angf, b_angf = SB.get("angf", (NT, 32), F32)
                tmpc, b_tmpc = SB.get("tmpc", (NT, 32), F32)

                def ld_pos(e, s, sq=sq):
                    with nc.allow_non_contiguous_dma(reason="positions to token-major columns"):
                        e.dma_start(out=posi, in_=dr["pos"][sq].rearrange("(t p) -> p t", p=128)).then_inc(s, 16)
                P.dma("sp", ld_pos, writes=[b_posi])
                P.op("dve", f_cp(posf, posi), reads=[b_posi], writes=[b_posf])
                P.op("dve", f_tt(ang, posf.unsqueeze(2).to_broadcast([128, NT, 32]),
                                 invf.unsqueeze(1).to_broadcast([128, NT, 32]), ALU.mult),
                     reads=[b_posf, b_invf, b_cgrp], writes=[b_ang])
                for (dst, b_dst, shift) in [(sinb, b_sin, 0.0), (cosb, b_cos, 0.25)]:
                    if shift != 0.0:
                        P.op("dve", f_ts(tmpc, ang, shift, ALU.add), reads=[b_ang], writes=[b_tmpc])
                        src, b_src = tmpc, b_tmpc
                    else:
                        src, b_src = ang, b_ang
                    P.op("dve", f_cp(angi, src), reads=[b_src], writes=[b_angi])
                    P.op("dve", f_cp(angf, angi), reads=[b_angi], writes=[b_angf])
                    P.op("dve", f_tt(angf, src, angf, ALU.subtract), reads=[b_src, b_angf], writes=[b_angf])
                    P.op("dve", f_ts(angi.bitcast(F32), angf, 0.5, ALU.is_gt), reads=[b_angf], writes=[b_angi])
                    P.op("dve", f_tt(angf, angf, angi.bitcast(F32), ALU.subtract), reads=[b_angf, b_angi], writes=[b_angf])
                    P.op("dve", f_ts(angi.bitcast(F32), angf, -0.5, ALU.is_lt), reads=[b_angf], writes=[b_angi])
                    P.op("dve", f_tt(angf, angf, angi.bitcast(F32), ALU.add), reads=[b_angf, b_angi], writes=[b_angf])
                    P.op("act", f_act(dst, angf, AF.Sin, scale=6.28318), reads=[b_angf], writes=[b_dst])

            for l in range(depth):
                b_ggrp = ggrps[l]
                if do_mlstm or do_mla:
                    load_w("pool", Win, b_Win, dr["w_in"][l].rearrange("(k p) n -> p k n", p=128))
                    load_w("pool", wuq, b_wuq, dr["w_uq"][l].rearrange("(k p) n -> p k n", p=128))
                    load_w("pool", wukv, b_wukv, dr["w_ukv"][l])
                    load_w("pool", wqk[:, :, 0:64], b_wqk, dr["wq_m"][l].rearrange("h d e -> d h e"))
                    load_w("pool", wqk[:, :, 64:128], b_wqk, dr["wk_m"][l].rearrange("h d e -> d h e"))
                    load_w("pool", wout, b_wout, dr["w_out"][l, 0:512, :].rearrange("(k p) n -> p k n", p=128))

                    RB.reset()
                    qnT, b_qnT = RB.get("qnT", (4, S), BF16)
                    qrT, b_qrT = RB.get("qrT", (2, S), BF16)
                    knT, b_knT = RB.get("knT", (4, S), BF16)
                    krT, b_krT = RB.get("krT", (S,), BF16)
                    va, b_va = RB.get("va", (NT, 4, 129), BF16)
                    SB.reset()
                    xnb, b_xnb = SB.get("xnb", (D,), BF16)
                    stv, b_st = SB.get("st", (32,), F32)
                    hT, b_hT = SB.get("hT", (8, 128), BF16)
                    sm, b_sm = SB.get("sm", (456,), F32)
                    ust, b_ust = SB.get("ust", (4, 131), F32)
                    Cst, b_Cst = SB.get("Cst", (4, 129), F32, parts=64)
                    Cbf, b_Cbf = SB.get("Cbf", (4, 129), BF16, parts=64)
                    g8, b_g8 = SB.get("g8", (64,), F32)
                    vaug, b_vaug = SB.get("vaug", (4, 129), BF16)
                    TA, b_TA = SB.get("TA", (4, 128), F32)
                    TB, b_TB = SB.get("TB", (4, 128), F32)
                    uc, b_uc = SB.get("uc", (4, 128), BF16)
                    og, b_og = SB.get("og", (512,), BF16)
                    qTs, b_qTs = SB.get("qTs", (4, 128), BF16, parts=64)
                    kTs, b_kTs = SB.get("kTs", (4, 128), BF16, parts=64)
                    PT, b_PT = SB.get("PT", (4, 128), BF16)
                    qtil, b_qtil = SB.at("qtil", b_kTs, (4, 128), BF16, parts=64)
                    ktil, b_ktil = SB.at("ktil", b_qTs, (4, 64), BF16)
                    yms = [A.view("ym%d" % i, W_LO + 46208 + i * 1024, (4, 128), BF16) for i in range(2)]
                    junk, b_junk = SB.at("junk", b_qTs, (128,), BF16, off=512)
                    cqn, b_cqn = SB.get("cqn", (256,), BF16)
                    ckvn, b_ckvn = SB.get("ckvn", (128,), BF16)
                    krn, b_krn = SB.get("krn", (64,), F32)
                    kt4, b_kt4 = SB.get("kt4", (4, 32), F32)
                    krot, b_krot = SB.at("krot", b_krn, (2, 64), BF16)
                    cqnT, b_cqnT = SB.at("cqnT", b_cqn, (2, 128), BF16)
                    ckvnT, b_ckvnT = SB.at("ckvnT", b_ckvn, (128,), BF16)
                    stq, b_stq = SB.get("stq", (32,), F32)
                    qr, b_qr = SB.get("qr", (4, 64), F32)
                    qt4, b_qt4 = SB.get("qt4", (2, 4, 32), F32)
                    qrot, b_qrot = SB.get("qrot", (4, 64), BF16)
                    junkk, b_junkk = SB.get("junkk", (128,), BF16)
                    qn, b_qn = SB.at("qn", b_xnb, (4, 128), BF16)
                    kn, b_kn = SB.at("kn", b_xnb, (4, 128), BF16, off=1024)
                    ymT, b_ymT = SB.at("ymT", b_qr, (4, 128), BF16)
                    PS_all = PS
                    PS_M = PSM([0, 1, 2, 3, 4])
                    PS_M1 = PSM([0, 1, 2])
                    PS_M2 = PSM([3, 4])
                    PS_K = PSM([5, 6, 7])

                    def mix(streams):
                        streams = [s_ for s_ in streams if s_]
                        pos = [0] * len(streams)
                        outl = []
                        for _ in range(sum(len(s_) for s_ in streams)):
                            best = None
                            for i, s_ in enumerate(streams):
                                if pos[i] < len(s_):
                                    fr = pos[i] / float(len(s_))
                                    if best is None or fr < best[0]:
                                        best = (fr, i)
                            outl.append(streams[best[1]][pos[best[1]]])
                            pos[best[1]] += 1
                        return outl

                    def emit_Y(Tp):
                        ymp, b_ymp = yms[Tp % 2]
                        bT = PS.get()
                        pT = pbank_bf(bT[0])[:, 0:512].rearrange("p (a b) -> p a b", a=4)
                        P.op("pe", f_tr([(pT[:, h, :], ymp[:, h, :]) for h in range(4)], identb),
                             reads=[b_ymp, b_identb], writes=[PB[bT[0]]])
                        P.op("dve", f_tt(ymT, pT, bc3(mog[:, l, :], [128, 4, 128]), ALU.mult),
                             reads=[PB[bT[0]], b_ggrp], writes=[b_ymT])
                        for hf in range(2):
                            P.op("pe", f_mm([(pbank(bT[0]), [(ymT[:, h, :], wout[:, h, hf * 512:(hf + 1) * 512]) for h in range(4)])]),
                                 reads=[b_ymT, b_wout], writes=[PB[bT[0]]])
                            P.op("dve", f_tt(X[:, Tp, hf * 512:(hf + 1) * 512], X[:, Tp, hf * 512:(hf + 1) * 512], pbank(bT[0]), ALU.add),
                                 reads=[XB[Tp], PB[bT[0]]], writes=[XB[Tp]])
                        PS.rel(bT)

                    if do_mlstm:
                        P.op("pool", f_memset(vaug[:, :, 128:129], 1.0), writes=[b_vaug])
                    if do_mla:
                        P.op("pool", f_memset(va[:, :, :, 128:129], 1.0), writes=[b_va])

                    for T in range(NT):
                        tc_ = slice(T * 128, (T + 1) * 128)
                        PS = PS_M
                        norm_to_hT(X[:, T, :], XB[T], gcol[:, l, 0, :], hT, b_hT, (xnb, b_xnb, stv, b_st))
                        bk = PS.get()
                        P.op("pe", f_mm([(psum[:, bk[0], 0:456], [(hT[:, k, :], Win[:, k, 1536:1992]) for k in range(8)])]),
                             reads=[b_hT, b_Win], writes=[PB[bk[0]]])
                        P.op("act", f_act(sm, psum[:, bk[0], 0:456], AF.Copy), reads=[PB[bk[0]]], writes=[b_sm])
                        PS.rel(bk)
                        strM1 = strM2 = strJ = []
                        if do_mlstm:
                            P.begin()
                            PS = PS_M1
                            bU = PS.get()
                            pU = pbank(bU[0]).rearrange("p (c t) -> p c t", c=4)
                            P.op("pe", f_mm([(pU[:, c, :], [(Win[:, k, c * 128:(c + 1) * 128], hT[:, k, :]) for k in range(8)])
                                             for c in range(4)]),
                                 reads=[b_hT, b_Win], writes=[PB[bU[0]]])
                            if T == 0:
                                P.op("pool", f_memset(ust[:, :, 0:3], 0.0), writes=[b_ust])
                            else:
                                P.op("dve", f_cp(ust[:, :, 0:3], ust[:, :, 128:131]), reads=[b_ust], writes=[b_ust])
                            P.op("act", f_act(ust[:, :, 3:131], pU, AF.Copy), reads=[PB[bU[0]]], writes=[b_ust])
                            for c in range(4):
                                P.op("dve", f_ts(TA[:, c, :], ust[:, c, 3:131], convw[:, l, c, 3:4], ALU.mult,
                                                 convb[:, l, c:c + 1], ALU.add),
                                     reads=[b_ust, b_ggrp], writes=[b_TA])
                                for j in range(3):
                                    P.op("dve", f_stt(TA[:, c, :], ust[:, c, j:j + 128], convw[:, l, c, j:j + 1], TA[:, c, :],
                                                      ALU.mult, ALU.add),
                                         reads=[b_ust, b_ggrp, b_TA], writes=[b_TA])
                            P.op("act", f_act(pU, TA, AF.Exp, scale=-1.0), reads=[b_TA], writes=[PB[bU[0]]])
                            P.op("act", f_act(pU, pU, AF.Ln, bias=1.0), reads=[PB[bU[0]]], writes=[PB[bU[0]]])
                            P.op("act", f_act(pU, pU, AF.Exp, scale=-1.0), reads=[PB[bU[0]]], writes=[PB[bU[0]]])
                            P.op("dve", f_tt(uc, TA, pU, ALU.mult), reads=[b_TA, PB[bU[0]]], writes=[b_uc])
                            PS.rel(bU)
                            bQ = PS.get()
                            bK = PS.get()
                            bKt = PS.get()
                            pQ = psum[0:64, bQ[0], :].rearrange("p (h t) -> p h t", h=4)
                            pK = psum[0:64, bK[0], :].rearrange("p (h t) -> p h t", h=4)
                            pKt = psum[:, bKt[0], 0:256].rearrange("p (h d) -> p h d", h=4)
                            P.op("pe", f_mm([(pQ[:, h, :], [(wqk[:, h, 0:64], uc[:, h, :])]) for h in range(4)]),
                                 reads=[b_wqk, b_uc], writes=[PB[bQ[0]]])
                            P.op("pe", f_mm([(pK[:, h, :], [(wqk[:, h, 64:128], uc[:, h, :])]) for h in range(4)]),
                                 reads=[b_wqk, b_uc], writes=[PB[bK[0]]])
                            P.op("pe", f_mm([(pKt[:, h, :], [(uc[:, h, :], wqk[:, h, 64:128])]) for h in range(4)]),
                                 reads=[b_wqk, b_uc], writes=[PB[bKt[0]]])
                            P.op("act", f_act(qTs, pQ, AF.Identity, scale=0.125), reads=[PB[bQ[0]]], writes=[b_qTs])
                            P.op("dve", f_cp(kTs, pK), reads=[PB[bK[0]]], writes=[b_kTs])
                            PS.rel(bK)
                            bS = PS.get()
                            pS2 = pbank(bS[0]).rearrange("p (h t) -> p h t", h=4)
                            P.op("pe", f_mm([(pS2[:, h, :], [(kTs[:, h, :], qTs[:, h, :])]) for h in range(4)]),
                                 reads=[b_kTs, b_qTs], writes=[PB[bS[0]]])
                            strM1 = P.end()
                            P.begin()
                            PS = PS_M2
                            gi = g8[:, 0:4]
                            lf = g8[:, 16:20]
                            P.op("dve", f_tt(g8[:, 0:8], sm[:, 0:8], bif[:, l, :], ALU.add), reads=[b_sm, b_ggrp], writes=[b_g8])
                            P.op("act", f_act(g8[:, 8:12], g8[:, 4:8], AF.Exp, scale=-1.0), reads=[b_g8], writes=[b_g8])
                            P.op("act", f_act(g8[:, 12:16], g8[:, 8:12], AF.Ln, bias=1.0), reads=[b_g8], writes=[b_g8])
                            P.op("dve", f_ts(lf, g8[:, 12:16], -1.0, ALU.mult), reads=[b_g8], writes=[b_g8])
                            P.op("dve", f_tt(TB, triu.unsqueeze(1).to_broadcast([128, 4, 128]), bc3(lf, [128, 4, 128]), ALU.mult),
                                 reads=[b_triu, b_g8], writes=[b_TB])
                            bB = PS.get()
                            pB = pbank(bB[0])
                            P.op("pe", f_mm([(pB, [(onesf, TB.rearrange("p a b -> p (a b)"))])]),
                                 reads=[b_onesf, b_TB], writes=[PB[bB[0]]])
                            bC = PS.get()
                            pC = pbank(bC[0])
                            P.op("pe", f_mm([(pC[:, 0:4], [(triu, lf)]), (pC[:, 4:8], [(trisl, lf)])]),
                                 reads=[b_triu, b_trisl, b_g8], writes=[PB[bC[0]]])
                            acol = g8[:, 20:24]
                            wa = g8[:, 28:32]
                            P.op("dve", f_tt(acol, gi, pC[:, 0:4], ALU.subtract), reads=[b_g8, PB[bC[0]]], writes=[b_g8])
                            P.op("dve", f_tt(g8[:, 24:28], gi, pC[:, 4:8], ALU.add), reads=[b_g8, PB[bC[0]]], writes=[b_g8])
                            PS.rel(bC)
                            P.op("act", f_act(wa, g8[:, 24:28], AF.Exp), reads=[b_g8], writes=[b_g8])
                            bk = PS.get()
                            P.op("pe", f_mm([(pbank(bk[0]), [(hT[:, k, :], Win[:, k, 512:1024]) for k in range(8)])]),
                                 reads=[b_hT, b_Win], writes=[PB[bk[0]]])
                            P.op("act", f_act(vaug[:, :, 0:128], pbank(bk[0]).rearrange("p (h e) -> p h e", h=4), AF.Copy),
                                 reads=[PB[bk[0]]], writes=[b_vaug])
                            PS.rel(bk)
                            bk = PS.get()
                            pO_ = pbank(bk[0])
                            P.op("pe", f_mm([(pO_, [(hT[:, k, :], Win[:, k, 1024:1536]) for k in range(8)])]),
                                 reads=[b_hT, b_Win], writes=[PB[bk[0]]])
                            P.op("act", f_act(pO_, pO_, AF.Exp, scale=-1.0), reads=[PB[bk[0]]], writes=[PB[bk[0]]])
                            P.op("act", f_act(pO_, pO_, AF.Ln, bias=1.0), reads=[PB[bk[0]]], writes=[PB[bk[0]]])
                            P.op("act", f_act(og, pO_, AF.Exp, scale=-1.0), reads=[PB[bk[0]]], writes=[b_og])
                            PS.rel(bk)
                            strM2 = P.end()
                            P.begin()
                            PS = PSM([0, 1, 2, 3, 4])
                            ymc, b_ymc = yms[T % 2]
                            P.op("dve", f_tt(ktil, pKt, bc3(wa, [128, 4, 64]), ALU.mult), reads=[PB[bKt[0]], b_g8], writes=[b_ktil])
                            pB3 = pB.rearrange("p (h t) -> p h t", h=4)
                            for h in range(4):
                                P.op("dve", f_stt(TA[:, h, :], pB3[:, h, :], acol[:, h:h + 1], mneg, ALU.add, ALU.add),
                                     reads=[PB[bB[0]], b_g8, b_mneg], writes=[b_TA])
                            P.op("act", f_act(TA, TA, AF.Exp), reads=[b_TA], writes=[b_TA])
                            P.op("dve", f_tt(PT, pS2, TA, ALU.mult), reads=[PB[bS[0]], b_TA], writes=[b_PT])
                            eB = TB[0:64]
                            P.op("act", f_act(eB, pB3[0:64], AF.Exp), reads=[PB[bB[0]]], writes=[b_TB])
                            P.op("dve", f_stt(qtil, pQ, 0.125, eB, ALU.mult, ALU.mult), reads=[PB[bQ[0]], b_TB], writes=[b_qtil])
                            bH = PS.get(2)

                            def Hh(h, bH=bH):
                                return psum[:, bH[0] + h // 2, (h % 2) * 129:(h % 2) * 129 + 129]
                            grp = []
                            for h in range(4):
                                pairs = []
                                if T > 0:
                                    pairs.append((qtil[:, h, :], Cbf[:, h, :]))
                                pairs.append((PT[:, h, :], vaug[:, h, :]))
                                grp.append((Hh(h), pairs))
                            P.op("pe", f_mm(grp), reads=[b_qtil, b_Cbf, b_PT, b_vaug], writes=[PB[bH[0]], PB[bH[1]]])
                            den = g8[:, 44:48]
                            den2 = den.rearrange("p (a b) -> p a b", b=2)
                            P.op("act", f_act(den2[:, :, 0], psum[:, bH[0]:bH[0] + 2, 128], AF.Abs), reads=[PB[bH[0]], PB[bH[1]]], writes=[b_g8])
                            P.op("act", f_act(den2[:, :, 1], psum[:, bH[0]:bH[0] + 2, 257], AF.Abs), reads=[PB[bH[0]], PB[bH[1]]], writes=[b_g8])
                            rr = g8[:, 48:52]
                            P.op("dve", f_ts(rr, den, 1.0, ALU.max), reads=[b_g8], writes=[b_g8])
                            P.op("dve", f_recip(rr, rr), reads=[b_g8], writes=[b_g8])
                            ssm = g8[:, 52:56]
                            for h in range(4):
                                P.op("act", f_act(junk, Hh(h)[:, 0:128], AF.Square, scale=rr[:, h:h + 1], accum=ssm[:, h:h + 1]),
                                     reads=[PB[bH[0]], PB[bH[1]], b_g8], writes=[b_junk, b_g8])
                            P.op("act", f_act(g8[:, 56:60], ssm, AF.Ln, scale=1.0 / 128.0, bias=EPS), reads=[b_g8], writes=[b_g8])
                            P.op("act", f_act(g8[:, 56:60], g8[:, 56:60], AF.Exp, scale=-0.5), reads=[b_g8], writes=[b_g8])
                            tot = g8[:, 60:64]
                            P.op("dve", f_tt(tot, rr, g8[:, 56:60], ALU.mult), reads=[b_g8], writes=[b_g8])
                            for h in range(4):
                                P.op("dve", f_stt(ymc[:, h, :], Hh(h)[:, 0:128], tot[:, h:h + 1], og[:, h * 128:(h + 1) * 128],
                                                  ALU.mult, ALU.mult),
                                     reads=[PB[bH[0]], PB[bH[1]], b_g8, b_og], writes=[b_ymc])
                            PS.rel(bH)
                            if T < NT - 1:
                                bL = PS.get(2)

                                def Cl(h, bL=bL):
                                    return psum[0:64, bL[0] + h // 2, (h % 2) * 129:(h % 2) * 129 + 129]
                                P.op("pe", f_mm([(Cl(h), [(ktil[:, h, :], vaug[:, h, :])]) for h in range(4)]),
                                     reads=[b_ktil, b_vaug], writes=[PB[bL[0]], PB[bL[1]]])
                                for h in range(4):
                                    if T == 0:
                                        P.op("dve", f_cp(Cst[:, h, :], Cl(h)), reads=[PB[bL[0]], PB[bL[1]]], writes=[b_Cst])
                                    else:
                                        P.op("dve", f_stt(Cst[:, h, :], Cst[:, h, :], eB[:, h, 127:128], Cl(h), ALU.mult, ALU.add),
                                             reads=[b_Cst, b_TB, PB[bL[0]], PB[bL[1]]], writes=[b_Cst])
                                PS.rel(bL)
                                P.op("act", f_act(Cbf, Cst, AF.Copy), reads=[b_Cst], writes=[b_Cbf])
                            strJ = P.end()
                            PS_M1.free = [0, 1, 2]
                            PS_M2.free = [3, 4]
                        strM = mix([strM1, strM2]) + strJ
                        PS = PS_K
                        P.begin()
                        if do_mlstm and T > 0:
                            emit_Y(T - 1)
                        if do_mla:
                            P.op("act", f_act(junkk, sm[:, 8:136], AF.Square, scale=256.0 ** -0.5, accum=stq[:, 0:1]),
                                 reads=[b_sm], writes=[b_junkk, b_stq])
                            P.op("act", f_act(junkk, sm[:, 136:264], AF.Square, scale=256.0 ** -0.5, accum=stq[:, 3:4]),
                                 reads=[b_sm], writes=[b_junkk, b_stq])
                            P.op("act", f_act(junkk, sm[:, 264:392], AF.Square, scale=128.0 ** -0.5, accum=stq[:, 1:2]),
                                 reads=[b_sm], writes=[b_junkk, b_stq])
                            P.op("act", f_act(junkk[:, 0:64], sm[:, 392:456], AF.Square, scale=64.0 ** -0.5, accum=stq[:, 2:3]),
                                 reads=[b_sm], writes=[b_junkk, b_stq])
                            P.op("dve", f_tt(stq[:, 0:1], stq[:, 0:1], stq[:, 3:4], ALU.add), reads=[b_stq], writes=[b_stq])
                            rstd_from_ms(stq, b_stq, 0, 4, n=3)
                            P.op("dve", f_ts(cqn, sm[:, 8:264], stq[:, 4:5], ALU.mult), reads=[b_sm, b_stq], writes=[b_cqn])
                            P.op("dve", f_ts(ckvn, sm[:, 264:392], stq[:, 5:6], ALU.mult), reads=[b_sm, b_stq], writes=[b_ckvn])
                            P.op("dve", f_stt(krn, sm[:, 392:456], stq[:, 6:7], gkr[:, l, :], ALU.mult, ALU.mult),
                                 reads=[b_sm, b_stq, b_ggrp], writes=[b_krn])
                            cT = cosb[:, T, :]
                            sT = sinb[:, T, :]
                            P.op("dve", f_tt(kt4[:, 0, :], krn[:, 0:32], cT, ALU.mult), reads=[b_krn, b_cos], writes=[b_kt4])
                            P.op("dve", f_tt(kt4[:, 1, :], krn[:, 32:64], sT, ALU.mult), reads=[b_krn, b_sin], writes=[b_kt4])
                            P.op("dve", f_tt(kt4[:, 2, :], krn[:, 32:64], cT, ALU.mult), reads=[b_krn, b_cos], writes=[b_kt4])
                            P.op("dve", f_tt(kt4[:, 3, :], krn[:, 0:32], sT, ALU.mult), reads=[b_krn, b_sin], writes=[b_kt4])
                            P.op("dve", f_tt(krot[:, 0, 0:32], kt4[:, 0, :], kt4[:, 1, :], ALU.subtract), reads=[b_kt4], writes=[b_krot])
                            P.op("dve", f_tt(krot[:, 0, 32:64], kt4[:, 2, :], kt4[:, 3, :], ALU.add), reads=[b_kt4], writes=[b_krot])
                            P.op("dve", f_cp(krot[:, 1, :], krot[:, 0, :]), reads=[b_krot], writes=[b_krot])
                            bT = PS.get()
                            pT = pbank_bf(bT[0]).rearrange("p (a b) -> p a b", a=8)
                            P.op("pe", f_tr([(pT[:, 0, :], cqn[:, 0:128]), (pT[:, 1, :], cqn[:, 128:256]),
                                             (pT[:, 2, :], ckvn), (pT[:, 3, :], krot.rearrange("p a b -> p (a b)"))], identb),
                                 reads=[b_cqn, b_ckvn, b_krot, b_identb], writes=[PB[bT[0]]])
                            P.op("dve", f_tt(cqnT, pT[:, 0:2, :], bc3(cqg[:, l, :], [128, 2, 128]), ALU.mult),
                                 reads=[PB[bT[0]], b_ggrp], writes=[b_cqnT])
                            P.op("dve", f_ts(ckvnT, pT[:, 2, :], misc[:, l, 0:1], ALU.mult), reads=[PB[bT[0]], b_ggrp], writes=[b_ckvnT])
                            P.op("act", f_act(krT[:, tc_], pT[:, 3, :], AF.Copy), reads=[PB[bT[0]]], writes=[b_krT])
                            PS.rel(bT)
                            bq = PS.get(2)
                            P.op("pe", f_mm([(psum[:, bq[0], :], [(cqnT[:, kc, :], wuq[:, kc, 0:512]) for kc in range(2)]),
                                             (psum[:, bq[1], 0:256], [(cqnT[:, kc, :], wuq[:, kc, 512:768]) for kc in range(2)])]),
                                 reads=[b_cqnT, b_wuq], writes=[PB[bq[0]], PB[bq[1]]])
                            qa2 = psum[:, bq[0]:bq[0] + 2, :].rearrange("p a b -> p (a b)")[:, 0:768]
                            qa = qa2.rearrange("p (h c) -> p h c", h=4)
                            rq = [PB[bq[0]], PB[bq[1]]]
                            for h in range(4):
                                P.op("act", f_act(junkk, qa[:, h, 0:128], AF.Square, scale=128.0 ** -0.5, accum=stq[:, 20 + h:21 + h]),
                                     reads=rq, writes=[b_junkk, b_stq])
                                P.op("act", f_act(junkk[:, 0:64], qa[:, h, 128:192], AF.Square, scale=64.0 ** -0.5, accum=stq[:, 24 + h:25 + h]),
                                     reads=rq, writes=[b_junkk, b_stq])
                            rstd_from_ms(stq, b_stq, 20, 8, n=8)
                            P.op("dve", f_tt(qn, qa[:, :, 0:128], bc3(stq[:, 8:12], [128, 4, 128]), ALU.mult), reads=rq + [b_stq], writes=[b_qn])
                            P.op("dve", f_tt(qr, qa[:, :, 128:192], bc3(stq[:, 12:16], [128, 4, 64]), ALU.mult), reads=rq + [b_stq], writes=[b_qr])
                            PS.rel(bq)
                            P.op("dve", f_tt(qr, qr, gqr[:, l, :].unsqueeze(1).to_broadcast([128, 4, 64]), ALU.mult),
                                 reads=[b_qr, b_ggrp], writes=[b_qr])
                            cT4 = cT.unsqueeze(1).to_broadcast([128, 4, 32])
                            sT4 = sT.unsqueeze(1).to_broadcast([128, 4, 32])
                            P.op("dve", f_tt(qt4[:, 0], qr[:, :, 0:32], cT4, ALU.mult), reads=[b_qr, b_cos], writes=[b_qt4])
                            P.op("dve", f_tt(qt4[:, 1], qr[:, :, 32:64], sT4, ALU.mult), reads=[b_qr, b_sin], writes=[b_qt4])
                            P.op("dve", f_tt(qrot[:, :, 0:32], qt4[:, 0], qt4[:, 1], ALU.subtract), reads=[b_qt4], writes=[b_qrot])
                            P.op("dve", f_tt(qt4[:, 0], qr[:, :, 32:64], cT4, ALU.mult), reads=[b_qr, b_cos], writes=[b_qt4])
                            P.op("dve", f_tt(qt4[:, 1], qr[:, :, 0:32], sT4, ALU.mult), reads=[b_qr, b_sin], writes=[b_qt4])
                            P.op("dve", f_tt(qrot[:, :, 32:64], qt4[:, 0], qt4[:, 1], ALU.add), reads=[b_qt4], writes=[b_qrot])
                            bT = PS.get()
                            pT = pbank_bf(bT[0]).rearrange("p (a b) -> p a b", a=8)
                            qrot2 = qrot.rearrange("p (i a) b -> p i (a b)", i=2)
                            P.op("pe", f_tr([(pT[:, h, :], qn[:, h, :]) for h in range(4)] +
                                            [(pT[:, 4 + i, :], qrot2[:, i, :]) for i in range(2)], identb),
                                 reads=[b_qn, b_qrot, b_identb], writes=[PB[bT[0]]])
                            P.op("dve", f_ts(qnT[:, :, tc_], pT[:, 0:4, :], misc[:, l, 1:2], ALU.mult, QSC, ALU.mult),
                                 reads=[PB[bT[0]], b_ggrp], writes=[b_qnT])
                            P.op("act", f_act(qrT[:, :, tc_], pT[:, 4:6, :], AF.Identity, scale=QSC), reads=[PB[bT[0]]], writes=[b_qrT])
                            PS.rel(bT)
                            bkv = PS.get(2)
                            P.op("pe", f_mm([(pbank(bkv[hf]), [(ckvnT, wukv[:, hf * 512:(hf + 1) * 512])]) for hf in range(2)]),
                                 reads=[b_ckvnT, b_wukv], writes=[PB[bkv[0]], PB[bkv[1]]])
                            kv = psum[:, bkv[0]:bkv[0] + 2, :].rearrange("p a (h c) -> p (a h) c", c=256)
                            rkv = [PB[bkv[0]], PB[bkv[1]]]
                            for h in range(4):
                                P.op("act", f_act(junkk, kv[:, h, 0:128], AF.Square, scale=128.0 ** -0.5, accum=stq[:, 28 + h:29 + h]),
                                     reads=rkv, writes=[b_junkk, b_stq])
                            rstd_from_ms(stq, b_stq, 28, 16, n=4)
                            P.op("dve", f_tt(kn, kv[:, :, 0:128], bc3(stq[:, 16:20], [128, 4, 128]), ALU.mult), reads=rkv + [b_stq], writes=[b_kn])
                            P.op("act", f_act(va[:, T, :, 0:128], kv[:, :, 128:256], AF.Copy), reads=rkv, writes=[b_va])
                            PS.rel(bkv)
                            bT = PS.get()
                            pT = pbank_bf(bT[0])[:, 0:512].rearrange("p (a b) -> p a b", a=4)
                            P.op("pe", f_tr([(pT[:, h, :], kn[:, h, :]) for h in range(4)], identb),
                                 reads=[b_kn, b_identb], writes=[PB[bT[0]]])
                            P.op("dve", f_ts(knT[:, :, tc_], pT, misc[:, l, 2:3], ALU.mult), reads=[PB[bT[0]], b_ggrp], writes=[b_knT])
                            PS.rel(bT)
                        strK = P.end()
                        P.merge([strM, strK])
                    PS = PS_K
                    if do_mlstm:
                        emit_Y(NT - 1)
                    PS = PS_all

                if do_mla:
                    load_w("pool", wout, b_wout, dr["w_out"][l, 512:1024, :].rearrange("(k p) n -> p k n", p=128))
                    SB.reset()
                    LOOKAHEAD = 2
                    ePs = [SB.get("eP%d" % i, (512,), BF16) for i in range(LOOKAHEAD + 2)]
                    ya, b_ya = SB.get("ya", (4, 128), BF16)
                    yaT, b_yaT = SB.get("yaT", (4, 512), BF16)
                    stb, b_stb = SB.get("stb", (16,), F32)
                    junk2, b_junk2 = SB.get("junk2", (128,), BF16)
                    epi = [0]
                    PS_all = PS
                    PS = PSM([4, 5])
                    BT_ = 7
                    BY_ = 6

                    def rec_N(qg, h, bO):
                        P.begin()
                        r_ = h % 2
                        ip = h // 2

                        def Oq(j):
                            return psum[:, bO[0] + j // 2, (j % 2) * 129:(j % 2) * 129 + 129]
                        nkb = 4 * qg + 4

                        def emit_S(kb):
                            j0 = max(0, kb - 4 * qg)
                            n0 = j0 * 128
                            kc_ = slice(kb * 128, (kb + 1) * 128)
                            bS = PS.get()
                            pS = pbank(bS[0])
                            P.op("pe", f_mm([(pS[:, n0:512], [(knT[:, h, kc_], qnT[:, h, qg * 512 + n0:(qg + 1) * 512]),
                                                               (krT[r_ * 64:(r_ + 1) * 64, kc_],
                                                                qrT[r_ * 64:(r_ + 1) * 64, ip, qg * 512 + n0:(qg + 1) * 512])])]),
                                 reads=[b_knT, b_qnT, b_krT, b_qrT], writes=[PB[bS[0]]])
                            eP, b_eP = ePs[epi[0] % len(ePs)]
                            epi[0] += 1
                            P.op("act", f_act(eP[:, n0:512], pS[:, n0:512], AF.Exp), reads=[PB[bS[0]]], writes=[b_eP])
                            PS.rel(bS)
                            if kb >= 4 * qg:
                                P.op("dve", f_tt(eP[:, n0:n0 + 128], eP[:, n0:n0 + 128], m01b, ALU.mult),
                                     reads=[b_eP, b_m01b], writes=[b_eP])
                            return (kb, j0, eP, b_eP)

                        def emit_PV(item):
                            kb, j0, eP, b_eP = item
                            grp = []
                            for j in range(j0, 4):
                                qb = 4 * qg + j
                                grp.append((Oq(j), eP[:, j * 128:(j + 1) * 128], va[:, kb, h, :],
                                            (kb == 0 and j % 2 == 0), kb == qb))

                            def pv(e, grp=grp):
                                ins = None
                                for (o_, l_, r2, st_, sp_) in grp:
                                    ins = e.matmul(o_, lhsT=l_, rhs=r2, start=st_, stop=sp_, skip_group_check=True)
                                return ins
                            P.op("pe", pv, reads=[b_eP, b_va], writes=[PB[bO[0]], PB[bO[1]]])
                        pend_s = []
                        for kb in range(nkb):
                            pend_s.append(emit_S(kb))
                            if len(pend_s) > LOOKAHEAD:
                                emit_PV(pend_s.pop(0))
                        while pend_s:
                            emit_PV(pend_s.pop(0))
                        return P.end()

                    def rec_E(qg, h, bO):
                        P.begin()

                        def Oq(j):
                            return psum[:, bO[0] + j // 2, (j % 2) * 129:(j % 2) * 129 + 129]
                        rO = [PB[bO[0]], PB[bO[1]]]
                        den = stb[:, 0:4]
                        den2 = den.rearrange("p (a b) -> p a b", b=2)
                        P.op("act", f_act(den2[:, :, 0], psum[:, bO[0]:bO[0] + 2, 128], AF.Copy), reads=rO, writes=[b_stb])
                        P.op("act", f_act(den2[:, :, 1], psum[:, bO[0]:bO[0] + 2, 257], AF.Copy), reads=rO, writes=[b_stb])
                        P.op("dve", f_recip(stb[:, 4:8], den), reads=[b_stb], writes=[b_stb])
                        for j in range(4):
                            P.op("act", f_act(junk2, Oq(j)[:, 0:128], AF.Square, scale=stb[:, 4 + j:5 + j], accum=stb[:, 8 + j:9 + j]),
                                 reads=rO + [b_stb], writes=[b_junk2, b_stb])
                        P.op("act", f_act(stb[:, 12:16], stb[:, 8:12], AF.Ln, scale=1.0 / 128.0, bias=EPS), reads=[b_stb], writes=[b_stb])
                        P.op("act", f_act(stb[:, 12:16], stb[:, 12:16], AF.Exp, scale=-0.5), reads=[b_stb], writes=[b_stb])
                        P.op("dve", f_tt(stb[:, 12:16], stb[:, 12:16], stb[:, 4:8], ALU.mult), reads=[b_stb], writes=[b_stb])
                        for j in range(4):
                            P.op("act", f_act(ya[:, j, :], Oq(j)[:, 0:128], AF.Identity, scale=stb[:, 12 + j:13 + j]),
                                 reads=rO + [b_stb], writes=[b_ya])
                        pT = pbank_bf(BT_)[:, 0:512]
                        P.op("pe", f_tr([(pT[:, j * 128:(j + 1) * 128], ya[:, j, :]) for j in range(4)], identb),
                             reads=[b_ya, b_identb], writes=[PB[BT_]])
                        P.op("dve", f_ts(yaT[:, h, :], pT, aog[:, l, h:h + 1], ALU.mult), reads=[PB[BT_], b_ggrp], writes=[b_yaT])
                        return P.end()

                    def rec_W(qg):
                        P.begin()
                        for j in range(4):
                            T = 4 * qg + j
                            for hf in range(2):
                                P.op("pe", f_mm([(pbank(BY_), [(yaT[:, h, j * 128:(j + 1) * 128], wout[:, h, hf * 512:(hf + 1) * 512])
                                                               for h in range(4)])]),
                                     reads=[b_yaT, b_wout], writes=[PB[BY_]])
                                P.op("dve", f_tt(X[:, T, hf * 512:(hf + 1) * 512], X[:, T, hf * 512:(hf + 1) * 512], pbank(BY_), ALU.add),
                                     reads=[XB[T], PB[BY_]], writes=[XB[T]])
                        return P.end()

                    prev = None
                    u = 0
                    for qg in range(4):
                        for h in range(4):
                            bO = [0, 1] if u % 2 == 0 else [2, 3]
                            u += 1
                            strN = rec_N(qg, h, bO)
                            P.merge([prev, strN] if prev else [strN])
                            prev = rec_E(qg, h, bO)
                            if h == 3:
                                prev = prev + rec_W(qg)
                    P.merge([prev])
                    PS = PS_all

                if do_xattn:
                    load_w("pool", wqx, b_wqx, dr["wq_x"][l].rearrange("(k p) n -> p k n", p=128))
                    load_w("pool", wkvx, b_wkvx, dr["wkv_x"][l].rearrange("(k p) n -> p k n", p=128))
                    load_w("pool", wox, b_wox, dr["wo_x"][l].rearrange("(k p) n -> p k n", p=128))
                    RB.reset()
                    memf, b_memf = RB.get("memf", (2, D), F32)
                    memT, b_memT = RB.get("memT", (2, 8, 128), BF16)
                    xkT, b_xkT = RB.get("xkT", (4, 256), BF16)
                    xv, b_xv = RB.get("xv", (2, 512), BF16)
                    hTg, b_hTg = RB.get("hTg", (4, 8, 128), BF16)
                    xqT, b_xqT = RB.get("xqT", (4, 512), BF16)
                    oT, b_oT = RB.get("oT", (4, 512), BF16)
                    lnS, b_lnS = RB.get("lnS", (512,), F32)
                    ePx = [RB.get("ePx%d" % i, (512,), BF16) for i in range(2)]
                    xkn, b_xkn = RB.get("xkn", (4, 128), BF16)
                    SB.reset()
                    xnb, b_xnb = SB.get("xnb", (D,), BF16)
                    stv, b_st = SB.get("st", (32,), F32)
                    junk3, b_junk3 = SB.get("junk3", (128,), BF16)
                    qscr = [SB.get("xnbq%d" % i, (D,), BF16) + SB.get("stq%d" % i, (32,), F32) for i in range(2)]
                    qjk = [SB.get("junkq%d" % i, (128,), BF16) for i in range(2)]
                    qxk = [RB.get("xknq%d" % i, (4, 128), BF16) for i in range(2)]
                    P.dma("sp", (lambda e, s, sq=sq: e.dma_start(out=memf, in_=dr["mem"][sq].rearrange("(m p) d -> p m d", p=128)).then_inc(s, 16)),
                          writes=[b_memf])
                    PS_Call = PS
                    PS = PSM([0, 1, 2, 3, 4])
                    P.begin()
                    for m in range(2):
                        norm_to_hT(memf[:, m, :], b_memf, gcol[:, l, 2, :], memT[:, m], b_memT, (xnb, b_xnb, stv, b_st))
                    for m in range(2):
                        bkv = PS.get(2)
                        P.op("pe", f_mm([(pbank(bkv[hf]), [(memT[:, m, k, :], wkvx[:, k, hf * 512:(hf + 1) * 512]) for k in range(8)])
                                         for hf in range(2)]),
                             reads=[b_memT, b_wkvx], writes=[PB[bkv[0]], PB[bkv[1]]])
                        pK4 = pbank(bkv[0]).rearrange("p (h d) -> p h d", h=4)
                        for h in range(4):
                            P.op("act", f_act(junk3, pK4[:, h, :], AF.Square, scale=XSC, accum=stv[:, 8 + h:9 + h]),
                                 reads=[PB[bkv[0]]], writes=[b_junk3, b_st])
                        rstd_from_ms(stv, b_st, 8, 12, n=4)
                        P.op("dve", f_tt(xkn, pK4, bc3(stv[:, 12:16], [128, 4, 128]), ALU.mult), reads=[PB[bkv[0]], b_st], writes=[b_xkn])
                        P.op("act", f_act(xv[:, m, :], pbank(bkv[1]), AF.Copy), reads=[PB[bkv[1]]], writes=[b_xv])
                        PS.rel(bkv)
                        bT = PS.get()
                        pT = pbank_bf(bT[0])[:, 0:512].rearrange("p (a b) -> p a b", a=4)
                        P.op("pe", f_tr([(pT[:, h, :], xkn[:, h, :]) for h in range(4)], identb),
                             reads=[b_xkn, b_identb], writes=[PB[bT[0]]])
                        P.op("dve", f_ts(xkT[:, :, m * 128:(m + 1) * 128], pT, misc[:, l, 4:5], ALU.mult),
                             reads=[PB[bT[0]], b_ggrp], writes=[b_xkT])
                        PS.rel(bT)
                    strKV = P.end()
                    PS_Q = PSM([5, 6, 7])
                    PS_AT = PSM([0, 1, 2, 3, 4])
                    hTgs = [(hTg, b_hTg), RB.get("hTg1", (4, 8, 128), BF16)]
                    xqTs = [(xqT, b_xqT), RB.get("xqT1", (4, 512), BF16)]

                    def rec_Q(g):
                        hTg_, b_hTg_ = hTgs[g % 2]
                        xqT_, b_xqT_ = xqTs[g % 2]
                        P.begin()
                        for tt in range(4):
                            T = 4 * g + tt
                            xq_, bxq_, sq_, bsq_ = qscr[tt % 2]
                            jq_, bjq_ = qjk[tt % 2]
                            xkq_, bxkq_ = qxk[tt % 2]
                            norm_to_hT(X[:, T, :], XB[T], gcol[:, l, 1, :], hTg_[:, tt], b_hTg_, (xq_, bxq_, sq_, bsq_))
                            bq = PS.get()
                            pQ4 = pbank(bq[0]).rearrange("p (h d) -> p h d", h=4)
                            P.op("pe", f_mm([(pbank(bq[0]), [(hTg_[:, tt, k, :], wqx[:, k, :]) for k in range(8)])]),
                                 reads=[b_hTg_, b_wqx], writes=[PB[bq[0]]])
                            for h in range(4):
                                P.op("act", f_act(jq_, pQ4[:, h, :], AF.Square, scale=XSC, accum=sq_[:, 16 + h:17 + h]),
                                     reads=[PB[bq[0]]], writes=[bjq_, bsq_])
                            rstd_from_ms(sq_, bsq_, 16, 20, n=4)
                            P.op("dve", f_tt(xkq_, pQ4, bc3(sq_[:, 20:24], [128, 4, 128]), ALU.mult), reads=[PB[bq[0]], bsq_], writes=[bxkq_])
                            PS.rel(bq)
                            bT = PS.get()
                            pT = pbank_bf(bT[0])[:, 0:512].rearrange("p (a b) -> p a b", a=4)
                            P.op("pe", f_tr([(pT[:, h, :], xkq_[:, h, :]) for h in range(4)], identb),
                                 reads=[bxkq_, b_identb], writes=[PB[bT[0]]])
                            P.op("dve", f_ts(xqT_[:, :, tt * 128:(tt + 1) * 128], pT, misc[:, l, 3:4], ALU.mult, XSC, ALU.mult),
                                 reads=[PB[bT[0]], b_ggrp], writes=[b_xqT_])
                            PS.rel(bT)
                        return P.end()

                    def rec_AT(g):
                        xqT_, b_xqT_ = xqTs[g % 2]
                        P.begin()
                        for h in range(4):
                            for m in range(2):
                                bS = PS.get()
                                P.op("pe", f_mm([(pbank(bS[0]), [(xkT[:, h, m * 128:(m + 1) * 128], xqT_[:, h, :])])]),
                                     reads=[b_xkT, b_xqT_], writes=[PB[bS[0]]])
                                P.op("act", f_act(ePx[m][0], pbank(bS[0]), AF.Exp), reads=[PB[bS[0]]], writes=[ePx[m][1]])
                                PS.rel(bS)
                            bO = PS.get()
                            bSm = PS.get()
                            P.op("pe", f_mm([(pbank(bO[0]), [(xv[:, m, h * 128:(h + 1) * 128], ePx[m][0]) for m in range(2)]),
                                             (pbank(bSm[0]), [(onesb, ePx[m][0]) for m in range(2)])]),
                                 reads=[b_xv, b_onesb, ePx[0][1], ePx[1][1]], writes=[PB[bO[0]], PB[bSm[0]]])
                            P.op("act", f_act(lnS, pbank(bSm[0]), AF.Ln), reads=[PB[bSm[0]]], writes=[b_lnS])
                            PS.rel(bSm)
                            P.op("act", f_act(lnS, lnS, AF.Exp, scale=-1.0), reads=[b_lnS], writes=[b_lnS])
                            P.op("dve", f_tt(oT[:, h, :], pbank(bO[0]), lnS, ALU.mult), reads=[PB[bO[0]], b_lnS], writes=[b_oT])
                            PS.rel(bO)
                        for tt in range(4):
                            T = 4 * g + tt
                            bY = PS.get(2)
                            P.op("pe", f_mm([(pbank(bY[0] + hf), [(oT[:, h, tt * 128:(tt + 1) * 128], wox[:, h, hf * 512:(hf + 1) * 512])
                                                                  for h in range(4)]) for hf in range(2)]),
                                 reads=[b_oT, b_wox], writes=[PB[bY[0]], PB[bY[1]]])
                            resid_add(T, psum[:, bY[0]:bY[0] + 2, :].rearrange("p a b -> p (a b)"), bY)
                            PS.rel(bY)
                        return P.end()

                    PS = PS_Q
                    strQ = rec_Q(0)
                    P.merge([strKV, strQ])
                    for g in range(4):
                        PS = PS_AT
                        strs = [rec_AT(g)]
                        if g < 3:
                            PS = PS_Q
                            strs.append(rec_Q(g + 1))
                        P.merge(strs)
                    PS = PS_Call

                if do_ffn:
                    RB.reset()
                    hTall, b_hTall0 = RB.get("hTall", (NT, 8, 128), BF16)
                    b_hTt = []
                    for T in range(NT):
                        _, bb = A.view("hTall%d" % T, R1_LO + T * 2048, (1024,), BF16)
                        b_hTt.append(bb)
                    SB.reset()
                    dscr = [SB.get("xnbd%d" % i, (D,), BF16) + SB.get("std%d" % i, (32,), F32) for i in range(3)]
                    h1s = [SB.get("h1T%d" % i, (FSL // 128, 512), BF16) for i in range(2)]
                    rls = [SB.get("rl%d" % i, (512,), BF16) for i in range(2)]

                    def load_slice(j):
                        w1s, b1, w2s, b2 = fslots[fslice_ctr[0] % 3]
                        fslice_ctr[0] += 1
                        load_w("pool", w1s, b1, dr["w_ff1"][l][:, j * FSL:(j + 1) * FSL].rearrange("(k p) n -> p k n", p=128))
                        load_w("pool", w2s, b2, dr["w_ff2"][l][j * FSL:(j + 1) * FSL, :].rearrange("(c p) n -> p c n", p=128))
                        return (w1s, b1, w2s, b2)
                    pend = [load_slice(0), load_slice(1)]
                    for T in range(NT):
                        norm_to_hT(X[:, T, :], XB[T], gcol[:, l, 3, :], hTall[:, T], b_hTt[T], dscr[T % 3])
                    it = 0
                    for j in range(NSL):
                        w1s, b1, w2s, b2 = pend.pop(0)
                        for g in range(4):
                            h1T, b_h1T = h1s[it % 2]
                            it += 1
                            for fc in range(FSL // 128):
                                bH1 = PS.get()
                                pH1 = pbank(bH1[0])
                                P.op("pe", f_mm([(pH1.rearrange("p (a b) -> p a b", a=4),
                                                  [(w1s[:, k, fc * 128:(fc + 1) * 128], hTall[:, 4 * g:4 * g + 4, k, :]) for k in range(8)])]),
                                     reads=[b1] + b_hTt[4 * g:4 * g + 4], writes=[PB[bH1[0]]])
                                rl, b_rl = rls[fc % 2]
                                P.op("act", f_act(rl, pH1, AF.Relu), reads=[PB[bH1[0]]], writes=[b_rl])
                                P.op("dve", f_tt(h1T[:, fc, :], pH1, rl, ALU.mult), reads=[PB[bH1[0]], b_rl], writes=[b_h1T])
                                PS.rel(bH1)
                            for tt in range(4):
                                T = 4 * g + tt
                                bY = PS.get(2)
                                P.op("pe", f_mm([(pbank(bY[0] + hf), [(h1T[:, fc, tt * 128:(tt + 1) * 128], w2s[:, fc, hf * 512:(hf + 1) * 512])
                                                                      for fc in range(FSL // 128)]) for hf in range(2)]),
                                     reads=[b_h1T, b2], writes=[PB[bY[0]], PB[bY[1]]])
                                resid_add(T, psum[:, bY[0]:bY[0] + 2, :].rearrange("p a b -> p (a b)"), bY)
                                PS.rel(bY)
                                if j == NSL - 1 and l == depth - 1:
                                    P.dma("sp", (lambda e, s, T=T, sq=sq: e.dma_start(out=out_d[sq, T * 128:(T + 1) * 128, :], in_=X[:, T, :]).then_inc(s, 16)),
                                          reads=[XB[T]])
                        if j + 2 < NSL:
                            pend.append(load_slice(j + 2))
                elif l == depth - 1:
                    for T in range(NT):
                        P.dma("sp", (lambda e, s, T=T, sq=sq: e.dma_start(out=out_d[sq, T * 128:(T + 1) * 128, :], in_=X[:, T, :]).then_inc(s, 16)),
                              reads=[XB[T]])
        P.op("sp", None, reads=[], writes=XB)
        P.emit(st)
    return nc


def _consts():
    idx = np.arange(128)
    triu = (idx[:, None] <= idx[None, :]).astype(np.float32)
    trisl = (idx[:, None] > idx[None, :]).astype(np.float32)
    mneg = ((1.0 - triu) * -30000.0).astype(np.float32)
    inv = (1.0 / (10000.0 ** (np.arange(0, 64, 2, dtype=np.float32) / 64.0))).astype(np.float32)
    invf = np.broadcast_to((inv / np.float32(2.0 * np.pi)).astype(np.float32)[None, :], (128, 32)).copy()
    return {"c_ident": np.eye(128, dtype=np.float32), "c_triu": triu, "c_trisl": trisl, "c_mneg": mneg, "c_invf": invf}


_NC_CACHE = {}


def kernel(**inputs):
    x = np.ascontiguousarray(inputs["x"], dtype=np.float32)
    mem = np.ascontiguousarray(inputs["mem"], dtype=np.float32)
    pos = np.ascontiguousarray(inputs["positions"], dtype=np.int32)
    if "nc" not in _NC_CACHE:
        _NC_CACHE["nc"] = build_program()
    nc = _NC_CACHE["nc"]
    cst = _consts()
    in_maps = []
    for c in range(NCORES):
        sl = slice(c * SEQ_PER_CORE, (c + 1) * SEQ_PER_CORE)
        m = {"x": x[sl], "mem": mem[sl], "pos": pos[sl]}
        for n in WNAMES:
            m[n] = np.ascontiguousarray(inputs[n], dtype=np.float32)
        m.update(cst)
        in_maps.append(m)
    res = run_bass_kernel_spmd(nc, in_maps, core_ids=list(range(NCORES)))
    out = np.concatenate([np.asarray(r["out"]) for r in res.results], axis=0)
    return out.astype(np.float32)
```
